# Optimizing a Trainium2 kernel written in Bass

```python
import math
import jax
import jax.numpy as jnp
from jax import lax
import numpy as np

D_MODEL = 2048
BATCH = 2
SEQ = 16384
DEPTH = 2

DEEPNORM_ALPHA = (2 * DEPTH) ** 0.25
DEEPNORM_BETA = (8 * DEPTH) ** -0.25
LN_EPS = 1e-5
RMS_EPS = 1e-6

N_BRANCHES = 3
BRANCH_WIDTH = D_MODEL
GATE_COLS = N_BRANCHES * BRANCH_WIDTH

SSD_HEAD_DIM = 64
SSD_HEADS = BRANCH_WIDTH // SSD_HEAD_DIM
SSD_GROUPS = 4
SSD_HPG = SSD_HEADS // SSD_GROUPS
SSD_STATE = 128
SSD_CONV = 4
SSD_CHUNK = 128
SSD_WIDTH = SSD_HEADS * SSD_HEAD_DIM
SSD_BC = SSD_GROUPS * SSD_STATE
SSD_CONV_CH = SSD_WIDTH + 2 * SSD_BC
SSD_COLS = SSD_WIDTH + SSD_CONV_CH + SSD_HEADS

RWKV_HEAD = 64
RWKV_WIDTH = BRANCH_WIDTH
RWKV_HEADS = RWKV_WIDTH // RWKV_HEAD
RWKV_DECAY_LORA = 96
RWKV_A_LORA = 96
RWKV_V_LORA = 64
RWKV_G_LORA = 256
RWKV_COLS = 3 * RWKV_WIDTH + RWKV_DECAY_LORA + RWKV_A_LORA + RWKV_G_LORA
RWKV_GN_EPS = 64e-5

MLA_NOPE = 128
MLA_ROPE = 64
MLA_V = 128
MLA_QK = MLA_NOPE + MLA_ROPE
MLA_HEADS = BRANCH_WIDTH // MLA_V
MLA_Q_RANK = 512
MLA_KV_RANK = 512
MLA_COLS = MLA_Q_RANK + MLA_KV_RANK + MLA_ROPE
ROPE_THETA = 10000.0
ATTN_BLOCK = 128

D_FF = 5632
N_EXPERTS = 8
TOP_K = 2
MOE_BLOCK = 256
N_DENSE = (DEPTH + 1) // 2
N_MOE = DEPTH // 2

IN_COLS = GATE_COLS + SSD_COLS + MLA_COLS + RWKV_COLS

kernel_name = 'hybrid_ssd_rwkv7_mla_moe_deepnorm'


def _split(x, sizes):
    return jnp.split(x, [int(s) for s in np.cumsum(sizes)[:-1]], axis=-1)


def _layernorm(x, w, b):
    xf = x.astype(jnp.float32)
    mu = jnp.mean(xf, axis=-1, keepdims=True)
    var = jnp.mean(jnp.square(xf - mu), axis=-1, keepdims=True)
    return ((xf - mu) * lax.rsqrt(var + LN_EPS) * w + b).astype(x.dtype)


def _rmsnorm(x, w):
    xf = x.astype(jnp.float32)
    y = xf * lax.rsqrt(jnp.mean(xf * xf, axis=-1, keepdims=True) + RMS_EPS)
    return (y * w).astype(x.dtype)


def _rope_tables(positions):
    inv_freq = ROPE_THETA ** (-jnp.arange(0, MLA_ROPE, 2, dtype=jnp.float32) / MLA_ROPE)
    ang = positions.astype(jnp.float32)[..., None] * inv_freq
    return jnp.cos(ang), jnp.sin(ang)


def _rope(x, cos, sin):
    x1, x2 = jnp.split(x, 2, axis=-1)
    return jnp.concatenate([x1 * cos - x2 * sin, x2 * cos + x1 * sin], axis=-1).astype(x.dtype)


def _causal_depthwise_conv(x, w, b):
    k_width, ch = w.shape
    y = lax.conv_general_dilated(x, w[:, None, :].astype(x.dtype), window_strides=(1,),
                                 padding=[(k_width - 1, 0)],
                                 dimension_numbers=('NWC', 'WIO', 'NWC'),
                                 feature_group_count=ch)
    return y + b


def _token_shift(p, mu):
    prev = jnp.pad(p, ((0, 0), (1, 0), (0, 0)))[:, :-1]
    return p + (prev - p) * mu


def _swiglu(x, w1, w3, w2):
    return (jax.nn.silu(x @ w1) * (x @ w3)) @ w2


def _ssd_mixer(p, conv_w, conv_b, dt_bias, a_log, d_skip, norm_w):
    bn, sn, _ = p.shape
    nc = sn // SSD_CHUNK
    z, xbc, dt = _split(p, [SSD_WIDTH, SSD_CONV_CH, SSD_HEADS])
    xbc = jax.nn.silu(_causal_depthwise_conv(xbc, conv_w, conv_b))
    xs, b_in, c_in = _split(xbc, [SSD_WIDTH, SSD_BC, SSD_BC])
    xs = xs.astype(jnp.float32).reshape(bn, nc, SSD_CHUNK, SSD_GROUPS, SSD_HPG, SSD_HEAD_DIM)
    b_in = b_in.astype(jnp.float32).reshape(bn, nc, SSD_CHUNK, SSD_GROUPS, SSD_STATE)
    c_in = c_in.astype(jnp.float32).reshape(bn, nc, SSD_CHUNK, SSD_GROUPS, SSD_STATE)
    dt = jax.nn.softplus(dt.astype(jnp.float32) + dt_bias).reshape(bn, nc, SSD_CHUNK, SSD_GROUPS, SSD_HPG)
    a = -jnp.exp(a_log.astype(jnp.float32)).reshape(SSD_GROUPS, SSD_HPG)
    causal = jnp.tril(jnp.ones((SSD_CHUNK, SSD_CHUNK), dtype=bool))[None, :, :, None, None]

    def chunk_step(h, inp):
        xc, dtc, bc, cc = inp
        cum = jnp.cumsum(dtc * a, axis=1)
        seg = cum[:, :, None] - cum[:, None, :]
        decay = jnp.exp(jnp.where(causal, seg, -jnp.inf))
        xdt = xc * dtc[..., None]
        cb = jnp.einsum('bign,bjgn->bijg', cc, bc)
        y = jnp.einsum('bijg,bijge,bjgep->bigep', cb, decay, xdt)
        y = y + jnp.einsum('bign,bgepn->bigep', cc, h) * jnp.exp(cum)[..., None]
        to_end = jnp.exp(cum[:, -1:] - cum)
        h = h * jnp.exp(cum[:, -1])[..., None, None] + jnp.einsum('bjgn,bjge,bjgep->bgepn', bc, to_end, xdt)
        return h, y

    mv = lambda t: jnp.moveaxis(t, 1, 0)
    h0 = jnp.zeros((bn, SSD_GROUPS, SSD_HPG, SSD_HEAD_DIM, SSD_STATE), jnp.float32)
    _, y = lax.scan(chunk_step, h0, (mv(xs), mv(dt), mv(b_in), mv(c_in)))
    y = jnp.moveaxis(y, 0, 1)
    y = y + d_skip.astype(jnp.float32).reshape(SSD_GROUPS, SSD_HPG)[..., None] * xs
    y = y.reshape(bn, sn, SSD_WIDTH) * jax.nn.silu(z.astype(jnp.float32))
    yg = y.reshape(bn, sn, SSD_GROUPS, SSD_WIDTH // SSD_GROUPS)
    yg = yg * lax.rsqrt(jnp.mean(yg * yg, axis=-1, keepdims=True) + RMS_EPS)
    return (yg.reshape(bn, sn, SSD_WIDTH) * norm_w).astype(p.dtype)


def _rwkv7_mixer(p, v_first, w0, w2, a0, a2, g2, k_k, k_a, r_k, ln_w, ln_b, v0, v2):
    bn, sn, _ = p.shape
    sizes = [RWKV_WIDTH, RWKV_WIDTH, RWKV_WIDTH, RWKV_DECAY_LORA, RWKV_A_LORA, RWKV_G_LORA]
    if v2 is not None:
        sizes.append(RWKV_V_LORA)
    parts = _split(p, sizes)
    r, k, v, w_lo, a_lo, g_lo = parts[:6]
    log_w = -jax.nn.softplus(-(w0 + jnp.tanh(w_lo) @ w2).astype(jnp.float32)) - 0.5
    decay = jnp.exp(-jnp.exp(log_w))
    a = jax.nn.sigmoid((a0 + a_lo @ a2).astype(jnp.float32))
    g = jax.nn.sigmoid(g_lo) @ g2
    if v2 is None:
        v_first = v
    else:
        v = v + (v_first - v) * jax.nn.sigmoid(v0 + parts[6] @ v2)
    heads = lambda t: t.astype(jnp.float32).reshape(bn, sn, RWKV_HEADS, RWKV_HEAD)
    kk = heads(k * k_k)
    kk = kk / jnp.maximum(jnp.sqrt(jnp.sum(kk * kk, axis=-1, keepdims=True)), 1e-12)
    k = k * (1.0 + (a - 1.0) * k_a)
    rh, kh, vh, ah, wh = heads(r), heads(k), heads(v), heads(a), heads(decay)

    def step(state, inp):
        r_t, w_t, k_t, v_t, kk_t, a_t = inp
        s_kk = jnp.einsum('bhvk,bhk->bhv', state, kk_t)
        state = (state * w_t[:, :, None, :] - s_kk[..., None] * (kk_t * a_t)[:, :, None, :]
                 + v_t[..., None] * k_t[:, :, None, :])
        return state, jnp.einsum('bhvk,bhk->bhv', state, r_t)

    tm = lambda t: jnp.moveaxis(t, 1, 0)
    s0 = jnp.zeros((bn, RWKV_HEADS, RWKV_HEAD, RWKV_HEAD), jnp.float32)
    _, y = lax.scan(step, s0, (tm(rh), tm(wh), tm(kh), tm(vh), tm(kk), tm(ah)))
    y = jnp.moveaxis(y, 0, 1)
    mu = jnp.mean(y, axis=-1, keepdims=True)
    var = jnp.mean(jnp.square(y - mu), axis=-1, keepdims=True)
    y = ((y - mu) * lax.rsqrt(var + RWKV_GN_EPS)).reshape(bn, sn, RWKV_WIDTH) * ln_w + ln_b
    bonus = jnp.sum(rh * kh * r_k.reshape(RWKV_HEADS, RWKV_HEAD), axis=-1, keepdims=True) * vh
    y = (y + bonus.reshape(bn, sn, RWKV_WIDTH)) * g
    return y.astype(p.dtype), v_first


def _causal_mla_attention(q_nope, q_pe, k_nope, k_pe, v):
    bn, sn = q_nope.shape[:2]
    nb = sn // ATTN_BLOCK
    scale = MLA_QK ** -0.5
    key_idx = jnp.arange(sn)

    def block(args):
        i, qn, qp = args
        s = jnp.einsum('bqhd,bkhd->bhqk', qn, k_nope) + jnp.einsum('bqhr,bkr->bhqk', qp, k_pe)
        s = s.astype(jnp.float32) * scale
        q_idx = i * ATTN_BLOCK + jnp.arange(ATTN_BLOCK)
        s = jnp.where(key_idx[None, :] <= q_idx[:, None], s, -jnp.inf)
        prob = jax.nn.softmax(s, axis=-1).astype(v.dtype)
        return jnp.einsum('bhqk,bkhv->bqhv', prob, v)

    blocks = lambda t: jnp.moveaxis(t.reshape(bn, nb, ATTN_BLOCK, *t.shape[2:]), 1, 0)
    out = lax.map(block, (jnp.arange(nb), blocks(q_nope), blocks(q_pe)))
    return jnp.moveaxis(out, 0, 1).reshape(bn, sn, MLA_HEADS, MLA_V)


def _mla_mixer(p, cos, sin, q_norm_w, w_q_b, kv_norm_w, w_kv_b):
    bn, sn, _ = p.shape
    q_lat, kv_lat, k_pe = _split(p, [MLA_Q_RANK, MLA_KV_RANK, MLA_ROPE])
    q = (_rmsnorm(q_lat, q_norm_w) @ w_q_b).reshape(bn, sn, MLA_HEADS, MLA_QK)
    q_nope, q_pe = _split(q, [MLA_NOPE, MLA_ROPE])
    kv = (_rmsnorm(kv_lat, kv_norm_w) @ w_kv_b).reshape(bn, sn, MLA_HEADS, MLA_NOPE + MLA_V)
    k_nope, v = _split(kv, [MLA_NOPE, MLA_V])
    q_pe = _rope(q_pe, cos[:, :, None, :], sin[:, :, None, :])
    k_pe = _rope(k_pe, cos, sin)
    out = _causal_mla_attention(q_nope, q_pe, k_nope, k_pe, v)
    return out.reshape(bn, sn, MLA_HEADS * MLA_V)


def _moe_swiglu(x, router, w1, w3, w2):
    bn, sn, d = x.shape
    t = bn * sn
    xt = x.reshape(t, d)
    logits = (xt @ router).astype(jnp.float32)
    top_logits, top_idx = lax.top_k(logits, TOP_K)
    top_w = jax.nn.softmax(top_logits, axis=-1)
    flat_e = top_idx.reshape(-1)
    n_assign = t * TOP_K
    order = jnp.argsort(flat_e)
    sorted_e = flat_e[order]
    counts = jnp.bincount(flat_e, length=N_EXPERTS)
    start = jnp.cumsum(counts) - counts
    padded = (counts + MOE_BLOCK - 1) // MOE_BLOCK * MOE_BLOCK
    pad_end = jnp.cumsum(padded)
    pad_start = pad_end - padded
    dest = pad_start[sorted_e] + jnp.arange(n_assign) - start[sorted_e]
    n_blocks = -(-(n_assign + N_EXPERTS * (MOE_BLOCK - 1)) // MOE_BLOCK)
    token_of_row = jnp.full((n_blocks * MOE_BLOCK,), t, dtype=jnp.int32).at[dest].set((order // TOP_K).astype(jnp.int32))
    x_pad = jnp.concatenate([xt, jnp.zeros((1, d), xt.dtype)], axis=0)
    xb = x_pad[token_of_row].reshape(n_blocks, MOE_BLOCK, d)
    block_expert = jnp.minimum(jnp.searchsorted(pad_end, jnp.arange(n_blocks) * MOE_BLOCK, side='right'), N_EXPERTS - 1)

    def expert_block(args):
        xblk, e = args
        return _swiglu(xblk, w1[e], w3[e], w2[e])

    yb = lax.map(expert_block, (xb, block_expert)).reshape(n_blocks * MOE_BLOCK, d)
    contrib = yb[dest].astype(jnp.float32) * top_w.reshape(-1)[order][:, None]
    out = jnp.zeros((t, d), jnp.float32).at[order // TOP_K].add(contrib)
    return out.astype(x.dtype).reshape(bn, sn, d)


def setup_inputs(seed: int = 0) -> dict:
    key = jax.random.key(seed)
    keys = iter(jax.random.split(key, 48))
    f32 = jnp.float32

    def normal(shape, scale):
        return jax.random.normal(next(keys), shape, f32) * scale

    def uniform(shape, lo, hi):
        return jax.random.uniform(next(keys), shape, f32, lo, hi)

    nl, nv = DEPTH, DEPTH - 1
    x = jax.random.normal(next(keys), (BATCH, SEQ, D_MODEL), f32)
    steps = jax.random.randint(next(keys), (BATCH, SEQ), 1, 3)
    offset = jax.random.randint(next(keys), (BATCH, 1), 0, 4096)
    positions = (offset + jnp.cumsum(steps, axis=1) - 1).astype(jnp.int32)
    dt = jnp.exp(uniform((nl, SSD_HEADS), math.log(1e-3), math.log(1e-1)))
    return {
        'x': x,
        'positions': positions,
        'w_in': normal((nl, D_MODEL, IN_COLS), D_MODEL ** -0.5),
        'w_in_vres': normal((nv, D_MODEL, RWKV_V_LORA), D_MODEL ** -0.5),
        'w_out': normal((nl, BRANCH_WIDTH, D_MODEL), DEEPNORM_BETA * BRANCH_WIDTH ** -0.5),
        'ssd_conv_w': normal((nl, SSD_CONV, SSD_CONV_CH), SSD_CONV ** -0.5),
        'ssd_conv_b': normal((nl, SSD_CONV_CH), 0.02),
        'ssd_dt_bias': dt + jnp.log(-jnp.expm1(-dt)),
        'ssd_a_log': jnp.log(uniform((nl, SSD_HEADS), 1.0, 16.0)),
        'ssd_d': 1.0 + normal((nl, SSD_HEADS), 0.1),
        'ssd_norm_w': 1.0 + normal((nl, SSD_WIDTH), 0.02),
        'rwkv_mu': uniform((nl, RWKV_COLS), 0.0, 1.0),
        'rwkv_mu_vres': uniform((nv, RWKV_V_LORA), 0.0, 1.0),
        'rwkv_w0': uniform((nl, RWKV_WIDTH), -6.5, -1.5),
        'rwkv_w2': normal((nl, RWKV_DECAY_LORA, RWKV_WIDTH), 0.5 * RWKV_DECAY_LORA ** -0.5),
        'rwkv_a0': normal((nl, RWKV_WIDTH), 0.1),
        'rwkv_a2': normal((nl, RWKV_A_LORA, RWKV_WIDTH), 0.5 * RWKV_A_LORA ** -0.5),
        'rwkv_g2': normal((nl, RWKV_G_LORA, RWKV_WIDTH), RWKV_G_LORA ** -0.5),
        'rwkv_v0': normal((nv, RWKV_WIDTH), 0.1),
        'rwkv_v2': normal((nv, RWKV_V_LORA, RWKV_WIDTH), 0.5 * RWKV_V_LORA ** -0.5),
        'rwkv_k_k': 0.85 + normal((nl, RWKV_WIDTH), 0.02),
        'rwkv_k_a': 1.0 + normal((nl, RWKV_WIDTH), 0.02),
        'rwkv_r_k': normal((nl, RWKV_WIDTH), 0.1),
        'rwkv_ln_w': 1.0 + normal((nl, RWKV_WIDTH), 0.02),
        'rwkv_ln_b': normal((nl, RWKV_WIDTH), 0.02),
        'mla_q_norm_w': 1.0 + normal((nl, MLA_Q_RANK), 0.02),
        'mla_w_q_b': normal((nl, MLA_Q_RANK, MLA_HEADS * MLA_QK), MLA_Q_RANK ** -0.5),
        'mla_kv_norm_w': 1.0 + normal((nl, MLA_KV_RANK), 0.02),
        'mla_w_kv_b': normal((nl, MLA_KV_RANK, MLA_HEADS * (MLA_NOPE + MLA_V)), MLA_KV_RANK ** -0.5),
        'ln1_w': 1.0 + normal((nl, D_MODEL), 0.02),
        'ln1_b': normal((nl, D_MODEL), 0.02),
        'ln2_w': 1.0 + normal((nl, D_MODEL), 0.02),
        'ln2_b': normal((nl, D_MODEL), 0.02),
        'ffn_w1': normal((N_DENSE, D_MODEL, D_FF), D_MODEL ** -0.5),
        'ffn_w3': normal((N_DENSE, D_MODEL, D_FF), D_MODEL ** -0.5),
        'ffn_w2': normal((N_DENSE, D_FF, D_MODEL), DEEPNORM_BETA * D_FF ** -0.5),
        'moe_router': normal((N_MOE, D_MODEL, N_EXPERTS), D_MODEL ** -0.5),
        'moe_w1': normal((N_MOE, N_EXPERTS, D_MODEL, D_FF), D_MODEL ** -0.5),
        'moe_w3': normal((N_MOE, N_EXPERTS, D_MODEL, D_FF), D_MODEL ** -0.5),
        'moe_w2': normal((N_MOE, N_EXPERTS, D_FF, D_MODEL), DEEPNORM_BETA * D_FF ** -0.5),
    }


def reference(x, positions, w_in, w_in_vres, w_out,
              ssd_conv_w, ssd_conv_b, ssd_dt_bias, ssd_a_log, ssd_d, ssd_norm_w,
              rwkv_mu, rwkv_mu_vres, rwkv_w0, rwkv_w2, rwkv_a0, rwkv_a2, rwkv_g2,
              rwkv_v0, rwkv_v2, rwkv_k_k, rwkv_k_a, rwkv_r_k, rwkv_ln_w, rwkv_ln_b,
              mla_q_norm_w, mla_w_q_b, mla_kv_norm_w, mla_w_kv_b,
              ln1_w, ln1_b, ln2_w, ln2_b,
              ffn_w1, ffn_w3, ffn_w2,
              moe_router, moe_w1, moe_w3, moe_w2):
    cos, sin = _rope_tables(positions)
    v_first = None
    for l in range(DEPTH):
        if l == 0:
            w_cat, mu, v0, v2 = w_in[l], rwkv_mu[l], None, None
        else:
            w_cat = jnp.concatenate([w_in[l], w_in_vres[l - 1]], axis=1)
            mu = jnp.concatenate([rwkv_mu[l], rwkv_mu_vres[l - 1]], axis=0)
            v0, v2 = rwkv_v0[l - 1], rwkv_v2[l - 1]
        proj = x @ w_cat
        gates, p_ssd, p_mla, p_rwkv = jnp.split(
            proj, [GATE_COLS, GATE_COLS + SSD_COLS, GATE_COLS + SSD_COLS + MLA_COLS], axis=-1)
        y_ssd = _ssd_mixer(p_ssd, ssd_conv_w[l], ssd_conv_b[l], ssd_dt_bias[l], ssd_a_log[l],
                           ssd_d[l], ssd_norm_w[l])
        y_rwkv, v_first = _rwkv7_mixer(_token_shift(p_rwkv, mu), v_first, rwkv_w0[l], rwkv_w2[l],
                                       rwkv_a0[l], rwkv_a2[l], rwkv_g2[l], rwkv_k_k[l], rwkv_k_a[l],
                                       rwkv_r_k[l], rwkv_ln_w[l], rwkv_ln_b[l], v0, v2)
        y_mla = _mla_mixer(p_mla, cos, sin, mla_q_norm_w[l], mla_w_q_b[l], mla_kv_norm_w[l], mla_w_kv_b[l])
        g_ssd, g_rwkv, g_mla = jnp.split(jax.nn.sigmoid(gates), N_BRANCHES, axis=-1)
        merged = g_ssd * y_ssd + g_rwkv * y_rwkv + g_mla * y_mla
        x = _layernorm(DEEPNORM_ALPHA * x + merged @ w_out[l], ln1_w[l], ln1_b[l])
        if l % 2 == 0:
            f = _swiglu(x, ffn_w1[l // 2], ffn_w3[l // 2], ffn_w2[l // 2])
        else:
            f = _moe_swiglu(x, moe_router[l // 2], moe_w1[l // 2], moe_w3[l // 2], moe_w2[l // 2])
        x = _layernorm(DEEPNORM_ALPHA * x + f, ln2_w[l], ln2_b[l])
    return x
```

```python
import numpy as np
import concourse.bass as bass
import concourse.mybir as mybir
from concourse.bass_utils import run_bass_kernel_spmd

F32 = mybir.dt.float32; BF16 = mybir.dt.bfloat16; I32 = mybir.dt.int32
AF = mybir.ActivationFunctionType; ALU = mybir.AluOpType; AX = mybir.AxisListType

D = 2048; DC = 16; DFF = 5632; FC = 44; NEXP = 8
ALPHA = 4 ** 0.25
LN_EPS = 1e-5


class Res:
    __slots__ = ("lw", "rs")
    def __init__(self):
        self.lw = None
        self.rs = []


class MK:
    ENG = ("pe", "act", "dve", "pool", "sp")
    def __init__(self, nc):
        self.nc = nc
        self.h = {"pe": nc.tensor, "act": nc.scalar, "dve": nc.vector, "pool": nc.gpsimd, "sp": nc.sync}
        self.sems = {}; self.cnt = {}
        self.seen = {e: {} for e in self.ENG}
        self.res = {}; self.stack = []; self.tstack = []; self.psum_keys = set()
        self.nins = {e: 0 for e in self.ENG}; self.nwait = 0
        for e in self.ENG:
            self._sem(("eng", e))
    def _sem(self, key):
        if key not in self.sems:
            cm = self.nc.semaphore("s_" + "_".join(str(k) for k in key))
            self.sems[key] = cm.__enter__(); self.stack.append(cm); self.cnt[key] = 0
        return self.sems[key]
    def sb(self, name, shape, dt=F32):
        cm = self.nc.sbuf_tensor(name, list(shape), dt); t = cm.__enter__(); self.tstack.append(cm); return t
    def ps(self, name, shape, dt=F32):
        cm = self.nc.psum_tensor(name, list(shape), dt); t = cm.__enter__(); self.tstack.append(cm); self.psum_keys.add(name); return t
    def mark(self):
        return len(self.tstack)
    def release(self, mark):
        self.barrier()
        while len(self.tstack) > mark:
            self.tstack.pop().__exit__(None, None, None)
    def barrier(self):
        for e in self.ENG:
            self.finish(e)
    def R(self, key):
        r = self.res.get(key)
        if r is None:
            r = self.res[key] = Res()
        return r
    def _deps(self, eng, reads, writes):
        deps = {}
        def add(tok):
            if tok is None: return
            k, v = tok
            if eng == "pe" and k == ("eng", "pe"): return
            if deps.get(k, 0) < v: deps[k] = v
        for r in reads: add(self.R(r).lw)
        for w in writes:
            rr = self.R(w); add(rr.lw)
            for t in rr.rs: add(t)
        seen = self.seen[eng]
        for k, v in deps.items():
            if seen.get(k, 0) >= v: continue
            self.h[eng].wait_ge(self.sems[k], v); seen[k] = v; self.nwait += 1
    def _commit(self, tok, reads, writes):
        for r in reads:
            rr = self.R(r); rr.rs.append(tok)
            if len(rr.rs) > 64:
                mx = {}
                for k, v in rr.rs:
                    if mx.get(k, 0) < v: mx[k] = v
                rr.rs = list(mx.items())
        for w in writes:
            rr = self.R(w); rr.lw = tok; rr.rs = []
    def op(self, eng, fn, reads=(), writes=()):
        px = [r for r in reads if r in self.psum_keys]
        if px:
            writes = list(writes) + px
        self._deps(eng, reads, writes)
        ins = fn(self.h[eng])
        k = ("eng", eng); self.cnt[k] += 1
        ins.then_inc(self.sems[k], 1)
        self.nins[eng] += 1
        self._commit((k, self.cnt[k]), reads, writes)
        return ins
    def dma(self, eng, out, in_, reads=(), writes=(), semkey=None, **kw):
        k = ("dma", semkey)
        self._sem(k)
        prev = (k, self.cnt[k]) if self.cnt[k] else None
        self._deps(eng, reads, writes)
        if prev is not None and self.seen[eng].get(k, 0) < prev[1]:
            self.h[eng].wait_ge(self.sems[k], prev[1]); self.seen[eng][k] = prev[1]; self.nwait += 1
        ins = self.h[eng].dma_start(out=out, in_=in_, **kw)
        self.cnt[k] += 16
        ins.then_inc(self.sems[k], 16)
        self._commit((k, self.cnt[k]), reads, writes)
        return ins
    def finish(self, eng="sp"):
        for k, v in self.cnt.items():
            if v and self.seen[eng].get(k, 0) < v:
                self.h[eng].wait_ge(self.sems[k], v); self.seen[eng][k] = v
    def close(self):
        for cm in reversed(self.tstack):
            cm.__exit__(None, None, None)
        self.tstack = []
        for cm in reversed(self.stack):
            cm.__exit__(None, None, None)
        self.stack = []


def lay_w(w):
    K, N = w.shape
    return np.ascontiguousarray(w.reshape(K // 128, 128, N // 128, 128).transpose(2, 1, 0, 3))


def lay_vec(v):
    return np.ascontiguousarray(v.reshape(-1, 128).T)


def build_ffn(NT, n_exp, TB=512):
    moe = n_exp > 0
    NE = max(n_exp, 1)
    nc = bass.Bass("TRN2", target_bir_lowering=False)
    m = MK(nc)
    dr = lambda name, shape, kind="ExternalInput", dt=F32: nc.dram_tensor(name, list(shape), dt, kind=kind).ap()
    xT = dr("xT", [D, NT]); mTs = [dr("m%d" % i, [D, NT]) for i in range(3)]
    wout = dr("wout", [DC, 128, DC, 128])
    w1 = dr("w1", [NE, FC, 128, DC, 128]); w3 = dr("w3", [NE, FC, 128, DC, 128]); w2 = dr("w2", [NE, DC, 128, FC, 128])
    lnp = dr("lnp", [128, 4, DC])
    ident_d = dr("ident", [128, 128])
    if moe:
        router_d = dr("router", [128, DC, NEXP]); sel_d = dr("sel", [NEXP, NEXP, 128])
    yT = dr("yT", [D, NT], kind="ExternalOutput")
    xT_v = xT.rearrange("(c p) t -> p c t", p=128); yT_v = yT.rearrange("(c p) t -> p c t", p=128)
    mT_v = [a.rearrange("(c p) t -> p c t", p=128) for a in mTs]
    NSUB = TB // 128

    xs = m.sb("xs", [128, DC, TB]); x1 = m.sb("x1", [128, DC, TB]); x1b = m.sb("x1b", [128, DC, TB], BF16)
    acc = m.sb("acc", [128, DC, TB])
    big = m.sb("big", [128, 48, TB], BF16)
    wa = [m.sb("wa%d" % i, [128, DC, 128], BF16) for i in range(2)]
    wb = [m.sb("wb%d" % i, [128, DC, 128], BF16) for i in range(2)]
    HF = FC // 4
    wc = [m.sb("wc%d" % i, [128, HF, 128], BF16) for i in range(2)]
    lnp_s = m.sb("lnp_s", [128, 4, DC]); ones = m.sb("ones", [128, 128]); ident = m.sb("ident_s", [128, 128])
    st_mean = m.sb("st_mean", [128, TB]); st_rstd = m.sb("st_rstd", [128, TB]); st_tmp = m.sb("st_tmp", [128, TB])
    sq0 = m.sb("sq0", [128, TB]); sq = [sq0, sq0]
    sil0 = m.sb("sil0", [128, TB]); sil = [sil0, sil0]
    if moe:
        router = m.sb("router_s", [128, DC, NEXP]); sel = m.sb("sel_s", [NEXP, NEXP, 128])
        gB = m.sb("gB", [128, NEXP, TB], BF16); gT = m.sb("gT", [NEXP, TB])
        L = m.sb("L", [128, NEXP]); L2 = m.sb("L2", [128, NEXP]); eq1 = m.sb("eq1", [128, NEXP]); eq2 = m.sb("eq2", [128, NEXP])
        gg = m.sb("gg", [128, NEXP]); sm = m.sb("sm", [128, 8])
    pz = [m.ps("pz%d" % i, [128, TB]) for i in range(2)]
    pst = [m.ps("pst%d" % i, [128, TB]) for i in range(2)]
    ph1 = [m.ps("ph1_%d" % i, [128, TB]) for i in range(2)]
    ph3 = [m.ps("ph3_%d" % i, [128, TB]) for i in range(2)]

    m.dma("sp", lnp_s[:], lnp[:, :, :], writes=["lnp"], semkey="c0")
    m.dma("sp", ident[:], ident_d[:, :], writes=["ident"], semkey="c1")
    m.op("dve", lambda e: e.memset(ones[:], 1.0), writes=["ones"])
    if moe:
        m.dma("sp", router[:], router_d[:, :, :], writes=["router"], semkey="c2")
        m.dma("sp", sel[:], sel_d[:, :, :], writes=["sel"], semkey="c3")

    def layernorm(src, sk, which, dst32, dk, dstb, dbk):
        for c in range(DC):
            m.op("pe", lambda e, c=c: e.matmul(pst[0][:], ones[:], src[:, c, :], start=(c == 0), stop=(c == DC - 1)),
                 reads=["ones", (sk, c)], writes=["pst0"])
        for c in range(DC):
            s = sq[c % 2]
            m.op("act", lambda e, c=c, s=s: e.activation(out=s[:], in_=src[:, c, :], func=AF.Square),
                 reads=[(sk, c)], writes=["sq0"])
            m.op("pe", lambda e, c=c, s=s: e.matmul(pst[1][:], ones[:], s[:], start=(c == 0), stop=(c == DC - 1)),
                 reads=["ones", "sq0"], writes=["pst1"])
        m.op("dve", lambda e: e.tensor_scalar(st_mean[:], pst[0][:], 1.0 / D, None, ALU.mult), reads=["pst0"], writes=["st_mean"])
        m.op("dve", lambda e: e.tensor_tensor(st_tmp[:], st_mean[:], st_mean[:], ALU.mult), reads=["st_mean"], writes=["st_tmp"])
        m.op("dve", lambda e: e.scalar_tensor_tensor(st_rstd[:], pst[1][:], 1.0 / D, st_tmp[:], ALU.mult, ALU.subtract),
             reads=["pst1", "st_tmp"], writes=["st_rstd"])
        m.op("dve", lambda e: e.tensor_scalar(st_rstd[:], st_rstd[:], LN_EPS, None, ALU.add), reads=["st_rstd"], writes=["st_rstd"])
        m.op("act", lambda e: e.activation(out=st_tmp[:], in_=st_rstd[:], func=AF.Sqrt), reads=["st_rstd"], writes=["st_tmp"])
        m.op("dve", lambda e: e.reciprocal(st_rstd[:], st_tmp[:]), reads=["st_tmp"], writes=["st_rstd"])
        for c in range(DC):
            eng = "dve" if c % 2 == 0 else "pool"
            m.op(eng, lambda e, c=c: e.tensor_tensor(src[:, c, :], src[:, c, :], st_mean[:], ALU.subtract),
                 reads=[(sk, c), "st_mean"], writes=[(sk, c)])
            m.op(eng, lambda e, c=c: e.tensor_tensor(src[:, c, :], src[:, c, :], st_rstd[:], ALU.mult),
                 reads=[(sk, c), "st_rstd"], writes=[(sk, c)])
            m.op("act", lambda e, c=c: e.activation(out=dst32[:, c, :], in_=src[:, c, :], func=AF.Identity,
                                                    scale=lnp_s[:, 2 * which, c:c + 1], bias=lnp_s[:, 2 * which + 1, c:c + 1]),
                 reads=[(sk, c), "lnp"], writes=[(dk, c)])
            if dstb is not None:
                m.op("act", lambda e, c=c: e.activation(out=dstb[:, c, :], in_=src[:, c, :], func=AF.Identity,
                                                        scale=lnp_s[:, 2 * which, c:c + 1], bias=lnp_s[:, 2 * which + 1, c:c + 1]),
                     reads=[(sk, c), "lnp"], writes=[(dbk, c)])

    wcnt = {"a": 0, "b": 0, "c": 0}
    def wload(kind, tiles, src):
        i = wcnt[kind] % 2; wcnt[kind] += 1
        key = "w%s%d" % (kind, i)
        m.dma("pool", tiles[i][:], src, writes=[key], semkey=key)
        return tiles[i], key

    for mt in range(NT // TB):
        ts = slice(mt * TB, (mt + 1) * TB)
        m.dma("sp", xs[:], xT_v[:, :, ts], writes=[("xs", c) for c in range(DC)], semkey="xs")
        for b in range(3):
            m.dma("pool", big[:, b * DC:(b + 1) * DC, :], mT_v[b][:, :, ts],
                  writes=[("big", b * DC + i) for i in range(DC)], semkey="bigm%d" % b)
        for c in range(DC):
            wt, wk = wload("a", wa, wout[c])
            p = pz[c % 2]; pk = "pz%d" % (c % 2)
            for n in range(48):
                m.op("pe", lambda e, p=p, wt=wt, n=n: e.matmul(p[:], wt[:, n % DC, :], big[:, n, :], start=(n == 0), stop=(n == 47)),
                     reads=[wk, ("big", n)], writes=[pk])
            m.op("dve", lambda e, p=p, c=c: e.scalar_tensor_tensor(xs[:, c, :], xs[:, c, :], ALPHA, p[:], ALU.mult, ALU.add),
                 reads=[pk, ("xs", c)], writes=[("xs", c)])
        layernorm(xs, "xs", 0, x1, "x1", x1b, "x1b")
        if moe:
            for sub in range(NSUB):
                tsub = slice(sub * 128, (sub + 1) * 128)
                for c in range(DC):
                    m.op("pe", lambda e, c=c, tsub=tsub: e.matmul(pst[0][:, 0:NEXP], x1[:, c, tsub], router[:, c, :], start=(c == 0), stop=(c == DC - 1)),
                         reads=[("x1", c), "router"], writes=["pst0"])
                m.op("dve", lambda e: e.tensor_copy(L[:], pst[0][:, 0:NEXP]), reads=["pst0"], writes=["L"])
                m.op("dve", lambda e: e.tensor_reduce(sm[:, 0:1], L[:], AX.X, ALU.max), reads=["L"], writes=["sm"])
                m.op("dve", lambda e: e.tensor_scalar(eq1[:], L[:], sm[:, 0:1], None, ALU.is_equal), reads=["L", "sm"], writes=["eq1"])
                m.op("dve", lambda e: e.scalar_tensor_tensor(L2[:], eq1[:], -1e30, L[:], ALU.mult, ALU.add), reads=["eq1", "L"], writes=["L2"])
                m.op("dve", lambda e: e.tensor_reduce(sm[:, 1:2], L2[:], AX.X, ALU.max), reads=["L2", "sm"], writes=["sm"])
                m.op("dve", lambda e: e.tensor_scalar(eq2[:], L2[:], sm[:, 1:2], None, ALU.is_equal), reads=["L2", "sm"], writes=["eq2"])
                m.op("dve", lambda e: e.tensor_tensor(sm[:, 2:3], sm[:, 1:2], sm[:, 0:1], ALU.subtract), reads=["sm"], writes=["sm"])
                m.op("act", lambda e: e.activation(out=sm[:, 3:4], in_=sm[:, 2:3], func=AF.Exp), reads=["sm"], writes=["sm"])
                m.op("dve", lambda e: e.tensor_scalar(sm[:, 4:5], sm[:, 3:4], 1.0, None, ALU.add), reads=["sm"], writes=["sm"])
                m.op("dve", lambda e: e.reciprocal(sm[:, 5:6], sm[:, 4:5]), reads=["sm"], writes=["sm"])
                m.op("dve", lambda e: e.tensor_tensor(sm[:, 6:7], sm[:, 3:4], sm[:, 5:6], ALU.mult), reads=["sm"], writes=["sm"])
                m.op("dve", lambda e: e.tensor_scalar(gg[:], eq1[:], sm[:, 5:6], None, ALU.mult), reads=["eq1", "sm"], writes=["gg"])
                m.op("dve", lambda e: e.scalar_tensor_tensor(gg[:], eq2[:], sm[:, 6:7], gg[:], ALU.mult, ALU.add), reads=["eq2", "sm", "gg"], writes=["gg"])
                m.op("pe", lambda e: e.transpose(pst[1][0:NEXP, 0:128], gg[:], ident[:]), reads=["gg", "ident"], writes=["pst1"])
                m.op("dve", lambda e, tsub=tsub: e.tensor_copy(gT[:, tsub], pst[1][0:NEXP, 0:128]), reads=["pst1"], writes=["gT"])
            for ex in range(NEXP):
                p = ph1[ex % 2]; pk = "ph1_%d" % (ex % 2)
                m.op("pe", lambda e, p=p, ex=ex: e.matmul(p[:], sel[:, ex, :], gT[:], start=True, stop=True), reads=["sel", "gT"], writes=[pk])
                m.op("act", lambda e, p=p, ex=ex: e.copy(gB[:, ex, :], p[:]), reads=[pk], writes=[("gB", ex)])
        for ex in range(NE):
            for f in range(FC):
                w1t, w1k = wload("a", wa, w1[ex, f]); w3t, w3k = wload("b", wb, w3[ex, f])
                p1 = ph1[f % 2]; p1k = "ph1_%d" % (f % 2); p3 = ph3[f % 2]; p3k = "ph3_%d" % (f % 2)
                for c in range(DC):
                    m.op("pe", lambda e, c=c, p1=p1, w1t=w1t: e.matmul(p1[:], w1t[:, c, :], x1b[:, c, :], start=(c == 0), stop=(c == DC - 1)),
                         reads=[w1k, ("x1b", c)], writes=[p1k])
                for c in range(DC):
                    m.op("pe", lambda e, c=c, p3=p3, w3t=w3t: e.matmul(p3[:], w3t[:, c, :], x1b[:, c, :], start=(c == 0), stop=(c == DC - 1)),
                         reads=[w3k, ("x1b", c)], writes=[p3k])
                sl = sil[f % 2]; slk = "sil0"
                m.op("act", lambda e, sl=sl, p1=p1: e.activation(out=sl[:], in_=p1[:], func=AF.Silu), reads=[p1k], writes=[slk])
                if moe:
                    m.op("pool", lambda e, sl=sl, ex=ex: e.tensor_tensor(sl[:], sl[:], gB[:, ex, :], ALU.mult), reads=[slk, ("gB", ex)], writes=[slk])
                m.op("dve", lambda e, sl=sl, p3=p3, f=f: e.tensor_tensor(big[:, f, :], sl[:], p3[:], ALU.mult), reads=[slk, p3k], writes=[("big", f)])
            for c in range(DC):
                p = pz[c % 2]; pk = "pz%d" % (c % 2)
                for half in range(4):
                    wt, wk = wload("c", wc, w2[ex, c][:, half * HF:(half + 1) * HF, :])
                    for j in range(HF):
                        f = half * HF + j
                        m.op("pe", lambda e, p=p, wt=wt, j=j, f=f: e.matmul(p[:], wt[:, j, :], big[:, f, :], start=(f == 0), stop=(f == FC - 1)),
                             reads=[wk, ("big", f)], writes=[pk])
                if ex == 0:
                    m.op("dve", lambda e, p=p, c=c: e.scalar_tensor_tensor(acc[:, c, :], x1[:, c, :], ALPHA, p[:], ALU.mult, ALU.add),
                         reads=[pk, ("x1", c)], writes=[("acc", c)])
                else:
                    m.op("dve", lambda e, p=p, c=c: e.tensor_tensor(acc[:, c, :], acc[:, c, :], p[:], ALU.add),
                         reads=[pk, ("acc", c)], writes=[("acc", c)])
        layernorm(acc, "acc", 1, xs, "xs", None, None)
        m.dma("sp", yT_v[:, :, ts], xs[:], reads=[("xs", c) for c in range(DC)], semkey="xs")
    m.finish("sp")
    return nc, m


def ffn_inputs(layer, inp, moe):
    d = {}
    d["wout"] = lay_w(inp["w_out"][layer])
    if not moe:
        d["w1"] = lay_w(inp["ffn_w1"][layer // 2])[None]; d["w3"] = lay_w(inp["ffn_w3"][layer // 2])[None]
        d["w2"] = lay_w(inp["ffn_w2"][layer // 2])[None]
    else:
        d["w1"] = np.stack([lay_w(inp["moe_w1"][layer // 2][e]) for e in range(NEXP)])
        d["w3"] = np.stack([lay_w(inp["moe_w3"][layer // 2][e]) for e in range(NEXP)])
        d["w2"] = np.stack([lay_w(inp["moe_w2"][layer // 2][e]) for e in range(NEXP)])
        d["router"] = np.ascontiguousarray(inp["moe_router"][layer // 2].reshape(DC, 128, NEXP).transpose(1, 0, 2))
        sel = np.zeros((NEXP, NEXP, 128), np.float32)
        for e in range(NEXP): sel[e, e, :] = 1.0
        d["sel"] = sel
    d["lnp"] = np.ascontiguousarray(np.stack([lay_vec(inp["ln1_w"][layer]), lay_vec(inp["ln1_b"][layer]),
                                              lay_vec(inp["ln2_w"][layer]), lay_vec(inp["ln2_b"][layer])], axis=1))
    d["ident"] = np.eye(128, dtype=np.float32)
    return d


HM = 4
SC_ATT = 192 ** -0.5
TWO_PI = 2 * np.pi
C1 = 6.28125
C2 = float(TWO_PI - C1)
RMS_EPS = 1e-6


def build_mla(S, TB=512):
    nc = bass.Bass("TRN2", target_bir_lowering=False)
    m = MK(nc)
    dr = lambda name, shape, kind="ExternalInput", dt=F32: nc.dram_tensor(name, list(shape), dt, kind=kind).ap()
    xT = dr("xT", [D, S]); wl_d = dr("wl", [10, 128, DC, 128]); wg_d = dr("wg", [128, DC, 512])
    wq_d = dr("wq", [HM, 128, 4, 256]); wk_d = dr("wk", [HM, 128, 4, 128]); wv_d = dr("wv", [128, 4, 512])
    nw_d = dr("nw", [128, 2, 4]); pos_d = dr("pos", [1, S], dt=I32); rc_d = dr("rc", [64, 2]); cm_d = dr("cmask", [128, 4, 512])
    om = dr("om", [S, 512], kind="ExternalOutput")
    qT = dr("qT_s", [HM, 192, S], kind="Internal", dt=BF16); kT = dr("kT_s", [HM, 128, S], kind="Internal", dt=BF16)
    krT = dr("krT_s", [64, S], kind="Internal", dt=BF16); Vd = dr("V_s", [HM, S, 128], kind="Internal", dt=BF16)
    gate_d = dr("gate_s", [S, 512], kind="Internal")
    xT_v = xT.rearrange("(c p) t -> p c t", p=128)
    NSUB = TB // 128; NMT = S // TB

    pb = [m.ps("pb%d" % i, [128, 512]) for i in range(8)]
    pcnt = [0]
    def nextp():
        i = pcnt[0] % 8; pcnt[0] += 1
        return pb[i], "pb%d" % i
    ones = m.sb("ones", [128, 128]); rc = m.sb("rc_s", [64, 2]); nw = m.sb("nw_s", [128, 2, 4])
    m.op("dve", lambda e: e.memset(ones[:], 1.0), writes=["ones"])
    m.dma("sp", rc[:], rc_d[:, :], writes=["rc"], semkey="c0")
    m.dma("sp", nw[:], nw_d[:, :, :], writes=["nw"], semkey="c1")

    mk1 = m.mark()
    wl = m.sb("wl_s", [128, 10, DC, 128], BF16); wg = m.sb("wg_s", [128, DC, 512], BF16)
    wq = m.sb("wq_s", [128, HM, 4, 256], BF16); wk = m.sb("wk_s", [128, HM, 4, 128], BF16); wv = m.sb("wv_s", [128, 4, 512], BF16)
    for j in range(10):
        m.dma("pool", wl[:, j, :, :], wl_d[j], writes=["wl"], semkey="wl")
    m.dma("pool", wg[:], wg_d[:, :, :], writes=["wg"], semkey="wg")
    for h in range(HM):
        m.dma("pool", wq[:, h, :, :], wq_d[h], writes=["wq"], semkey="wq")
        m.dma("pool", wk[:, h, :, :], wk_d[h], writes=["wk"], semkey="wk")
    m.dma("pool", wv[:], wv_d[:, :, :], writes=["wv"], semkey="wv")
    xb = [m.sb("xb%d" % i, [128, DC, TB], BF16) for i in range(2)]
    posi = m.sb("posi", [64, TB], I32); ang = m.sb("ang", [64, TB]); ki = m.sb("ki", [64, TB], I32); kf = m.sb("kf", [64, TB])
    rr = m.sb("rr", [64, TB]); cos2 = m.sb("cos2", [64, TB]); sinS = m.sb("sinS", [64, TB]); cos2s = m.sb("cos2s", [64, TB]); sinSs = m.sb("sinSs", [64, TB])
    lat = m.sb("lat", [128, 8, TB]); sqt = m.sb("sqt", [128, TB]); rstd = m.sb("rstd", [128, 2, TB]); stt_ = m.sb("stt_", [128, TB])
    latn = m.sb("latn", [128, 8, TB], BF16)
    ev = [m.sb("ev%d" % i, [128, TB], BF16) for i in range(2)]
    evr = [m.sb("evr%d" % i, [64, TB], BF16) for i in range(2)]
    t1 = m.sb("t1", [64, TB]); t2 = m.sb("t2", [64, TB])
    gst = [m.sb("gst%d" % i, [128, 512]) for i in range(2)]
    ecnt = {"ev": 0, "evr": 0, "gst": 0}
    def nxt(kind, tiles):
        i = ecnt[kind] % 2; ecnt[kind] += 1
        return tiles[i], "%s%d" % (kind, i)

    def load_x(mt):
        i = mt % 2
        m.dma("pool", xb[i][:], xT_v[:, :, mt * TB:(mt + 1) * TB], writes=["xb%d" % i], semkey="xb%d" % i)
    load_x(0)
    for mt in range(NMT):
        ts = slice(mt * TB, (mt + 1) * TB)
        if mt + 1 < NMT: load_x(mt + 1)
        x = xb[mt % 2]; xk = "xb%d" % (mt % 2)
        m.dma("sp", posi[:], pos_d[0:1, ts].partition_broadcast(64), writes=["posi"], semkey="posi")
        m.op("dve", lambda e: e.tensor_copy(ang[:], posi[:]), reads=["posi"], writes=["ang"])
        m.op("dve", lambda e: e.tensor_scalar(ang[:], ang[:], rc[:, 0:1], None, ALU.mult), reads=["ang", "rc"], writes=["ang"])
        for which in range(2):
            dst = sinS if which == 0 else cos2; dk = "sinS" if which == 0 else "cos2"
            m.op("dve", lambda e, which=which: e.tensor_scalar(ki[:], ang[:], 1.0 / TWO_PI, 0.25 * which, ALU.mult, ALU.add), reads=["ang"], writes=["ki"])
            m.op("dve", lambda e: e.tensor_copy(kf[:], ki[:]), reads=["ki"], writes=["kf"])
            m.op("dve", lambda e: e.scalar_tensor_tensor(rr[:], kf[:], -C1, ang[:], ALU.mult, ALU.add), reads=["kf", "ang"], writes=["rr"])
            if which == 1:
                m.op("dve", lambda e: e.tensor_scalar(rr[:], rr[:], float(np.pi / 2), None, ALU.add), reads=["rr"], writes=["rr"])
            m.op("dve", lambda e: e.scalar_tensor_tensor(rr[:], kf[:], -C2, rr[:], ALU.mult, ALU.add), reads=["kf", "rr"], writes=["rr"])
            m.op("dve", lambda e: e.tensor_scalar(rr[:], rr[:], float(np.pi), float(-np.pi), ALU.min, ALU.max), reads=["rr"], writes=["rr"])
            if which == 0:
                m.op("act", lambda e, dst=dst: e.activation(out=dst[:], in_=rr[:], func=AF.Sin, scale=rc[:, 1:2]), reads=["rr", "rc"], writes=[dk])
            else:
                m.op("act", lambda e, dst=dst: e.activation(out=dst[:], in_=rr[:], func=AF.Sin), reads=["rr"], writes=[dk])
        m.op("pool", lambda e: e.tensor_scalar(cos2s[:], cos2[:], SC_ATT, None, ALU.mult), reads=["cos2"], writes=["cos2s"])
        m.op("pool", lambda e: e.tensor_scalar(sinSs[:], sinS[:], SC_ATT, None, ALU.mult), reads=["sinS"], writes=["sinSs"])
        for grp in range(2):
            pss, pssk = nextp()
            for j in range(4):
                jj = grp * 4 + j
                p, pk = nextp()
                for c in range(DC):
                    m.op("pe", lambda e, p=p, jj=jj, c=c: e.matmul(p[:], wl[:, jj, c, :], x[:, c, :], start=(c == 0), stop=(c == DC - 1)),
                         reads=["wl", xk], writes=[pk])
                m.op("act", lambda e, p=p, jj=jj: e.copy(lat[:, jj, :], p[:]), reads=[pk], writes=[("lat", jj)])
                m.op("act", lambda e, jj=jj: e.activation(out=sqt[:], in_=lat[:, jj, :], func=AF.Square), reads=[("lat", jj)], writes=["sqt"])
                m.op("pe", lambda e, pss=pss, j=j: e.matmul(pss[:], ones[:], sqt[:], start=(j == 0), stop=(j == 3)), reads=["ones", "sqt"], writes=[pssk])
            m.op("dve", lambda e, pss=pss, grp=grp: e.tensor_scalar(rstd[:, grp, :], pss[:], 1.0 / 512, RMS_EPS, ALU.mult, ALU.add), reads=[pssk], writes=[("rstd", grp)])
            m.op("act", lambda e, grp=grp: e.activation(out=stt_[:], in_=rstd[:, grp, :], func=AF.Sqrt), reads=[("rstd", grp)], writes=["stt_"])
            m.op("dve", lambda e, grp=grp: e.reciprocal(rstd[:, grp, :], stt_[:]), reads=["stt_"], writes=[("rstd", grp)])
            for j in range(4):
                jj = grp * 4 + j
                m.op("dve", lambda e, jj=jj, grp=grp, j=j: e.scalar_tensor_tensor(latn[:, jj, :], lat[:, jj, :], nw[:, grp, j:j + 1], rstd[:, grp, :], ALU.mult, ALU.mult),
                     reads=[("lat", jj), "nw", ("rstd", grp)], writes=[("latn", jj)])
        def rope(wsel_pe, wsel_rot, rhs_fn, nk, rkeys, ctab, stab, ck, sk, dst_ap):
            pA, pAk = nextp(); pB, pBk = nextp()
            for c in range(nk):
                m.op("pe", lambda e, c=c: e.matmul(pA[0:64, :], wsel_pe(c), rhs_fn(c), start=(c == 0), stop=(c == nk - 1)), reads=rkeys, writes=[pAk])
            for c in range(nk):
                m.op("pe", lambda e, c=c: e.matmul(pB[0:64, :], wsel_rot(c), rhs_fn(c), start=(c == 0), stop=(c == nk - 1)), reads=rkeys, writes=[pBk])
            m.op("dve", lambda e: e.tensor_tensor(t1[:], pA[0:64, :], ctab[:], ALU.mult), reads=[pAk, ck], writes=["t1"])
            m.op("dve", lambda e: e.tensor_tensor(t2[:], pB[0:64, :], stab[:], ALU.mult), reads=[pBk, sk], writes=["t2"])
            o, ok = nxt("evr", evr)
            m.op("dve", lambda e: e.tensor_tensor(o[:], t1[:], t2[:], ALU.add), reads=["t1", "t2"], writes=[ok])
            m.dma("sp", dst_ap, o[:], reads=[ok], semkey=ok)
        rope(lambda c: wl[:, 8, c, 0:64], lambda c: wl[:, 9, c, 0:64], lambda c: x[:, c, :], DC, ["wl", xk], cos2, sinS, "cos2", "sinS", krT[:, ts])
        for h in range(HM):
            p, pk = nextp()
            for c in range(4):
                m.op("pe", lambda e, p=p, h=h, c=c: e.matmul(p[:], wq[:, h, c, 0:128], latn[:, c, :], start=(c == 0), stop=(c == 3)),
                     reads=["wq", ("latn", c)], writes=[pk])
            o, ok = nxt("ev", ev)
            m.op("act", lambda e, o=o, p=p: e.activation(out=o[:], in_=p[:], func=AF.Copy, scale=SC_ATT), reads=[pk], writes=[ok])
            m.dma("sp", qT[h, 0:128, ts], o[:], reads=[ok], semkey=ok)
            rope(lambda c, h=h: wq[:, h, c, 128:192], lambda c, h=h: wq[:, h, c, 192:256], lambda c: latn[:, c, :], 4,
                 ["wq"] + [("latn", c) for c in range(4)], cos2s, sinSs, "cos2s", "sinSs", qT[h, 128:192, ts])
            p, pk = nextp()
            for c in range(4):
                m.op("pe", lambda e, p=p, h=h, c=c: e.matmul(p[:], wk[:, h, c, :], latn[:, 4 + c, :], start=(c == 0), stop=(c == 3)),
                     reads=["wk", ("latn", 4 + c)], writes=[pk])
            o, ok = nxt("ev", ev)
            m.op("act", lambda e, o=o, p=p: e.copy(o[:], p[:]), reads=[pk], writes=[ok])
            m.dma("sp", kT[h, :, ts], o[:], reads=[ok], semkey=ok)
        for sub in range(NSUB):
            tsub = slice(sub * 128, (sub + 1) * 128); tg = slice(mt * TB + sub * 128, mt * TB + (sub + 1) * 128)
            p, pk = nextp()
            for c in range(4):
                m.op("pe", lambda e, p=p, c=c, tsub=tsub: e.matmul(p[:], latn[:, 4 + c, tsub], wv[:, c, :], start=(c == 0), stop=(c == 3)),
                     reads=["wv", ("latn", 4 + c)], writes=[pk])
            o, ok = nxt("ev", ev)
            m.op("act", lambda e, o=o, p=p: e.copy(o[:], p[:]), reads=[pk], writes=[ok])
            m.dma("sp", Vd[:, tg, :].rearrange("h t v -> t h v"), o[:].rearrange("p (h v) -> p h v", h=HM), reads=[ok], semkey=ok)
            p, pk = nextp()
            for c in range(DC):
                m.op("pe", lambda e, p=p, c=c, tsub=tsub: e.matmul(p[:], x[:, c, tsub], wg[:, c, :], start=(c == 0), stop=(c == DC - 1)),
                     reads=["wg", xk], writes=[pk])
            o, ok = nxt("gst", gst)
            m.op("act", lambda e, o=o, p=p: e.activation(out=o[:], in_=p[:], func=AF.Sigmoid), reads=[pk], writes=[ok])
            m.dma("sp", gate_d[tg, :], o[:], reads=[ok], semkey=ok)
    m.release(mk1)
    NKC = S // 128
    Kn = m.sb("Kn", [128, S], BF16); Kr = m.sb("Kr", [64, S], BF16); Va = m.sb("Va", [128, NKC, 129], BF16)
    cmask = m.sb("cmask_s", [128, 4, 512], BF16)
    qn = [m.sb("qn%d" % i, [128, TB], BF16) for i in range(2)]; qr = [m.sb("qr%d" % i, [64, TB], BF16) for i in range(2)]
    P = [m.sb("P%d" % i, [128, TB], BF16) for i in range(3)]
    gt = [m.sb("gt%d" % i, [128, 4, 128]) for i in range(2)]; ost = [m.sb("ost%d" % i, [128, 4, 128]) for i in range(2)]
    rden = m.sb("rden", [128, 4])
    m.dma("pool", cmask[:], cm_d[:, :, :], writes=["cmask"], semkey="cmask")
    m.op("dve", lambda e: e.memset(Va[:, :, 128:129], 1.0), writes=["Va_ones"])
    m.dma("sp", Kr[:], krT[:, :], writes=["Kr"], semkey="Kr")
    it = 0; pc = 0
    for h in range(HM):
        m.dma("sp", Kn[:], kT[h], writes=["Kn"], semkey="Kn")
        for k0 in range(0, NKC, 16):
            k1 = min(NKC, k0 + 16)
            m.dma("sp", Va[:, k0:k1, 0:128], Vd[h, k0 * 128:k1 * 128, :].rearrange("(k p) v -> p k v", p=128), writes=["Va"], semkey="Va")
        for qm in range(NMT):
            ts = slice(qm * TB, (qm + 1) * TB)
            qi = it % 2; it += 1
            m.dma("sp", qn[qi][:], qT[h, 0:128, ts], writes=["qn%d" % qi], semkey="qn%d" % qi)
            m.dma("sp", qr[qi][:], qT[h, 128:192, ts], writes=["qr%d" % qi], semkey="qr%d" % qi)
            m.dma("sp", gt[qi][:], gate_d[ts, h * 128:(h + 1) * 128].rearrange("(q p) v -> p q v", p=128), writes=["gt%d" % qi], semkey="gt%d" % qi)
            nkc = 4 * qm + 4
            for kc in range(nkc):
                p = pb[kc % 2]; pk = "pb%d" % (kc % 2)
                ks = slice(kc * 128, (kc + 1) * 128)
                m.op("pe", lambda e, p=p, ks=ks, qi=qi: e.matmul(p[:], Kn[:, ks], qn[qi][:], start=True, stop=False), reads=["Kn", "qn%d" % qi], writes=[pk])
                m.op("pe", lambda e, p=p, ks=ks, qi=qi: e.matmul(p[:], Kr[:, ks], qr[qi][:], start=False, stop=True), reads=["Kr", "qr%d" % qi], writes=[pk])
                Pt = P[pc % 3]; Pk = "P%d" % (pc % 3); pc += 1
                m.op("act", lambda e, Pt=Pt, p=p: e.activation(out=Pt[:], in_=p[:], func=AF.Exp), reads=[pk], writes=[Pk])
                r = kc - 4 * qm
                if r >= 0:
                    m.op("pool", lambda e, Pt=Pt, r=r: e.tensor_tensor(Pt[:], Pt[:], cmask[:, r, :], ALU.mult), reads=[Pk, "cmask"], writes=[Pk])
                for qs in range(4):
                    if kc > 4 * qm + qs: continue
                    a = pb[2 + qs]; ak = "pb%d" % (2 + qs)
                    m.op("pe", lambda e, a=a, qs=qs, Pt=Pt, kc=kc, qm=qm: e.matmul(a[:, 0:129], Pt[:, qs * 128:(qs + 1) * 128], Va[:, kc, :],
                                                                             start=(kc == 0), stop=(kc == 4 * qm + qs)),
                         reads=[Pk, "Va", "Va_ones"], writes=[ak])
            o = ost[qi]; okk = "ost%d" % qi
            for qs in range(4):
                a = pb[2 + qs]; ak = "pb%d" % (2 + qs)
                m.op("dve", lambda e, a=a, qs=qs: e.reciprocal(rden[:, qs:qs + 1], a[:, 128:129]), reads=[ak], writes=[("rden", qs)])
                m.op("dve", lambda e, a=a, qs=qs, o=o, qi=qi: e.scalar_tensor_tensor(o[:, qs, :], a[:, 0:128], rden[:, qs:qs + 1], gt[qi][:, qs, :], ALU.mult, ALU.mult),
                     reads=[ak, ("rden", qs), "gt%d" % qi], writes=[(okk, qs)])
            m.dma("sp", om[ts, h * 128:(h + 1) * 128].rearrange("(q p) v -> p q v", p=128), o[:], reads=[(okk, qs) for qs in range(4)], semkey=okk)
    m.finish("sp")
    return nc, m


def mla_inputs(layer, inp, b, g):
    GC = 3 * 2048; SSDC = 2048 + 3072 + 32
    o_mla = GC + SSDC
    W = inp["w_in"][layer]
    kpe = W[:, o_mla + 1024:o_mla + 1088]
    krot = np.concatenate([kpe[:, 32:64], kpe[:, 0:32]], axis=1)
    z64 = np.zeros((2048, 64), np.float32)
    wl = np.concatenate([W[:, o_mla:o_mla + 1024], kpe, z64, krot, z64], axis=1)
    d = {"wl": lay_w(wl)}
    gcol = 2 * 2048 + g * 512
    d["wg"] = np.ascontiguousarray(W[:, gcol:gcol + 512].reshape(DC, 128, 512).transpose(1, 0, 2))
    wqb = inp["mla_w_q_b"][layer].reshape(512, 16, 192)[:, g * 4:(g + 1) * 4, :]
    wq = np.concatenate([wqb[:, :, 0:192], wqb[:, :, 160:192], wqb[:, :, 128:160]], axis=2)
    d["wq"] = np.ascontiguousarray(wq.reshape(4, 128, 4, 256).transpose(2, 1, 0, 3))
    wkv = inp["mla_w_kv_b"][layer].reshape(512, 16, 256)[:, g * 4:(g + 1) * 4, :]
    d["wk"] = np.ascontiguousarray(wkv[:, :, 0:128].reshape(4, 128, 4, 128).transpose(2, 1, 0, 3))
    d["wv"] = np.ascontiguousarray(wkv[:, :, 128:256].reshape(4, 128, 512).transpose(1, 0, 2))
    d["nw"] = np.ascontiguousarray(np.stack([lay_vec(inp["mla_q_norm_w"][layer]), lay_vec(inp["mla_kv_norm_w"][layer])], axis=1))
    d["pos"] = np.ascontiguousarray(inp["positions"][b][None, :]).astype(np.int32)
    inv = (10000.0 ** (-np.arange(0, 64, 2, dtype=np.float32) / 64)).astype(np.float32)
    rc = np.zeros((64, 2), np.float32); rc[:, 0] = np.concatenate([inv, inv]); rc[:32, 1] = -1.0; rc[32:, 1] = 1.0
    d["rc"] = rc
    pp = np.arange(128)[:, None]; jj = np.arange(512)[None, :]
    d["cmask"] = np.ascontiguousarray(np.stack([((r * 128 + pp) <= jj).astype(np.float32) for r in range(4)], axis=1))
    return d


def build_ssd(S, TB=512):
    nc = bass.Bass("TRN2", target_bir_lowering=False)
    m = MK(nc)
    dr = lambda name, shape, kind="ExternalInput", dt=F32: nc.dram_tensor(name, list(shape), dt, kind=kind).ap()
    xT = dr("xT", [D, S]); wfm_d = dr("wfm", [6, 128, DC, 128]); wtm_d = dr("wtm", [128, DC, 1032])
    cw_d = dr("cw", [128, 6, 4]); cb_d = dr("cb", [128, 6]); rows_d = dr("rows", [1, 24 + 1024])
    tri_d = dr("triU", [128, 128]); nm_d = dr("negmask", [128, 128]); id_d = dr("ident", [128, 128])
    om = dr("om", [S, 512], kind="ExternalOutput")
    xT_v = xT.rearrange("(c p) t -> p c t", p=128)
    NSUB = TB // 128; NMT = S // TB
    pb = [m.ps("pb%d" % i, [128, 512]) for i in range(8)]
    pcnt = [0]
    def nextp():
        i = pcnt[0] % 8; pcnt[0] += 1
        return pb[i], "pb%d" % i
    ones = m.sb("ones", [128, 128]); ident = m.sb("ident_s", [128, 128]); triU = m.sb("triU_s", [128, 128]); negm = m.sb("negm_s", [128, 128])
    cw = m.sb("cw_s", [128, 6, 4]); cb = m.sb("cb_s", [128, 6]); rows = m.sb("rows_s", [128, 24 + 1024])
    wfm = m.sb("wfm_s", [128, 6, DC, 128], BF16); wtm = m.sb("wtm_s", [128, DC, 1032], BF16)
    m.op("dve", lambda e: e.memset(ones[:], 1.0), writes=["ones"])
    m.dma("sp", ident[:], id_d[:, :], writes=["ident"], semkey="c0"); m.dma("sp", triU[:], tri_d[:, :], writes=["triU"], semkey="c1")
    m.dma("sp", negm[:], nm_d[:, :], writes=["negm"], semkey="c2"); m.dma("sp", cw[:], cw_d[:, :, :], writes=["cw"], semkey="c3")
    m.dma("sp", cb[:], cb_d[:, :], writes=["cb"], semkey="c4")
    m.dma("sp", rows[:], rows_d[0:1, :].partition_broadcast(128), writes=["rows"], semkey="c5")
    for j in range(6):
        m.dma("pool", wfm[:, j, :, :], wfm_d[j], writes=["wfm"], semkey="wfm")
    m.dma("pool", wtm[:], wtm_d[:, :, :], writes=["wtm"], semkey="wtm")
    a_b = m.sb("a_b", [128, 8])
    m.op("act", lambda e: e.activation(out=a_b[:], in_=rows[:, 8:16], func=AF.Exp), reads=["rows"], writes=["a_b"])
    m.op("dve", lambda e: e.tensor_scalar(a_b[:], a_b[:], -1.0, None, ALU.mult), reads=["a_b"], writes=["a_b"])
    dtb = rows[:, 0:8]; dsk = rows[:, 24:24 + 512]; nwb = rows[:, 24 + 512:24 + 1024]

    xb = [m.sb("xb%d" % i, [128, DC, TB], BF16) for i in range(2)]
    xh = m.sb("xh", [128, 6, TB + 3]); xc = m.sb("xc", [128, 6, TB]); cacc = m.sb("cacc", [128, TB])
    hT = m.sb("hT", [128, 512])
    xs_tok = m.sb("xs_tok", [128, 512]); B_tok = m.sb("B_tok", [128, 128]); sm = m.sb("sm8", [128, 8, 8])
    tri_e = [m.sb("tri_e%d" % i, [128, 128]) for i in range(2)]
    cbT = m.sb("cbT", [128, 128]); decT = m.sb("decT", [128, 8, 128]); MT = m.sb("MT", [128, 8, 128])
    xdt = m.sb("xdt", [128, 512]); xw = m.sb("xw", [128, 512]); yi = m.sb("yi", [128, 512]); y = m.sb("y", [128, 512])
    zs = m.sb("zs", [128, 512]); gs = m.sb("gs", [128, 512]); junk = m.sb("junk", [128, 512]); ss = m.sb("ss", [128, 4])
    outt = [m.sb("outt%d" % i, [128, 512]) for i in range(2)]
    m.op("dve", lambda e: e.memset(hT[:], 0.0), writes=["hT"])
    m.op("dve", lambda e: e.memset(xh[:, :, 0:3], 0.0), writes=[("xh_halo", j) for j in range(6)])
    def bc(ap8):
        return ap8.unsqueeze(2).to_broadcast([128, 8, 64])
    v3 = lambda t: t[:].rearrange("p (e q) -> p e q", e=8)

    def load_x(mt):
        i = mt % 2
        m.dma("pool", xb[i][:], xT_v[:, :, mt * TB:(mt + 1) * TB], writes=["xb%d" % i], semkey="xb%d" % i)
    load_x(0)
    oc = 0
    for mt in range(NMT):
        if mt + 1 < NMT: load_x(mt + 1)
        x = xb[mt % 2]; xk = "xb%d" % (mt % 2)
        for j in range(6):
            p, pk = nextp()
            for c in range(DC):
                m.op("pe", lambda e, p=p, j=j, c=c: e.matmul(p[:], wfm[:, j, c, :], x[:, c, :], start=(c == 0), stop=(c == DC - 1)), reads=["wfm", xk], writes=[pk])
            m.op("act", lambda e, p=p, j=j: e.copy(xh[:, j, 3:TB + 3], p[:]), reads=[pk, ("xh_halo", j)], writes=[("xh", j)])
            m.op("dve", lambda e, j=j: e.tensor_scalar(cacc[:], xh[:, j, 0:TB], cw[:, j, 0:1], None, ALU.mult), reads=[("xh", j), ("xh_halo", j), "cw"], writes=["cacc"])
            for k in range(1, 4):
                m.op("dve", lambda e, j=j, k=k: e.scalar_tensor_tensor(cacc[:], xh[:, j, k:TB + k], cw[:, j, k:k + 1], cacc[:], ALU.mult, ALU.add),
                     reads=[("xh", j), ("xh_halo", j), "cw", "cacc"], writes=["cacc"])
            m.op("act", lambda e, j=j: e.activation(out=xc[:, j, :], in_=cacc[:], func=AF.Silu, bias=cb[:, j:j + 1]), reads=["cacc", "cb"], writes=[("xc", j)])
            m.op("pool", lambda e, j=j: e.tensor_copy(xh[:, j, 0:3], xh[:, j, TB:TB + 3]), reads=[("xh", j), "cacc"], writes=[("xh_halo", j)])
        for sub in range(NSUB):
            tsub = slice(sub * 128, (sub + 1) * 128); tg = slice(mt * TB + sub * 128, mt * TB + (sub + 1) * 128)
            p, pk = nextp()
            for j in range(4):
                m.op("pe", lambda e, p=p, j=j, tsub=tsub: e.transpose(p[:, j * 128:(j + 1) * 128], xc[:, j, tsub], ident[:]), reads=[("xc", j), "ident"], writes=[pk])
            m.op("act", lambda e, p=p: e.copy(xs_tok[:], p[:]), reads=[pk], writes=["xs_tok"])
            p, pk = nextp()
            m.op("pe", lambda e, p=p, tsub=tsub: e.transpose(p[:, 0:128], xc[:, 4, tsub], ident[:]), reads=[("xc", 4), "ident"], writes=[pk])
            m.op("act", lambda e, p=p: e.copy(B_tok[:], p[:, 0:128]), reads=[pk], writes=["B_tok"])
            p, pk = nextp()
            for c in range(DC):
                m.op("pe", lambda e, p=p, c=c, tsub=tsub: e.matmul(p[:, 0:8], x[:, c, tsub], wtm[:, c, 1024:1032], start=(c == 0), stop=(c == DC - 1)), reads=["wtm", xk], writes=[pk])
            m.op("dve", lambda e, p=p: e.tensor_tensor(sm[:, 7, :], p[:, 0:8], dtb, ALU.add), reads=[pk, "rows"], writes=[("sm", 7)])
            m.op("act", lambda e: e.activation(out=sm[:, 7, :], in_=sm[:, 7, :], func=AF.Exp), reads=[("sm", 7)], writes=[("sm", 7)])
            m.op("act", lambda e: e.activation(out=sm[:, 0, :], in_=sm[:, 7, :], func=AF.Ln, bias=1.0), reads=[("sm", 7)], writes=[("sm", 0)])
            m.op("dve", lambda e: e.tensor_tensor(sm[:, 1, :], sm[:, 0, :], a_b[:], ALU.mult), reads=[("sm", 0), "a_b"], writes=[("sm", 1)])
            p, pk = nextp()
            m.op("pe", lambda e, p=p: e.matmul(p[:, 0:8], triU[:], sm[:, 1, :], start=True, stop=True), reads=["triU", ("sm", 1)], writes=[pk])
            m.op("dve", lambda e, p=p: e.tensor_copy(sm[:, 2, :], p[:, 0:8]), reads=[pk], writes=[("sm", 2)])
            m.op("dve", lambda e, p=p: e.tensor_scalar(sm[:, 3, :], p[:, 0:8], -1.0, None, ALU.mult), reads=[pk], writes=[("sm", 3)])
            m.op("act", lambda e, p=p: e.activation(out=sm[:, 4, :], in_=p[:, 0:8], func=AF.Exp), reads=[pk], writes=[("sm", 4)])
            p2, p2k = nextp()
            m.op("pe", lambda e, p2=p2: e.matmul(p2[:, 0:8], ones[:], sm[:, 1, :], start=True, stop=True), reads=["ones", ("sm", 1)], writes=[p2k])
            m.op("act", lambda e, p2=p2: e.activation(out=sm[:, 5, :], in_=p2[:, 0:8], func=AF.Exp), reads=[p2k], writes=[("sm", 5)])
            m.op("dve", lambda e, p2=p2: e.tensor_tensor(sm[:, 7, :], p2[:, 0:8], sm[:, 2, :], ALU.subtract), reads=[p2k, ("sm", 2)], writes=[("sm", 7)])
            m.op("act", lambda e: e.activation(out=sm[:, 6, :], in_=sm[:, 7, :], func=AF.Exp), reads=[("sm", 7)], writes=[("sm", 6)])
            m.op("dve", lambda e: e.tensor_tensor(v3(xdt), v3(xs_tok), bc(sm[:, 0, :]), ALU.mult), reads=["xs_tok", ("sm", 0)], writes=["xdt"])
            m.op("pool", lambda e: e.tensor_tensor(v3(xw), v3(xdt), bc(sm[:, 6, :]), ALU.mult), reads=["xdt", ("sm", 6)], writes=["xw"])
            p, pk = nextp()
            m.op("pe", lambda e, p=p, tsub=tsub: e.matmul(p[:, 0:128], xc[:, 4, tsub], xc[:, 5, tsub], start=True, stop=True), reads=[("xc", 4), ("xc", 5)], writes=[pk])
            m.op("act", lambda e, p=p: e.copy(cbT[:], p[:, 0:128]), reads=[pk], writes=["cbT"])
            for half in range(2):
                p, pk = nextp()
                for q in range(4):
                    hd = half * 4 + q
                    te = tri_e[hd % 2]; tek = "tri_e%d" % (hd % 2)
                    m.op("dve", lambda e, te=te, hd=hd: e.tensor_scalar(te[:], triU[:], sm[:, 1, hd:hd + 1], None, ALU.mult), reads=["triU", ("sm", 1)], writes=[tek])
                    m.op("pe", lambda e, p=p, q=q, te=te: e.matmul(p[:, q * 128:(q + 1) * 128], ones[:], te[:], start=True, stop=False), reads=["ones", tek], writes=[pk])
                    m.op("pe", lambda e, p=p, q=q: e.matmul(p[:, q * 128:(q + 1) * 128], ident[:], negm[:], start=False, stop=True), reads=["ident", "negm"], writes=[pk])
                    m.op("act", lambda e, p=p, q=q, hd=hd: e.activation(out=decT[:, hd, :], in_=p[:, q * 128:(q + 1) * 128], func=AF.Exp, bias=sm[:, 3, hd:hd + 1]),
                         reads=[pk, ("sm", 3)], writes=[("decT", hd)])
                eng = "dve" if half == 0 else "pool"
                m.op(eng, lambda e, half=half: e.tensor_tensor(MT[:, half * 4:(half + 1) * 4, :], decT[:, half * 4:(half + 1) * 4, :],
                                                               cbT[:].unsqueeze(1).to_broadcast([128, 4, 128]), ALU.mult),
                     reads=[("decT", half * 4 + q) for q in range(4)] + ["cbT"], writes=[("MT", half)])
            pY, pYk = nextp()
            for hd in range(8):
                m.op("pe", lambda e, pY=pY, hd=hd: e.matmul(pY[:, hd * 64:(hd + 1) * 64], MT[:, hd, :], xdt[:, hd * 64:(hd + 1) * 64], start=True, stop=True),
                     reads=[("MT", hd // 4), "xdt"], writes=[pYk])
            pI, pIk = nextp()
            m.op("pe", lambda e, pI=pI, tsub=tsub: e.matmul(pI[:], xc[:, 5, tsub], hT[:], start=True, stop=True), reads=[("xc", 5), "hT"], writes=[pIk])
            m.op("dve", lambda e, pI=pI: e.tensor_tensor(v3(yi), pI[:].rearrange("p (e q) -> p e q", e=8), bc(sm[:, 4, :]), ALU.mult), reads=[pIk, ("sm", 4)], writes=["yi"])
            m.op("dve", lambda e, pY=pY: e.tensor_tensor(y[:], pY[:], yi[:], ALU.add), reads=[pYk, "yi"], writes=["y"])
            m.op("pool", lambda e: e.tensor_tensor(yi[:], xs_tok[:], dsk, ALU.mult), reads=["xs_tok", "rows", "yi"], writes=["yi"])
            m.op("dve", lambda e: e.tensor_tensor(y[:], y[:], yi[:], ALU.add), reads=["y", "yi"], writes=["y"])
            pS, pSk = nextp()
            m.op("pe", lambda e, pS=pS: e.matmul(pS[:], B_tok[:], xw[:], start=True, stop=True), reads=["B_tok", "xw"], writes=[pSk])
            m.op("pool", lambda e: e.tensor_tensor(v3(hT), v3(hT), bc(sm[:, 5, :]), ALU.mult), reads=["hT", ("sm", 5)], writes=["hT"])
            m.op("dve", lambda e, pS=pS: e.tensor_tensor(hT[:], hT[:], pS[:], ALU.add), reads=["hT", pSk], writes=["hT"])
            p, pk = nextp()
            for c in range(DC):
                m.op("pe", lambda e, p=p, c=c, tsub=tsub: e.matmul(p[:], x[:, c, tsub], wtm[:, c, 0:512], start=(c == 0), stop=(c == DC - 1)), reads=["wtm", xk], writes=[pk])
            m.op("act", lambda e, p=p: e.activation(out=zs[:], in_=p[:], func=AF.Silu), reads=[pk], writes=["zs"])
            m.op("dve", lambda e: e.tensor_tensor(y[:], y[:], zs[:], ALU.mult), reads=["y", "zs"], writes=["y"])
            m.op("act", lambda e: e.activation(out=junk[:], in_=y[:], func=AF.Square, accum_out=ss[:, 0:1]), reads=["y"], writes=["junk", "ss"])
            m.op("dve", lambda e: e.tensor_scalar(ss[:, 1:2], ss[:, 0:1], 1.0 / 512, RMS_EPS, ALU.mult, ALU.add), reads=["ss"], writes=["ss"])
            m.op("act", lambda e: e.activation(out=ss[:, 2:3], in_=ss[:, 1:2], func=AF.Sqrt), reads=["ss"], writes=["ss"])
            m.op("dve", lambda e: e.reciprocal(ss[:, 3:4], ss[:, 2:3]), reads=["ss"], writes=["ss"])
            m.op("dve", lambda e: e.scalar_tensor_tensor(y[:], y[:], ss[:, 3:4], nwb, ALU.mult, ALU.mult), reads=["y", "ss", "rows"], writes=["y"])
            p, pk = nextp()
            for c in range(DC):
                m.op("pe", lambda e, p=p, c=c, tsub=tsub: e.matmul(p[:], x[:, c, tsub], wtm[:, c, 512:1024], start=(c == 0), stop=(c == DC - 1)), reads=["wtm", xk], writes=[pk])
            m.op("act", lambda e, p=p: e.activation(out=gs[:], in_=p[:], func=AF.Sigmoid), reads=[pk], writes=["gs"])
            o = outt[oc % 2]; ok = "outt%d" % (oc % 2); oc += 1
            m.op("dve", lambda e, o=o: e.tensor_tensor(o[:], y[:], gs[:], ALU.mult), reads=["y", "gs"], writes=[ok])
            m.dma("sp", om[tg, :], o[:], reads=[ok], semkey=ok)
    m.finish("sp")
    return nc, m


def ssd_inputs(layer, inp, b, g):
    o = 3 * 2048
    W = inp["w_in"][layer]
    xo = o + 2048
    cols_x = slice(xo + g * 512, xo + (g + 1) * 512); cols_B = slice(xo + 2048 + g * 128, xo + 2048 + (g + 1) * 128)
    cols_C = slice(xo + 2560 + g * 128, xo + 2560 + (g + 1) * 128)
    d = {"wfm": lay_w(np.concatenate([W[:, cols_x], W[:, cols_B], W[:, cols_C]], axis=1))}
    wt = np.concatenate([W[:, o + g * 512:o + (g + 1) * 512], W[:, g * 512:(g + 1) * 512], W[:, xo + 3072 + g * 8:xo + 3072 + (g + 1) * 8]], axis=1)
    d["wtm"] = np.ascontiguousarray(wt.reshape(DC, 128, 1032).transpose(1, 0, 2))
    ch = np.concatenate([np.arange(g * 512, (g + 1) * 512), np.arange(2048 + g * 128, 2048 + (g + 1) * 128), np.arange(2560 + g * 128, 2560 + (g + 1) * 128)])
    cwv = inp["ssd_conv_w"][layer][:, ch]
    d["cw"] = np.ascontiguousarray(cwv.T.reshape(6, 128, 4).transpose(1, 0, 2))
    d["cb"] = np.ascontiguousarray(inp["ssd_conv_b"][layer][ch].reshape(6, 128).T)
    hs = slice(g * 8, (g + 1) * 8)
    rows = np.concatenate([inp["ssd_dt_bias"][layer][hs], inp["ssd_a_log"][layer][hs], np.zeros(8, np.float32),
                           np.repeat(inp["ssd_d"][layer][hs], 64), inp["ssd_norm_w"][layer][g * 512:(g + 1) * 512]]).astype(np.float32)
    d["rows"] = rows[None, :]
    ii = np.arange(128)
    d["triU"] = (ii[:, None] <= ii[None, :]).astype(np.float32)
    d["negmask"] = np.where(ii[None, :] >= ii[:, None], 0.0, -30000.0).astype(np.float32)
    d["ident"] = np.eye(128, dtype=np.float32)
    return d


DEC_C = float(np.exp(-0.5))
GN_EPS = 64e-5


def build_rwkv(S, has_v, TB=512, TB2=256):
    NCH = 16 + (1 if has_v else 0)
    nc = bass.Bass("TRN2", target_bir_lowering=False)
    m = MK(nc)
    dr = lambda name, shape, kind="ExternalInput", dt=F32: nc.dram_tensor(name, list(shape), dt, kind=kind).ap()
    xT = dr("xT", [D, S]); wfm_d = dr("wfm", [NCH, 128, DC, 128]); wg_d = dr("wg", [128, DC, 512])
    mu_d = dr("mu", [128, 17]); prm_d = dr("prm", [128, 4, 8]); rows_d = dr("rows", [1, 1024])
    w2_d = dr("w2", [96, 512]); a2_d = dr("a2", [96, 512]); g2_d = dr("g2", [256, 512])
    if has_v:
        v2_d = dr("v2", [64, 512])
    id_d = dr("ident", [128, 128]); bd_d = dr("blockdiag", [128, 128]); blk2_d = dr("blk8", [128, 4, 8]); hm_d = dr("hmask", [128, 2]); mask4_d = dr("mask4", [128, 512]); sl_d = dr("sl", [128, 128])
    if has_v:
        vfT = dr("vfT", [512, S])
    else:
        vf_out = dr("vf_out", [512, S], kind="ExternalOutput")
    om = dr("om", [S, 512], kind="ExternalOutput")
    shd = dr("shd", [NCH * 128, S], kind="Internal"); gate_d = dr("gate_s", [S, 512], kind="Internal")
    xT_v = xT.rearrange("(c p) t -> p c t", p=128)
    pb = [m.ps("pb%d" % i, [128, 512]) for i in range(8)]
    pcnt = [0]
    NROT = [8]
    def nextp():
        i = pcnt[0] % NROT[0]; pcnt[0] += 1
        return pb[i], "pb%d" % i
    ident = m.sb("ident_s", [128, 128]); mu = m.sb("mu_s", [128, 17])
    m.dma("sp", ident[:], id_d[:, :], writes=["ident"], semkey="c0"); m.dma("sp", mu[:], mu_d[:, :], writes=["mu"], semkey="c1")
    mk1 = m.mark()
    wfm = m.sb("wfm_s", [128, NCH, DC, 128], BF16); wg = m.sb("wg_s", [128, DC, 512], BF16)
    for j in range(NCH):
        m.dma("pool", wfm[:, j, :, :], wfm_d[j], writes=["wfm"], semkey="wfm")
    m.dma("pool", wg[:], wg_d[:, :, :], writes=["wg"], semkey="wg")
    xb = [m.sb("xb%d" % i, [128, DC, TB], BF16) for i in range(2)]
    ph = [m.sb("ph%d" % i, [128, TB + 1]) for i in range(2)]; dd = [m.sb("dd%d" % i, [128, TB]) for i in range(2)]
    sh = [m.sb("sh%d" % i, [128, TB]) for i in range(3)]; halo = m.sb("halo", [128, 17]); gst = [m.sb("gst%d" % i, [128, 512]) for i in range(2)]
    m.op("dve", lambda e: e.memset(halo[:], 0.0), writes=[("halo", j) for j in range(17)])
    def load_x(mt):
        i = mt % 2
        m.dma("pool", xb[i][:], xT_v[:, :, mt * TB:(mt + 1) * TB], writes=["xb%d" % i], semkey="xb%d" % i)
    load_x(0)
    NMT = S // TB
    cc = 0; gc = 0
    for mt in range(NMT):
        ts = slice(mt * TB, (mt + 1) * TB)
        if mt + 1 < NMT: load_x(mt + 1)
        x = xb[mt % 2]; xk = "xb%d" % (mt % 2)
        for j in range(NCH):
            p, pk = nextp()
            for c in range(DC):
                m.op("pe", lambda e, p=p, j=j, c=c: e.matmul(p[:], wfm[:, j, c, :], x[:, c, :], start=(c == 0), stop=(c == DC - 1)), reads=["wfm", xk], writes=[pk])
            a = ph[cc % 2]; ak = "ph%d" % (cc % 2); d_ = dd[cc % 2]; dk = "dd%d" % (cc % 2); o = sh[cc % 3]; ok = "sh%d" % (cc % 3); cc += 1
            m.op("act", lambda e, a=a, p=p: e.copy(a[:, 1:TB + 1], p[:]), reads=[pk], writes=[ak])
            m.op("act", lambda e, a=a, j=j: e.copy(a[:, 0:1], halo[:, j:j + 1]), reads=[("halo", j)], writes=[ak + "h"])
            m.op("pool", lambda e, a=a, d_=d_: e.tensor_tensor(d_[:], a[:, 0:TB], a[:, 1:TB + 1], ALU.subtract), reads=[ak, ak + "h"], writes=[dk])
            m.op("dve", lambda e, a=a, d_=d_, o=o, j=j: e.scalar_tensor_tensor(o[:], d_[:], mu[:, j:j + 1], a[:, 1:TB + 1], ALU.mult, ALU.add), reads=[dk, ak, "mu"], writes=[ok])
            m.op("pool", lambda e, a=a, j=j: e.tensor_copy(halo[:, j:j + 1], a[:, TB:TB + 1]), reads=[ak], writes=[("halo", j)])
            m.dma("sp", shd[j * 128:(j + 1) * 128, ts], o[:], reads=[ok], semkey=ok)
            if (not has_v) and 8 <= j < 12:
                m.dma("sp", vf_out[(j - 8) * 128:(j - 7) * 128, ts], o[:], reads=[ok], semkey=ok)
        for sub in range(TB // 128):
            tsub = slice(sub * 128, (sub + 1) * 128); tg = slice(mt * TB + sub * 128, mt * TB + (sub + 1) * 128)
            p, pk = nextp()
            for c in range(DC):
                m.op("pe", lambda e, p=p, c=c, tsub=tsub: e.matmul(p[:], x[:, c, tsub], wg[:, c, :], start=(c == 0), stop=(c == DC - 1)), reads=["wg", xk], writes=[pk])
            o = gst[gc % 2]; ok = "gst%d" % (gc % 2); gc += 1
            m.op("act", lambda e, o=o, p=p: e.activation(out=o[:], in_=p[:], func=AF.Sigmoid), reads=[pk], writes=[ok])
            m.dma("sp", gate_d[tg, :], o[:], reads=[ok], semkey=ok)
    m.release(mk1)
    NROT[0] = 5; pcnt[0] = 0
    pY = pb[7]; pS = pb[6]; pBn = pb[5]
    m.psum_keys.update(["pY", "pS", "pBn"])
    NT2 = TB2 // 128
    prm = m.sb("prm_s", [128, 4, 8]); rows = m.sb("rows_s", [128, 1024]); bdg = m.sb("bdg", [128, 128]); blk2 = m.sb("blk8_s", [128, 4, 8]); hm = m.sb("hm_s", [128, 2])
    mask4 = m.sb("mask4_s", [128, 512]); sl = m.sb("sl_s", [128, 128]); onesr = m.sb("onesr", [128, 128])
    w2b = m.sb("w2b", [128, 512], BF16); a2b = m.sb("a2b", [128, 512], BF16); g2b = m.sb("g2b", [128, 2, 512], BF16); v2b = m.sb("v2b", [64, 512], BF16)
    m.dma("sp", prm[:], prm_d[:, :, :], writes=["prm"], semkey="c2"); m.dma("sp", rows[:], rows_d[0:1, :].partition_broadcast(128), writes=["rows"], semkey="c3")
    m.dma("sp", bdg[:], bd_d[:, :], writes=["bdg"], semkey="c4"); m.dma("sp", blk2[:], blk2_d[:, :, :], writes=["blk2"], semkey="c5"); m.dma("sp", hm[:], hm_d[:, :], writes=["hm"], semkey="c12")
    m.dma("sp", mask4[:], mask4_d[:, :], writes=["mask4"], semkey="c6"); m.dma("sp", sl[:], sl_d[:, :], writes=["sl"], semkey="c7")
    m.dma("pool", w2b[0:96, :], w2_d[:, :], writes=["w2b"], semkey="c8"); m.dma("pool", a2b[0:96, :], a2_d[:, :], writes=["a2b"], semkey="c9")
    m.dma("pool", g2b[:], g2_d.rearrange("(c p) n -> p c n", p=128), writes=["g2b"], semkey="c10")
    if has_v:
        m.dma("pool", v2b[:], v2_d[:, :], writes=["v2b"], semkey="c11")
    m.op("dve", lambda e: e.memset(onesr[:], 1.0), writes=["onesr"])
    lnw = rows[:, 0:512]; lnb = rows[:, 512:1024]
    PK, PA, PR, PW0, PA0, PV0 = range(6)
    rkv = [m.sb("rkv%d" % i, [128, 3, 4, TB2]) for i in range(2)]; lo = [m.sb("lo%d" % i, [128, 5, TB2]) for i in range(2)]
    vfb = [m.sb("vfb%d" % i, [128, 4, TB2]) for i in range(2)] if has_v else None
    tanhw = m.sb("tanhw", [128, TB2], BF16); alob = m.sb("alob", [128, TB2], BF16); sgl = m.sb("sgl", [128, 2, TB2], BF16); vlob = m.sb("vlob", [128, TB2], BF16)
    T = {n: m.sb("t_" + n, [128, TB2]) for n in ["lw", "al", "vm", "kk", "sq", "inv", "kkn", "km", "b", "cl", "ep", "em", "ew", "ee", "t1", "kh", "bh", "rkr"]}
    AR = m.sb("AR", [128, 4, NT2, 2, 128]); BK = m.sb("BK", [128, 4, NT2, 2, 128])
    khat = m.sb("khat", [128, NT2, 512]); bhat = m.sb("bhat", [128, NT2, 512]); Vtok = m.sb("Vtok", [128, NT2, 512])
    bonus = m.sb("bonus", [128, NT2, 8]); gamT = m.sb("gamT", [128, 4, NT2])
    M4 = m.sb("M4", [128, 4, 512]); Mpow = m.sb("Mpow", [128, 4, 6, 128]); Ncur = [m.sb("Ncur%d" % i, [128, 4, 128]) for i in range(2)]
    Ub = [m.sb("Ub%d" % i, [128, 4, 64]) for i in range(2)]; Uall = m.sb("Uall", [128, 512])
    ST = m.sb("ST", [128, 4, 64])
    ysb = m.sb("ysb", [128, 512]); ysq = m.sb("ysq", [128, 512]); gtok = m.sb("gtok", [128, 512]); gts = [m.sb("gts%d" % i, [128, 512]) for i in range(2)]
    st8 = m.sb("st8", [128, 6, 8]); bv = m.sb("bv", [128, 512]); outt = [m.sb("outt%d" % i, [128, 512]) for i in range(2)]
    m.op("dve", lambda e: e.memset(ST[:], 0.0), writes=["ST"])
    ARf = AR[:].rearrange("p a n b t -> p (a n b t)"); BKf = BK[:].rearrange("p a n b t -> p (a n b t)")
    arc = lambda pr, n, a: ((pr * NT2 + n) * 2 + a) * 128
    ARm = m.sb("ARm", [128, 4, NT2, 2, 2, 128]); ARmf = ARm[:].rearrange("p a n h b t -> p (a n h b t)")
    armc = lambda pr, n, hf, a: (((pr * NT2 + n) * 2 + hf) * 2 + a) * 128
    M4f = M4[:].rearrange("p h c -> p (h c)"); Mpf = Mpow[:].rearrange("p h l t -> p (h l t)")
    Ncf = [t[:].rearrange("p h t -> p (h t)") for t in Ncur]; Ubf = [t[:].rearrange("p h v -> p (h v)") for t in Ub]
    STf = ST[:].rearrange("p a v -> p (a v)")
    khf = khat[:].rearrange("p n c -> p (n c)"); bhf = bhat[:].rearrange("p n c -> p (n c)"); Vtf = Vtok[:].rearrange("p n c -> p (n c)")
    sglf = sgl[:].rearrange("p k t -> p (k t)"); g2f = g2b[:].rearrange("p k c -> p (k c)")
    vv = m.sb("vv", [128, TB2])
    def bc(ap8):
        return ap8.unsqueeze(2).to_broadcast([128, 8, 64])
    v3 = lambda a: a.rearrange("p (e q) -> p e q", e=8)
    tv = lambda a: a.rearrange("p (n t) -> p n t", n=NT2)
    NM2 = S // TB2
    def load2(i2):
        bi = i2 % 2; ts = slice(i2 * TB2, (i2 + 1) * TB2)
        for kind in range(3):
            m.dma("sp", rkv[bi][:, kind, :, :], shd[kind * 512:(kind + 1) * 512, ts].rearrange("(c p) t -> p c t", p=128), writes=[("rkv", bi, kind, pr) for pr in range(4)], semkey="rkv%d" % bi)
        nl = NCH - 12
        m.dma("sp", lo[bi][:, 0:nl, :], shd[1536:1536 + nl * 128, ts].rearrange("(c p) t -> p c t", p=128), writes=[("lo", bi)], semkey="lo%d" % bi)
        if has_v:
            m.dma("sp", vfb[bi][:], vfT[:, ts].rearrange("(c p) t -> p c t", p=128), writes=[("vfb", bi)], semkey="vfb%d" % bi)
    load2(0)
    oc = 0; gcn = 0
    for i2 in range(NM2):
        if i2 + 1 < NM2: load2(i2 + 1)
        bi = i2 % 2
        RK = rkv[bi]; LO = lo[bi]
        m.op("act", lambda e: e.activation(out=tanhw[:], in_=LO[:, 0, :], func=AF.Tanh), reads=[("lo", bi)], writes=["tanhw"])
        m.op("act", lambda e: e.copy(alob[:], LO[:, 1, :]), reads=[("lo", bi)], writes=["alob"])
        m.op("act", lambda e: e.activation(out=sgl[:], in_=LO[:, 2:4, :], func=AF.Sigmoid), reads=[("lo", bi)], writes=["sgl"])
        if has_v:
            m.op("act", lambda e: e.copy(vlob[:], LO[:, 4, :]), reads=[("lo", bi)], writes=["vlob"])
        for pr in range(4):
            cs = slice(pr * 128, (pr + 1) * 128)
            r_ = RK[:, 0, pr, :]; k_ = RK[:, 1, pr, :]; v_ = RK[:, 2, pr, :]
            rk_keys = [("rkv", bi, kind, pr) for kind in range(3)]
            P_ = lambda col: prm[:, pr, col:col + 1]
            p, pk = nextp()
            m.op("pe", lambda e, p=p, cs=cs: e.matmul(p[:, 0:TB2], w2b[0:96, cs], tanhw[0:96, :], start=True, stop=True), reads=["w2b", "tanhw"], writes=[pk])
            m.op("act", lambda e, p=p: e.activation(out=T["lw"][:], in_=p[:, 0:TB2], func=AF.Sigmoid, bias=P_(PW0)), reads=[pk, "prm"], writes=["lw"])
            m.op("pool", lambda e: e.tensor_scalar(T["lw"][:], T["lw"][:], -DEC_C, None, ALU.mult), reads=["lw"], writes=["lw"])
            p, pk = nextp()
            m.op("pe", lambda e, p=p, cs=cs: e.matmul(p[:, 0:TB2], a2b[0:96, cs], alob[0:96, :], start=True, stop=True), reads=["a2b", "alob"], writes=[pk])
            m.op("act", lambda e, p=p: e.activation(out=T["al"][:], in_=p[:, 0:TB2], func=AF.Sigmoid, bias=P_(PA0)), reads=[pk, "prm"], writes=["al"])
            if has_v:
                p, pk = nextp()
                m.op("pe", lambda e, p=p, cs=cs: e.matmul(p[:, 0:TB2], v2b[0:64, cs], vlob[0:64, :], start=True, stop=True), reads=["v2b", "vlob"], writes=[pk])
                m.op("act", lambda e, p=p: e.activation(out=T["vm"][:], in_=p[:, 0:TB2], func=AF.Sigmoid, bias=P_(PV0)), reads=[pk, "prm"], writes=["vm"])
                m.op("pool", lambda e: e.tensor_tensor(T["t1"][:], vfb[bi][:, pr, :], v_, ALU.subtract), reads=[("vfb", bi), ("rkv", bi, 2, pr)], writes=["t1"])
                m.op("pool", lambda e: e.tensor_tensor(T["t1"][:], T["t1"][:], T["vm"][:], ALU.mult), reads=["t1", "vm"], writes=["t1"])
                m.op("dve", lambda e: e.tensor_tensor(v_, v_, T["t1"][:], ALU.add), reads=["t1", ("rkv", bi, 2, pr)], writes=[("rkv", bi, 2, pr)])
            m.op("pool", lambda e: e.tensor_scalar(T["kk"][:], k_, P_(PK), None, ALU.mult), reads=[("rkv", bi, 1, pr), "prm"], writes=["kk"])
            m.op("act", lambda e: e.activation(out=T["sq"][:], in_=T["kk"][:], func=AF.Square), reads=["kk"], writes=["sq"])
            p, pk = nextp()
            m.op("pe", lambda e, p=p: e.matmul(p[:, 0:TB2], bdg[:], T["sq"][:], start=True, stop=True), reads=["bdg", "sq"], writes=[pk])
            m.op("act", lambda e, p=p: e.activation(out=T["inv"][:], in_=p[:, 0:TB2], func=AF.Sqrt), reads=[pk], writes=["inv"])
            m.op("dve", lambda e: e.tensor_scalar(T["inv"][:], T["inv"][:], 1e-12, None, ALU.max), reads=["inv"], writes=["inv"])
            m.op("dve", lambda e: e.reciprocal(T["inv"][:], T["inv"][:]), reads=["inv"], writes=["inv"])
            m.op("dve", lambda e: e.tensor_tensor(T["kkn"][:], T["kk"][:], T["inv"][:], ALU.mult), reads=["kk", "inv"], writes=["kkn"])
            m.op("dve", lambda e: e.tensor_scalar(T["t1"][:], T["al"][:], -1.0, P_(PA), ALU.add, ALU.mult), reads=["al", "prm", "t1"], writes=["t1"])
            m.op("dve", lambda e: e.scalar_tensor_tensor(T["km"][:], T["t1"][:], 1.0, k_, ALU.add, ALU.mult), reads=["t1", ("rkv", bi, 1, pr)], writes=["km"])
            m.op("pool", lambda e: e.tensor_tensor(T["b"][:], T["kkn"][:], T["al"][:], ALU.mult), reads=["kkn", "al"], writes=["b"])
            for n in range(NT2):
                tn = slice(n * 128, (n + 1) * 128)
                m.op("dve", lambda e, tn=tn: e.tensor_tensor_scan(T["cl"][:, tn], onesr[:], T["lw"][:, tn], 0.0, ALU.mult, ALU.add), reads=["lw", "onesr"], writes=[("cl", n)])
            clk = [("cl", n) for n in range(NT2)]
            m.op("act", lambda e: e.activation(out=T["ep"][:], in_=T["cl"][:], func=AF.Exp), reads=clk, writes=["ep"])
            m.op("act", lambda e: e.activation(out=T["em"][:], in_=T["cl"][:], func=AF.Exp, scale=-1.0), reads=clk, writes=["em"])
            m.op("pool", lambda e: e.tensor_tensor(T["ew"][:], T["cl"][:], T["lw"][:], ALU.subtract), reads=clk + ["lw"], writes=["ew"])
            m.op("act", lambda e: e.activation(out=T["ew"][:], in_=T["ew"][:], func=AF.Exp), reads=["ew"], writes=["ew"])
            m.op("dve", lambda e: e.tensor_tensor(AR[:, pr, :, 1, :], tv(r_), tv(T["ep"][:]), ALU.mult), reads=[("rkv", bi, 0, pr), "ep"], writes=[("AR", pr)])
            m.op("dve", lambda e: e.scalar_tensor_tensor(AR[:, pr, :, 0, :], tv(T["kkn"][:]), -1.0, tv(T["ew"][:]), ALU.mult, ALU.mult), reads=["kkn", "ew", ("AR", pr)], writes=[("AR", pr)])
            for hf in range(2):
                m.op("pool", lambda e, hf=hf: e.tensor_scalar(ARm[:, pr, :, hf, :, :], AR[:, pr, :, :, :], hm[:, hf:hf + 1], None, ALU.mult), reads=[("AR", pr), "hm", ("ARm", pr)], writes=[("ARm", pr)])
            m.op("pool", lambda e: e.tensor_tensor(BK[:, pr, :, 0, :], tv(T["b"][:]), tv(T["em"][:]), ALU.mult), reads=["b", "em"], writes=[("BK", pr)])
            m.op("pool", lambda e: e.tensor_tensor(BK[:, pr, :, 1, :], tv(T["km"][:]), tv(T["em"][:]), ALU.mult), reads=["km", "em", ("BK", pr)], writes=[("BK", pr)])
            for n in range(NT2):
                tn = slice(n * 128, (n + 1) * 128); last = n * 128 + 127
                m.op("act", lambda e, tn=tn, last=last: e.activation(out=T["ee"][:, tn], in_=T["cl"][:, tn], func=AF.Exp, scale=-1.0, bias=T["cl"][:, last:last + 1]),
                     reads=clk, writes=[("ee", n)])
                m.op("pool", lambda e, n=n, last=last: e.tensor_copy(gamT[:, pr, n:n + 1], T["ep"][:, last:last + 1]), reads=["ep"], writes=[("gamT", pr)])
            eek = [("ee", n) for n in range(NT2)]
            m.op("dve", lambda e: e.tensor_tensor(T["kh"][:], T["km"][:], T["ee"][:], ALU.mult), reads=["km"] + eek, writes=["kh"])
            m.op("pool", lambda e: e.tensor_tensor(T["bh"][:], T["b"][:], T["ee"][:], ALU.mult), reads=["b"] + eek, writes=["bh"])
            m.op("dve", lambda e: e.scalar_tensor_tensor(T["rkr"][:], r_, P_(PR), T["km"][:], ALU.mult, ALU.mult), reads=[("rkv", bi, 0, pr), "prm", "km"], writes=["rkr"])
            m.op("dve", lambda e: e.tensor_copy(vv[:], v_), reads=[("rkv", bi, 2, pr)], writes=["vv"])
            for n in range(NT2):
                tn = slice(n * 128, (n + 1) * 128)
                p, pk = nextp()
                m.op("pe", lambda e, p=p, tn=tn: e.transpose(p[:, 0:128], T["kh"][:, tn], ident[:]), reads=["kh", "ident"], writes=[pk])
                m.op("pe", lambda e, p=p, tn=tn: e.transpose(p[:, 128:256], T["bh"][:, tn], ident[:]), reads=["bh", "ident"], writes=[pk])
                m.op("pe", lambda e, p=p, tn=tn: e.transpose(p[:, 256:384], vv[:, tn], ident[:]), reads=["vv", "ident"], writes=[pk])
                m.op("act", lambda e, p=p, n=n, cs=cs: e.copy(khat[:, n, cs], p[:, 0:128]), reads=[pk], writes=[("khat", n)])
                m.op("act", lambda e, p=p, n=n, cs=cs: e.copy(bhat[:, n, cs], p[:, 128:256]), reads=[pk], writes=[("bhat", n)])
                m.op("dve", lambda e, p=p, n=n, cs=cs: e.tensor_copy(Vtok[:, n, cs], p[:, 256:384]), reads=[pk], writes=[("Vtok", n)])
                m.op("pe", lambda e, n=n, tn=tn: e.matmul(pBn[:, n * 8:(n + 1) * 8], T["rkr"][:, tn], blk2[:, pr, :], start=(pr == 0 and n == 0), stop=(pr == 3), skip_group_check=True), reads=["rkr", "blk2"], writes=["pBn"])
        m.op("act", lambda e: e.copy(bonus[:].rearrange("p n e -> p (n e)"), pBn[:, 0:NT2 * 8]), reads=["pBn"], writes=["bonus"])
        for n in range(NT2):
            tn = slice(n * 128, (n + 1) * 128); tg = slice(i2 * TB2 + n * 128, i2 * TB2 + (n + 1) * 128)
            gi_ = gcn % 2; gcn += 1
            m.dma("sp", gts[gi_][:], gate_d[tg, :], writes=["gts%d" % gi_], semkey="gts%d" % gi_)
            for grp in range(2):
                def opnd(hh):
                    h = grp * 4 + hh; pr = h // 2
                    return h, pr, h % 2
                for hh in range(4):
                    h, pr, prt = opnd(hh)
                    p, pk = nextp()
                    rhsAR = ARmf[:, armc(pr, n, prt, 0):armc(pr, n, prt, 0) + 256]
                    m.op("pe", lambda e, p=p, prt=prt, pr=pr, rhsAR=rhsAR: e.matmul(p[:, 0:256], BKf[:, arc(pr, n, 0):arc(pr, n, 0) + 128], rhsAR, start=True, stop=True), reads=[("BK", pr), ("ARm", pr)], writes=[pk])
                    m.op("pe", lambda e, p=p, prt=prt, pr=pr, rhsAR=rhsAR: e.matmul(p[:, 256:512], BKf[:, arc(pr, n, 1):arc(pr, n, 1) + 128], rhsAR, start=True, stop=True), reads=[("BK", pr), ("ARm", pr)], writes=[pk])
                    m.op("dve", lambda e, p=p, hh=hh: e.tensor_tensor(M4[:, hh, :], p[:], mask4[:], ALU.mult), reads=[pk, "mask4"], writes=[("M4", hh)])
                p, pk = nextp()
                for hh in range(4):
                    h, pr, prt = opnd(hh)
                    m.op("pe", lambda e, p=p, hh=hh, prt=prt, pr=pr: e.matmul(p[:, hh * 128:(hh + 1) * 128], ARmf[:, armc(pr, n, prt, 0):armc(pr, n, prt, 0) + 128], BKf[:, arc(pr, n, 0):arc(pr, n, 0) + 128], start=True, stop=True), reads=[("BK", pr), ("ARm", pr)], writes=[pk])
                m.op("dve", lambda e, p=p: e.tensor_tensor(Ncur[0][:], p[:].rearrange("p (h t) -> p h t", h=4), sl[:].unsqueeze(1).to_broadcast([128, 4, 128]), ALU.mult), reads=[pk, "sl"], writes=["Ncur0"])
                Mlev = lambda hh, lev: (M4f[:, hh * 512:hh * 512 + 128] if lev == 0 else Mpf[:, (hh * 6 + lev - 1) * 128:(hh * 6 + lev) * 128])
                mkey = lambda hh, lev: (("M4", hh) if lev == 0 else ("Mpow", lev))
                for lev in range(1, 7):
                    nprev = Ncf[(lev - 1) % 2]; nprevk = "Ncur%d" % ((lev - 1) % 2); nnew = Ncur[lev % 2]; nnewk = "Ncur%d" % (lev % 2)
                    p, pk = nextp()
                    for hh in range(4):
                        m.op("pe", lambda e, p=p, hh=hh, lev=lev, nprev=nprev: e.matmul(p[:, hh * 128:(hh + 1) * 128], nprev[:, hh * 128:(hh + 1) * 128], Mlev(hh, lev - 1), start=True, stop=True),
                             reads=[nprevk, mkey(hh, lev - 1)], writes=[pk])
                    m.op("act", lambda e, p=p, lev=lev: e.copy(Mpow[:, :, lev - 1, :], p[:].rearrange("p (h t) -> p h t", h=4)), reads=[pk], writes=[("Mpow", lev)])
                    if lev < 6:
                        p2, p2k = nextp()
                        for hh in range(4):
                            m.op("pe", lambda e, p2=p2, hh=hh, lev=lev, nprev=nprev: e.matmul(p2[:, hh * 128:(hh + 1) * 128], Mlev(hh, lev - 1), nprev[:, hh * 128:(hh + 1) * 128], start=True, stop=True),
                                 reads=[nprevk, mkey(hh, lev - 1)], writes=[p2k])
                        m.op("dve", lambda e, p2=p2, nnew=nnew: e.tensor_copy(nnew[:], p2[:].rearrange("p (h t) -> p h t", h=4)), reads=[p2k], writes=[nnewk])
                p, pk = nextp()
                for hh in range(4):
                    h, pr, prt = opnd(hh)
                    m.op("pe", lambda e, p=p, hh=hh, prt=prt, pr=pr: e.matmul(p[:, hh * 64:(hh + 1) * 64], ARmf[:, armc(pr, n, prt, 0):armc(pr, n, prt, 0) + 128], STf[:, pr * 64:(pr + 1) * 64], start=True, stop=False), reads=[("ARm", pr), "ST"], writes=[pk])
                    m.op("pe", lambda e, p=p, hh=hh, h=h: e.matmul(p[:, hh * 64:(hh + 1) * 64], M4f[:, hh * 512 + 256:hh * 512 + 384], Vtf[:, n * 512 + h * 64:n * 512 + (h + 1) * 64], start=False, stop=True), reads=[("M4", hh), ("Vtok", n)], writes=[pk])
                m.op("act", lambda e, p=p: e.copy(Ub[0][:].rearrange("p h v -> p (h v)"), p[:, 0:256]), reads=[pk], writes=["Ub0"])
                for lev in range(7):
                    uc = Ubf[lev % 2]; uck = "Ub%d" % (lev % 2)
                    p, pk = nextp()
                    for hh in range(4):
                        m.op("pe", lambda e, p=p, hh=hh, uc=uc: e.matmul(p[:, hh * 64:(hh + 1) * 64], ident[:], uc[:, hh * 64:(hh + 1) * 64], start=True, stop=False), reads=["ident", uck], writes=[pk])
                        m.op("pe", lambda e, p=p, hh=hh, uc=uc, lev=lev: e.matmul(p[:, hh * 64:(hh + 1) * 64], Mlev(hh, lev), uc[:, hh * 64:(hh + 1) * 64], start=False, stop=True), reads=[mkey(hh, lev), uck], writes=[pk])
                    if lev < 6:
                        un = Ub[(lev + 1) % 2]; unk = "Ub%d" % ((lev + 1) % 2)
                        eng = "act" if lev % 2 == 0 else "dve"
                        if eng == "act":
                            m.op("act", lambda e, p=p, un=un: e.copy(un[:].rearrange("p h v -> p (h v)"), p[:, 0:256]), reads=[pk], writes=[unk])
                        else:
                            m.op("dve", lambda e, p=p, un=un: e.tensor_copy(un[:].rearrange("p h v -> p (h v)"), p[:, 0:256]), reads=[pk], writes=[unk])
                    else:
                        m.op("act", lambda e, p=p, grp=grp: e.copy(Uall[:, grp * 256:(grp + 1) * 256], p[:, 0:256]), reads=[pk], writes=[("Uall", grp)])
                for hh in range(4):
                    h, pr, prt = opnd(hh)
                    hs = slice(h * 64, (h + 1) * 64)
                    m.op("pe", lambda e, hs=hs, prt=prt, pr=pr: e.matmul(pY[:, hs], ARmf[:, armc(pr, n, prt, 1):armc(pr, n, prt, 1) + 128], STf[:, pr * 64:(pr + 1) * 64], start=True, stop=False), reads=[("ARm", pr), "ST"], writes=["pY"])
                    m.op("pe", lambda e, hs=hs, hh=hh: e.matmul(pY[:, hs], M4f[:, hh * 512 + 128:hh * 512 + 256], Uall[:, hs], start=False, stop=False), reads=[("M4", hh), ("Uall", grp)], writes=["pY"])
                    m.op("pe", lambda e, hs=hs, hh=hh, h=h: e.matmul(pY[:, hs], M4f[:, hh * 512 + 384:hh * 512 + 512], Vtf[:, n * 512 + h * 64:n * 512 + (h + 1) * 64], start=False, stop=True), reads=[("M4", hh), ("Vtok", n)], writes=["pY"])
            for pr in range(4):
                cs = slice(pr * 128, (pr + 1) * 128)
                m.op("pe", lambda e, cs=cs, pr=pr: e.matmul(pS[:, cs], bhf[:, n * 512 + pr * 128:n * 512 + (pr + 1) * 128], Uall[:, cs], start=True, stop=False), reads=[("bhat", n), ("Uall", 0), ("Uall", 1)], writes=["pS"])
                m.op("pe", lambda e, cs=cs, pr=pr: e.matmul(pS[:, cs], khf[:, n * 512 + pr * 128:n * 512 + (pr + 1) * 128], Vtf[:, n * 512 + pr * 128:n * 512 + (pr + 1) * 128], start=False, stop=True), reads=[("khat", n), ("Vtok", n)], writes=["pS"])
            for pr in range(4):
                for hf in range(2):
                    prt = slice(hf * 64, hf * 64 + 64); c0 = pr * 128 + hf * 64
                    m.op("dve", lambda e, pr=pr, prt=prt, c0=c0: e.scalar_tensor_tensor(ST[prt, pr, :], ST[prt, pr, :], gamT[prt, pr, n:n + 1], pS[prt, c0:c0 + 64], ALU.mult, ALU.add),
                         reads=["ST", ("gamT", pr), "pS", "pY"], writes=["ST"])
            p, pk = nextp()
            for kc in range(2):
                m.op("pe", lambda e, p=p, kc=kc, tn=tn: e.matmul(p[:], sglf[:, kc * TB2 + tn.start:kc * TB2 + tn.stop], g2f[:, kc * 512:(kc + 1) * 512], start=(kc == 0), stop=(kc == 1)), reads=["sgl", "g2b"], writes=[pk])
            m.op("act", lambda e, p=p: e.copy(gtok[:], p[:]), reads=[pk], writes=["gtok"])
            m.op("act", lambda e: e.copy(ysb[:], pY[:]), reads=["pY"], writes=["ysb"])
            m.op("act", lambda e: e.activation(out=ysq[:], in_=ysb[:], func=AF.Square), reads=["ysb"], writes=["ysq"])
            m.op("dve", lambda e: e.tensor_reduce(st8[:, 0, :], v3(ysb[:]), AX.X, ALU.add), reads=["ysb"], writes=["st8"])
            m.op("dve", lambda e: e.tensor_reduce(st8[:, 1, :], v3(ysq[:]), AX.X, ALU.add), reads=["ysq", "st8"], writes=["st8"])
            m.op("dve", lambda e: e.tensor_scalar(st8[:, 2, :], st8[:, 0, :], 1.0 / 64, None, ALU.mult), reads=["st8"], writes=["st8"])
            m.op("dve", lambda e: e.tensor_tensor(st8[:, 3, :], st8[:, 2, :], st8[:, 2, :], ALU.mult), reads=["st8"], writes=["st8"])
            m.op("dve", lambda e: e.scalar_tensor_tensor(st8[:, 4, :], st8[:, 1, :], 1.0 / 64, st8[:, 3, :], ALU.mult, ALU.subtract), reads=["st8"], writes=["st8"])
            m.op("dve", lambda e: e.tensor_scalar(st8[:, 4, :], st8[:, 4, :], GN_EPS, None, ALU.add), reads=["st8"], writes=["st8"])
            m.op("act", lambda e: e.activation(out=st8[:, 5, :], in_=st8[:, 4, :], func=AF.Sqrt), reads=["st8"], writes=["st8"])
            m.op("dve", lambda e: e.reciprocal(st8[:, 5, :], st8[:, 5, :]), reads=["st8"], writes=["st8"])
            m.op("dve", lambda e: e.tensor_tensor(v3(ysb[:]), v3(ysb[:]), bc(st8[:, 2, :]), ALU.subtract), reads=["ysb", "st8"], writes=["ysb"])
            m.op("dve", lambda e: e.tensor_tensor(v3(ysb[:]), v3(ysb[:]), bc(st8[:, 5, :]), ALU.mult), reads=["ysb", "st8"], writes=["ysb"])
            m.op("pool", lambda e: e.tensor_tensor(ysb[:], ysb[:], lnw, ALU.mult), reads=["ysb", "rows"], writes=["ysb"])
            m.op("pool", lambda e: e.tensor_tensor(ysb[:], ysb[:], lnb, ALU.add), reads=["ysb", "rows"], writes=["ysb"])
            m.op("dve", lambda e: e.tensor_tensor(v3(bv[:]), v3(Vtok[:, n, :]), bc(bonus[:, n, :]), ALU.mult), reads=[("Vtok", n), "bonus"], writes=["bv"])
            m.op("pool", lambda e: e.tensor_tensor(ysb[:], ysb[:], bv[:], ALU.add), reads=["ysb", "bv"], writes=["ysb"])
            m.op("pool", lambda e: e.tensor_tensor(ysb[:], ysb[:], gtok[:], ALU.mult), reads=["ysb", "gtok"], writes=["ysb"])
            o = outt[oc % 2]; ok = "outt%d" % (oc % 2); oc += 1
            m.op("dve", lambda e, o=o: e.tensor_tensor(o[:], ysb[:], gts[gi_][:], ALU.mult), reads=["ysb", "gts%d" % gi_], writes=[ok])
            m.dma("sp", om[tg, :], o[:], reads=[ok], semkey=ok)
    m.finish("sp")
    return nc, m


def rwkv_inputs(layer, inp, b, g):
    o = 3 * 2048 + 5152 + 1088
    W = inp["w_in"][layer]; mu = inp["rwkv_mu"][layer]
    has_v = layer > 0
    cs = slice(g * 512, (g + 1) * 512)
    def pad128(a):
        n = (-a.shape[-1]) % 128
        return np.concatenate([a, np.zeros(a.shape[:-1] + (n,), a.dtype)], axis=-1) if n else a
    cols = [W[:, o + g * 512:o + (g + 1) * 512], W[:, o + 2048 + g * 512:o + 2048 + (g + 1) * 512], W[:, o + 4096 + g * 512:o + 4096 + (g + 1) * 512],
            pad128(W[:, o + 6144:o + 6240]), pad128(W[:, o + 6240:o + 6336]), W[:, o + 6336:o + 6592]]
    mus = [mu[g * 512:(g + 1) * 512], mu[2048 + g * 512:2048 + (g + 1) * 512], mu[4096 + g * 512:4096 + (g + 1) * 512],
           pad128(mu[6144:6240]), pad128(mu[6240:6336]), mu[6336:6592]]
    if has_v:
        cols.append(pad128(inp["w_in_vres"][layer - 1])); mus.append(pad128(inp["rwkv_mu_vres"][layer - 1]))
    d = {"wfm": lay_w(np.concatenate(cols, axis=1))}
    muv = np.concatenate(mus)
    mu17 = np.zeros((128, 17), np.float32); mu17[:, :muv.size // 128] = muv.reshape(-1, 128).T
    d["mu"] = mu17
    gcol = 2048 + g * 512
    d["wg"] = np.ascontiguousarray(W[:, gcol:gcol + 512].reshape(DC, 128, 512).transpose(1, 0, 2))
    prm = np.zeros((128, 4, 8), np.float32)
    vecs = [inp["rwkv_k_k"][layer], inp["rwkv_k_a"][layer], inp["rwkv_r_k"][layer], inp["rwkv_w0"][layer], inp["rwkv_a0"][layer]]
    if has_v: vecs.append(inp["rwkv_v0"][layer - 1])
    for i, v in enumerate(vecs):
        prm[:, :, i] = v[cs].reshape(4, 128).T
    d["prm"] = prm
    d["rows"] = np.concatenate([inp["rwkv_ln_w"][layer][cs], inp["rwkv_ln_b"][layer][cs]])[None, :].astype(np.float32)
    d["w2"] = np.ascontiguousarray(inp["rwkv_w2"][layer][:, cs]); d["a2"] = np.ascontiguousarray(inp["rwkv_a2"][layer][:, cs])
    d["g2"] = np.ascontiguousarray(inp["rwkv_g2"][layer][:, cs])
    if has_v:
        d["v2"] = np.ascontiguousarray(inp["rwkv_v2"][layer - 1][:, cs])
    ii = np.arange(128)
    d["ident"] = np.eye(128, dtype=np.float32)
    d["blockdiag"] = ((ii[:, None] // 64) == (ii[None, :] // 64)).astype(np.float32)
    blk8 = np.zeros((128, 4, 8), np.float32)
    for pr in range(4):
        blk8[:64, pr, 2 * pr] = 1; blk8[64:, pr, 2 * pr + 1] = 1
    d["blk8"] = blk8
    hmk = np.zeros((128, 2), np.float32); hmk[:64, 0] = 1; hmk[64:, 1] = 1
    d["hmask"] = hmk
    su = (ii[:, None] < ii[None, :]).astype(np.float32); iu = (ii[:, None] <= ii[None, :]).astype(np.float32)
    d["mask4"] = np.concatenate([su, iu, su, iu], axis=1)
    d["sl"] = (ii[None, :] < ii[:, None]).astype(np.float32)
    return d


SEQ = 16384; NB = 2; NCORE = 8
_PROG = {}


def _prog(key, fn):
    if key not in _PROG:
        _PROG[key] = fn()[0]
    return _PROG[key]


def _launch(nc, in_maps):
    res = run_bass_kernel_spmd(nc, in_maps, core_ids=list(range(NCORE)))
    return res.results


def kernel(**inputs):
    import time
    t00 = time.time()
    inp = {k: np.asarray(v) for k, v in inputs.items()}
    x = inp["x"]
    xT = [np.ascontiguousarray(x[b].T) for b in range(NB)]
    vf = [None] * NCORE
    for layer in range(2):
        t0 = time.time()
        outs = {}
        for name, build, mk_in in (("ssd", lambda: build_ssd(SEQ), ssd_inputs), ("rwkv", None, rwkv_inputs), ("mla", lambda: build_mla(SEQ), mla_inputs)):
            if name == "rwkv":
                nc = _prog(("rwkv", layer > 0), lambda: build_rwkv(SEQ, layer > 0))
            else:
                nc = _prog(name, build)
            maps = []
            shared = {}
            for c in range(NCORE):
                b, g = c // 4, c % 4
                if g not in shared:
                    shared[g] = mk_in(layer, inp, 0, g)
                d = dict(shared[g])
                if name == "mla":
                    d["pos"] = np.ascontiguousarray(inp["positions"][b][None, :]).astype(np.int32)
                d["xT"] = xT[b]
                if name == "rwkv" and layer > 0:
                    d["vfT"] = vf[c]
                maps.append(d)
            r = _launch(nc, maps)
            outs[name] = [r[c]["om"] for c in range(NCORE)]
            if name == "rwkv" and layer == 0:
                vf = [r[c]["vf_out"] for c in range(NCORE)]
            print("[kernel] layer %d %s done %.1fs (total %.1fs)" % (layer, name, time.time() - t0, time.time() - t00), flush=True)
        mT = {name: [np.ascontiguousarray(np.concatenate([outs[name][b * 4 + g] for g in range(4)], axis=1).T) for b in range(NB)] for name in outs}
        moe = (layer % 2 == 1)
        NTOK = SEQ // 4
        nc = _prog(("ffn", moe), lambda: build_ffn(NTOK, NEXP if moe else 0))
        base = ffn_inputs(layer, inp, moe)
        maps = []
        for c in range(NCORE):
            b, q = c // 4, c % 4
            ts = slice(q * NTOK, (q + 1) * NTOK)
            d = dict(base)
            d["xT"] = np.ascontiguousarray(xT[b][:, ts])
            for i, name in enumerate(("ssd", "rwkv", "mla")):
                d["m%d" % i] = np.ascontiguousarray(mT[name][b][:, ts])
            maps.append(d)
        r = _launch(nc, maps)
        xT = [np.ascontiguousarray(np.concatenate([r[b * 4 + q]["yT"] for q in range(4)], axis=1)) for b in range(NB)]
        print("[kernel] layer %d ffn done (total %.1fs)" % (layer, time.time() - t00), flush=True)
    out = np.stack([np.ascontiguousarray(xT[b].T) for b in range(NB)], axis=0).astype(np.float32)
    return out
```

```python
import numpy as np
import concourse.bass as bass
import concourse.mybir as mybir
from concourse.bass_utils import run_bass_kernel_spmd

F32 = mybir.dt.float32; BF16 = mybir.dt.bfloat16; I32 = mybir.dt.int32
AF = mybir.ActivationFunctionType; ALU = mybir.AluOpType; AX = mybir.AxisListType

D = 2048; DC = 16; DFF = 5632; FC = 44; NEXP = 8
ALPHA = 4 ** 0.25
LN_EPS = 1e-5


class Res:
    __slots__ = ("lw", "rs")
    def __init__(self):
        self.lw = None
        self.rs = []


class MK:
    ENG = ("pe", "act", "dve", "pool", "sp")
    def __init__(self, nc):
        self.nc = nc
        self.h = {"pe": nc.tensor, "act": nc.scalar, "dve": nc.vector, "pool": nc.gpsimd, "sp": nc.sync}
        self.sems = {}; self.cnt = {}
        self.seen = {e: {} for e in self.ENG}
        self.res = {}; self.stack = []; self.tstack = []; self.psum_keys = set()
        self.nins = {e: 0 for e in self.ENG}; self.nwait = 0
        for e in self.ENG:
            self._sem(("eng", e))
    def _sem(self, key):
        if key not in self.sems:
            cm = self.nc.semaphore("s_" + "_".join(str(k) for k in key))
            self.sems[key] = cm.__enter__(); self.stack.append(cm); self.cnt[key] = 0
        return self.sems[key]
    def sb(self, name, shape, dt=F32):
        cm = self.nc.sbuf_tensor(getattr(self, "pfx", "") + name, list(shape), dt); t = cm.__enter__(); self.tstack.append(cm); return t
    def ps(self, name, shape, dt=F32):
        cm = self.nc.psum_tensor(name, list(shape), dt); t = cm.__enter__(); self.tstack.append(cm); self.psum_keys.add(name); return t
    def mark(self):
        return len(self.tstack)
    def release(self, mark):
        self.barrier()
        while len(self.tstack) > mark:
            self.tstack.pop().__exit__(None, None, None)
    def barrier(self):
        for e in self.ENG:
            self.finish(e)
    def R(self, key):
        r = self.res.get(key)
        if r is None:
            r = self.res[key] = Res()
        return r
    def _deps(self, eng, reads, writes):
        deps = {}
        def add(tok):
            if tok is None: return
            k, v = tok
            if eng == "pe" and k == ("eng", "pe"): return
            if deps.get(k, 0) < v: deps[k] = v
        for r in reads: add(self.R(r).lw)
        for w in writes:
            rr = self.R(w); add(rr.lw)
            for t in rr.rs: add(t)
        seen = self.seen[eng]
        for k, v in deps.items():
            if seen.get(k, 0) >= v: continue
            self.h[eng].wait_ge(self.sems[k], v); seen[k] = v; self.nwait += 1
    def _commit(self, tok, reads, writes):
        for r in reads:
            rr = self.R(r); rr.rs.append(tok)
            if len(rr.rs) > 64:
                mx = {}
                for k, v in rr.rs:
                    if mx.get(k, 0) < v: mx[k] = v
                rr.rs = list(mx.items())
        for w in writes:
            rr = self.R(w); rr.lw = tok; rr.rs = []
    def op(self, eng, fn, reads=(), writes=()):
        px = [r for r in reads if r in self.psum_keys]
        if px:
            writes = list(writes) + px
        self._deps(eng, reads, writes)
        ins = fn(self.h[eng])
        k = ("eng", eng); self.cnt[k] += 1
        ins.then_inc(self.sems[k], 1)
        self.nins[eng] += 1
        self._commit((k, self.cnt[k]), reads, writes)
        return ins
    def dma(self, eng, out, in_, reads=(), writes=(), semkey=None, **kw):
        k = ("dma", semkey)
        self._sem(k)
        prev = (k, self.cnt[k]) if self.cnt[k] else None
        self._deps(eng, reads, writes)
        if prev is not None and self.seen[eng].get(k, 0) < prev[1]:
            self.h[eng].wait_ge(self.sems[k], prev[1]); self.seen[eng][k] = prev[1]; self.nwait += 1
        ins = self.h[eng].dma_start(out=out, in_=in_, **kw)
        self.cnt[k] += 16
        ins.then_inc(self.sems[k], 16)
        self._commit((k, self.cnt[k]), reads, writes)
        return ins
    def finish(self, eng="sp"):
        for k, v in self.cnt.items():
            if v and self.seen[eng].get(k, 0) < v:
                self.h[eng].wait_ge(self.sems[k], v); self.seen[eng][k] = v
    def close(self):
        for cm in reversed(self.tstack):
            cm.__exit__(None, None, None)
        self.tstack = []
        for cm in reversed(self.stack):
            cm.__exit__(None, None, None)
        self.stack = []


def lay_w(w):
    K, N = w.shape
    return np.ascontiguousarray(w.reshape(K // 128, 128, N // 128, 128).transpose(2, 1, 0, 3))


def lay_vec(v):
    return np.ascontiguousarray(v.reshape(-1, 128).T)


def build_ffn(NT, n_exp, TB=512):
    moe = n_exp > 0
    NE = max(n_exp, 1)
    nc = bass.Bass("TRN2", target_bir_lowering=False)
    m = MK(nc)
    dr = lambda name, shape, kind="ExternalInput", dt=F32: nc.dram_tensor(name, list(shape), dt, kind=kind).ap()
    xT = dr("xT", [D, NT]); mTs = [dr("m%d" % i, [D, NT]) for i in range(3)]
    wout = dr("wout", [DC, 128, DC, 128])
    w1 = dr("w1", [NE, FC, 128, DC, 128]); w3 = dr("w3", [NE, FC, 128, DC, 128]); w2 = dr("w2", [NE, DC, 128, FC, 128])
    lnp = dr("lnp", [128, 4, DC])
    ident_d = dr("ident", [128, 128])
    if moe:
        router_d = dr("router", [128, DC, NEXP]); sel_d = dr("sel", [NEXP, NEXP, 128])
    yT = dr("yT", [D, NT], kind="ExternalOutput")
    xT_v = xT.rearrange("(c p) t -> p c t", p=128); yT_v = yT.rearrange("(c p) t -> p c t", p=128)
    mT_v = [a.rearrange("(c p) t -> p c t", p=128) for a in mTs]
    NSUB = TB // 128

    xs = m.sb("xs", [128, DC, TB]); x1 = m.sb("x1", [128, DC, TB]); x1b = m.sb("x1b", [128, DC, TB], BF16)
    acc = m.sb("acc", [128, DC, TB])
    big = m.sb("big", [128, 48, TB], BF16)
    wa = [m.sb("wa%d" % i, [128, DC, 128], BF16) for i in range(2)]
    wb = [m.sb("wb%d" % i, [128, DC, 128], BF16) for i in range(2)]
    HF = FC // 4
    wc = [m.sb("wc%d" % i, [128, HF, 128], BF16) for i in range(2)]
    lnp_s = m.sb("lnp_s", [128, 4, DC]); ones = m.sb("ones", [128, 128]); ident = m.sb("ident_s", [128, 128])
    st_mean = m.sb("st_mean", [128, TB]); st_rstd = m.sb("st_rstd", [128, TB]); st_tmp = m.sb("st_tmp", [128, TB])
    sq0 = m.sb("sq0", [128, TB]); sq = [sq0, sq0]
    sil0 = m.sb("sil0", [128, TB]); sil = [sil0, sil0]
    if moe:
        router = m.sb("router_s", [128, DC, NEXP]); sel = m.sb("sel_s", [NEXP, NEXP, 128])
        gB = m.sb("gB", [128, NEXP, TB], BF16); gT = m.sb("gT", [NEXP, TB])
        L = m.sb("L", [128, NEXP]); L2 = m.sb("L2", [128, NEXP]); eq1 = m.sb("eq1", [128, NEXP]); eq2 = m.sb("eq2", [128, NEXP])
        gg = m.sb("gg", [128, NEXP]); sm = m.sb("sm", [128, 8])
    pz = [m.ps("pz%d" % i, [128, TB]) for i in range(2)]
    pst = [m.ps("pst%d" % i, [128, TB]) for i in range(2)]
    ph1 = [m.ps("ph1_%d" % i, [128, TB]) for i in range(2)]
    ph3 = [m.ps("ph3_%d" % i, [128, TB]) for i in range(2)]

    m.dma("sp", lnp_s[:], lnp[:, :, :], writes=["lnp"], semkey="c0")
    m.dma("sp", ident[:], ident_d[:, :], writes=["ident"], semkey="c1")
    m.op("dve", lambda e: e.memset(ones[:], 1.0), writes=["ones"])
    if moe:
        m.dma("sp", router[:], router_d[:, :, :], writes=["router"], semkey="c2")
        m.dma("sp", sel[:], sel_d[:, :, :], writes=["sel"], semkey="c3")

    def layernorm(src, sk, which, dst32, dk, dstb, dbk):
        for c in range(DC):
            m.op("pe", lambda e, c=c: e.matmul(pst[0][:], ones[:], src[:, c, :], start=(c == 0), stop=(c == DC - 1)),
                 reads=["ones", (sk, c)], writes=["pst0"])
        for c in range(DC):
            s = sq[c % 2]
            m.op("act", lambda e, c=c, s=s: e.activation(out=s[:], in_=src[:, c, :], func=AF.Square),
                 reads=[(sk, c)], writes=["sq0"])
            m.op("pe", lambda e, c=c, s=s: e.matmul(pst[1][:], ones[:], s[:], start=(c == 0), stop=(c == DC - 1)),
                 reads=["ones", "sq0"], writes=["pst1"])
        m.op("dve", lambda e: e.tensor_scalar(st_mean[:], pst[0][:], 1.0 / D, None, ALU.mult), reads=["pst0"], writes=["st_mean"])
        m.op("dve", lambda e: e.tensor_tensor(st_tmp[:], st_mean[:], st_mean[:], ALU.mult), reads=["st_mean"], writes=["st_tmp"])
        m.op("dve", lambda e: e.scalar_tensor_tensor(st_rstd[:], pst[1][:], 1.0 / D, st_tmp[:], ALU.mult, ALU.subtract),
             reads=["pst1", "st_tmp"], writes=["st_rstd"])
        m.op("dve", lambda e: e.tensor_scalar(st_rstd[:], st_rstd[:], LN_EPS, None, ALU.add), reads=["st_rstd"], writes=["st_rstd"])
        m.op("act", lambda e: e.activation(out=st_tmp[:], in_=st_rstd[:], func=AF.Sqrt), reads=["st_rstd"], writes=["st_tmp"])
        m.op("dve", lambda e: e.reciprocal(st_rstd[:], st_tmp[:]), reads=["st_tmp"], writes=["st_rstd"])
        for c in range(DC):
            eng = "dve" if c % 2 == 0 else "pool"
            m.op(eng, lambda e, c=c: e.tensor_tensor(src[:, c, :], src[:, c, :], st_mean[:], ALU.subtract),
                 reads=[(sk, c), "st_mean"], writes=[(sk, c)])
            m.op(eng, lambda e, c=c: e.tensor_tensor(src[:, c, :], src[:, c, :], st_rstd[:], ALU.mult),
                 reads=[(sk, c), "st_rstd"], writes=[(sk, c)])
            m.op("act", lambda e, c=c: e.activation(out=dst32[:, c, :], in_=src[:, c, :], func=AF.Identity,
                                                    scale=lnp_s[:, 2 * which, c:c + 1], bias=lnp_s[:, 2 * which + 1, c:c + 1]),
                 reads=[(sk, c), "lnp"], writes=[(dk, c)])
            if dstb is not None:
                m.op("act", lambda e, c=c: e.activation(out=dstb[:, c, :], in_=src[:, c, :], func=AF.Identity,
                                                        scale=lnp_s[:, 2 * which, c:c + 1], bias=lnp_s[:, 2 * which + 1, c:c + 1]),
                     reads=[(sk, c), "lnp"], writes=[(dbk, c)])

    wcnt = {"a": 0, "b": 0, "c": 0}
    def wload(kind, tiles, src):
        i = wcnt[kind] % 2; wcnt[kind] += 1
        key = "w%s%d" % (kind, i)
        m.dma("pool", tiles[i][:], src, writes=[key], semkey=key)
        return tiles[i], key

    for mt in range(NT // TB):
        ts = slice(mt * TB, (mt + 1) * TB)
        m.dma("sp", xs[:], xT_v[:, :, ts], writes=[("xs", c) for c in range(DC)], semkey="xs")
        for b in range(3):
            m.dma("pool", big[:, b * DC:(b + 1) * DC, :], mT_v[b][:, :, ts],
                  writes=[("big", b * DC + i) for i in range(DC)], semkey="bigm%d" % b)
        for c in range(DC):
            wt, wk = wload("a", wa, wout[c])
            p = pz[c % 2]; pk = "pz%d" % (c % 2)
            for n in range(48):
                m.op("pe", lambda e, p=p, wt=wt, n=n: e.matmul(p[:], wt[:, n % DC, :], big[:, n, :], start=(n == 0), stop=(n == 47)),
                     reads=[wk, ("big", n)], writes=[pk])
            m.op("dve", lambda e, p=p, c=c: e.scalar_tensor_tensor(xs[:, c, :], xs[:, c, :], ALPHA, p[:], ALU.mult, ALU.add),
                 reads=[pk, ("xs", c)], writes=[("xs", c)])
        layernorm(xs, "xs", 0, x1, "x1", x1b, "x1b")
        if moe:
            for sub in range(NSUB):
                tsub = slice(sub * 128, (sub + 1) * 128)
                for c in range(DC):
                    m.op("pe", lambda e, c=c, tsub=tsub: e.matmul(pst[0][:, 0:NEXP], x1[:, c, tsub], router[:, c, :], start=(c == 0), stop=(c == DC - 1)),
                         reads=[("x1", c), "router"], writes=["pst0"])
                m.op("dve", lambda e: e.tensor_copy(L[:], pst[0][:, 0:NEXP]), reads=["pst0"], writes=["L"])
                m.op("dve", lambda e: e.tensor_reduce(sm[:, 0:1], L[:], AX.X, ALU.max), reads=["L"], writes=["sm"])
                m.op("dve", lambda e: e.tensor_scalar(eq1[:], L[:], sm[:, 0:1], None, ALU.is_equal), reads=["L", "sm"], writes=["eq1"])
                m.op("dve", lambda e: e.scalar_tensor_tensor(L2[:], eq1[:], -1e30, L[:], ALU.mult, ALU.add), reads=["eq1", "L"], writes=["L2"])
                m.op("dve", lambda e: e.tensor_reduce(sm[:, 1:2], L2[:], AX.X, ALU.max), reads=["L2", "sm"], writes=["sm"])
                m.op("dve", lambda e: e.tensor_scalar(eq2[:], L2[:], sm[:, 1:2], None, ALU.is_equal), reads=["L2", "sm"], writes=["eq2"])
                m.op("dve", lambda e: e.tensor_tensor(sm[:, 2:3], sm[:, 1:2], sm[:, 0:1], ALU.subtract), reads=["sm"], writes=["sm"])
                m.op("act", lambda e: e.activation(out=sm[:, 3:4], in_=sm[:, 2:3], func=AF.Exp), reads=["sm"], writes=["sm"])
                m.op("dve", lambda e: e.tensor_scalar(sm[:, 4:5], sm[:, 3:4], 1.0, None, ALU.add), reads=["sm"], writes=["sm"])
                m.op("dve", lambda e: e.reciprocal(sm[:, 5:6], sm[:, 4:5]), reads=["sm"], writes=["sm"])
                m.op("dve", lambda e: e.tensor_tensor(sm[:, 6:7], sm[:, 3:4], sm[:, 5:6], ALU.mult), reads=["sm"], writes=["sm"])
                m.op("dve", lambda e: e.tensor_scalar(gg[:], eq1[:], sm[:, 5:6], None, ALU.mult), reads=["eq1", "sm"], writes=["gg"])
                m.op("dve", lambda e: e.scalar_tensor_tensor(gg[:], eq2[:], sm[:, 6:7], gg[:], ALU.mult, ALU.add), reads=["eq2", "sm", "gg"], writes=["gg"])
                m.op("pe", lambda e: e.transpose(pst[1][0:NEXP, 0:128], gg[:], ident[:]), reads=["gg", "ident"], writes=["pst1"])
                m.op("dve", lambda e, tsub=tsub: e.tensor_copy(gT[:, tsub], pst[1][0:NEXP, 0:128]), reads=["pst1"], writes=["gT"])
            for ex in range(NEXP):
                p = ph1[ex % 2]; pk = "ph1_%d" % (ex % 2)
                m.op("pe", lambda e, p=p, ex=ex: e.matmul(p[:], sel[:, ex, :], gT[:], start=True, stop=True), reads=["sel", "gT"], writes=[pk])
                m.op("act", lambda e, p=p, ex=ex: e.copy(gB[:, ex, :], p[:]), reads=[pk], writes=[("gB", ex)])
        for ex in range(NE):
            for f in range(FC):
                w1t, w1k = wload("a", wa, w1[ex, f]); w3t, w3k = wload("b", wb, w3[ex, f])
                p1 = ph1[f % 2]; p1k = "ph1_%d" % (f % 2); p3 = ph3[f % 2]; p3k = "ph3_%d" % (f % 2)
                for c in range(DC):
                    m.op("pe", lambda e, c=c, p1=p1, w1t=w1t: e.matmul(p1[:], w1t[:, c, :], x1b[:, c, :], start=(c == 0), stop=(c == DC - 1)),
                         reads=[w1k, ("x1b", c)], writes=[p1k])
                for c in range(DC):
                    m.op("pe", lambda e, c=c, p3=p3, w3t=w3t: e.matmul(p3[:], w3t[:, c, :], x1b[:, c, :], start=(c == 0), stop=(c == DC - 1)),
                         reads=[w3k, ("x1b", c)], writes=[p3k])
                sl = sil[f % 2]; slk = "sil0"
                m.op("act", lambda e, sl=sl, p1=p1: e.activation(out=sl[:], in_=p1[:], func=AF.Silu), reads=[p1k], writes=[slk])
                if moe:
                    m.op("pool", lambda e, sl=sl, ex=ex: e.tensor_tensor(sl[:], sl[:], gB[:, ex, :], ALU.mult), reads=[slk, ("gB", ex)], writes=[slk])
                m.op("dve", lambda e, sl=sl, p3=p3, f=f: e.tensor_tensor(big[:, f, :], sl[:], p3[:], ALU.mult), reads=[slk, p3k], writes=[("big", f)])
            for c in range(DC):
                p = pz[c % 2]; pk = "pz%d" % (c % 2)
                for half in range(4):
                    wt, wk = wload("c", wc, w2[ex, c][:, half * HF:(half + 1) * HF, :])
                    for j in range(HF):
                        f = half * HF + j
                        m.op("pe", lambda e, p=p, wt=wt, j=j, f=f: e.matmul(p[:], wt[:, j, :], big[:, f, :], start=(f == 0), stop=(f == FC - 1)),
                             reads=[wk, ("big", f)], writes=[pk])
                if ex == 0:
                    m.op("dve", lambda e, p=p, c=c: e.scalar_tensor_tensor(acc[:, c, :], x1[:, c, :], ALPHA, p[:], ALU.mult, ALU.add),
                         reads=[pk, ("x1", c)], writes=[("acc", c)])
                else:
                    m.op("dve", lambda e, p=p, c=c: e.tensor_tensor(acc[:, c, :], acc[:, c, :], p[:], ALU.add),
                         reads=[pk, ("acc", c)], writes=[("acc", c)])
        layernorm(acc, "acc", 1, xs, "xs", None, None)
        m.dma("sp", yT_v[:, :, ts], xs[:], reads=[("xs", c) for c in range(DC)], semkey="xs")
    m.finish("sp")
    return nc, m


def ffn_inputs(layer, inp, moe):
    d = {}
    d["wout"] = lay_w(inp["w_out"][layer])
    if not moe:
        d["w1"] = lay_w(inp["ffn_w1"][layer // 2])[None]; d["w3"] = lay_w(inp["ffn_w3"][layer // 2])[None]
        d["w2"] = lay_w(inp["ffn_w2"][layer // 2])[None]
    else:
        d["w1"] = np.stack([lay_w(inp["moe_w1"][layer // 2][e]) for e in range(NEXP)])
        d["w3"] = np.stack([lay_w(inp["moe_w3"][layer // 2][e]) for e in range(NEXP)])
        d["w2"] = np.stack([lay_w(inp["moe_w2"][layer // 2][e]) for e in range(NEXP)])
        d["router"] = np.ascontiguousarray(inp["moe_router"][layer // 2].reshape(DC, 128, NEXP).transpose(1, 0, 2))
        sel = np.zeros((NEXP, NEXP, 128), np.float32)
        for e in range(NEXP): sel[e, e, :] = 1.0
        d["sel"] = sel
    d["lnp"] = np.ascontiguousarray(np.stack([lay_vec(inp["ln1_w"][layer]), lay_vec(inp["ln1_b"][layer]),
                                              lay_vec(inp["ln2_w"][layer]), lay_vec(inp["ln2_b"][layer])], axis=1))
    d["ident"] = np.eye(128, dtype=np.float32)
    return d


HM = 4
SC_ATT = 192 ** -0.5
TWO_PI = 2 * np.pi
C1 = 6.28125
C2 = float(TWO_PI - C1)
RMS_EPS = 1e-6


def build_mla(S, TB=512, ctx=None):
    if ctx is None:
        nc = bass.Bass("TRN2", target_bir_lowering=False); m = MK(nc); pfx = ""
    else:
        nc, m, pfx = ctx["nc"], ctx["m"], ctx["pfx"]
    m.pfx = pfx
    mark0 = m.mark()
    dr = lambda name, shape, kind="ExternalInput", dt=F32: nc.dram_tensor(pfx + name, list(shape), dt, kind=kind).ap()
    xT = ctx["xT"] if ctx else dr("xT", [D, S])
    wl_d = dr("wl", [10, 128, DC, 128]); wg_d = dr("wg", [128, DC, 512])
    wq_d = dr("wq", [HM, 128, 4, 256]); wk_d = dr("wk", [HM, 128, 4, 128]); wv_d = dr("wv", [128, 4, 512])
    nw_d = dr("nw", [128, 2, 4]); pos_d = dr("pos", [1, S], dt=I32); rc_d = dr("rc", [64, 2]); cm_d = dr("cmask", [128, 4, 512])
    om = dr("om", [S, 512], kind="ExternalOutput")
    qT = dr("qT_s", [HM, 192, S], kind="Internal", dt=BF16); kT = dr("kT_s", [HM, 128, S], kind="Internal", dt=BF16)
    krT = dr("krT_s", [64, S], kind="Internal", dt=BF16); Vd = dr("V_s", [HM, S, 128], kind="Internal", dt=BF16)
    gate_d = dr("gate_s", [S, 512], kind="Internal")
    xT_v = xT.rearrange("(c p) t -> p c t", p=128)
    NSUB = TB // 128; NMT = S // TB

    pb = ctx["pb"] if ctx else [m.ps("pb%d" % i, [128, 512]) for i in range(8)]
    pcnt = [0]
    def nextp():
        i = pcnt[0] % 8; pcnt[0] += 1
        return pb[i], "pb%d" % i
    ones = m.sb("ones", [128, 128]); rc = m.sb("rc_s", [64, 2]); nw = m.sb("nw_s", [128, 2, 4])
    m.op("dve", lambda e: e.memset(ones[:], 1.0), writes=["ones"])
    m.dma("sp", rc[:], rc_d[:, :], writes=["rc"], semkey="c0")
    m.dma("sp", nw[:], nw_d[:, :, :], writes=["nw"], semkey="c1")

    mk1 = m.mark()
    wl = m.sb("wl_s", [128, 10, DC, 128], BF16); wg = m.sb("wg_s", [128, DC, 512], BF16)
    wq = m.sb("wq_s", [128, HM, 4, 256], BF16); wk = m.sb("wk_s", [128, HM, 4, 128], BF16); wv = m.sb("wv_s", [128, 4, 512], BF16)
    for j in range(10):
        m.dma("pool", wl[:, j, :, :], wl_d[j], writes=["wl"], semkey="wl")
    m.dma("pool", wg[:], wg_d[:, :, :], writes=["wg"], semkey="wg")
    for h in range(HM):
        m.dma("pool", wq[:, h, :, :], wq_d[h], writes=["wq"], semkey="wq")
        m.dma("pool", wk[:, h, :, :], wk_d[h], writes=["wk"], semkey="wk")
    m.dma("pool", wv[:], wv_d[:, :, :], writes=["wv"], semkey="wv")
    xb = [m.sb("xb%d" % i, [128, DC, TB], BF16) for i in range(2)]
    posi = m.sb("posi", [64, TB], I32); ang = m.sb("ang", [64, TB]); ki = m.sb("ki", [64, TB], I32); kf = m.sb("kf", [64, TB])
    rr = m.sb("rr", [64, TB]); cos2 = m.sb("cos2", [64, TB]); sinS = m.sb("sinS", [64, TB]); cos2s = m.sb("cos2s", [64, TB]); sinSs = m.sb("sinSs", [64, TB])
    lat = m.sb("lat", [128, 8, TB]); sqt = m.sb("sqt", [128, TB]); rstd = m.sb("rstd", [128, 2, TB]); stt_ = m.sb("stt_", [128, TB])
    latn = m.sb("latn", [128, 8, TB], BF16)
    ev = [m.sb("ev%d" % i, [128, TB], BF16) for i in range(2)]
    evr = [m.sb("evr%d" % i, [64, TB], BF16) for i in range(2)]
    t1 = m.sb("t1", [64, TB]); t2 = m.sb("t2", [64, TB])
    gst = [m.sb("gst%d" % i, [128, 512]) for i in range(2)]
    ecnt = {"ev": 0, "evr": 0, "gst": 0}
    def nxt(kind, tiles):
        i = ecnt[kind] % 2; ecnt[kind] += 1
        return tiles[i], "%s%d" % (kind, i)

    def load_x(mt):
        i = mt % 2
        m.dma("pool", xb[i][:], xT_v[:, :, mt * TB:(mt + 1) * TB], writes=["xb%d" % i], semkey="xb%d" % i)
    load_x(0)
    for mt in range(NMT):
        ts = slice(mt * TB, (mt + 1) * TB)
        if mt + 1 < NMT: load_x(mt + 1)
        x = xb[mt % 2]; xk = "xb%d" % (mt % 2)
        m.dma("sp", posi[:], pos_d[0:1, ts].partition_broadcast(64), writes=["posi"], semkey="posi")
        m.op("dve", lambda e: e.tensor_copy(ang[:], posi[:]), reads=["posi"], writes=["ang"])
        m.op("dve", lambda e: e.tensor_scalar(ang[:], ang[:], rc[:, 0:1], None, ALU.mult), reads=["ang", "rc"], writes=["ang"])
        for which in range(2):
            dst = sinS if which == 0 else cos2; dk = "sinS" if which == 0 else "cos2"
            m.op("dve", lambda e, which=which: e.tensor_scalar(ki[:], ang[:], 1.0 / TWO_PI, 0.25 * which, ALU.mult, ALU.add), reads=["ang"], writes=["ki"])
            m.op("dve", lambda e: e.tensor_copy(kf[:], ki[:]), reads=["ki"], writes=["kf"])
            m.op("dve", lambda e: e.scalar_tensor_tensor(rr[:], kf[:], -C1, ang[:], ALU.mult, ALU.add), reads=["kf", "ang"], writes=["rr"])
            if which == 1:
                m.op("dve", lambda e: e.tensor_scalar(rr[:], rr[:], float(np.pi / 2), None, ALU.add), reads=["rr"], writes=["rr"])
            m.op("dve", lambda e: e.scalar_tensor_tensor(rr[:], kf[:], -C2, rr[:], ALU.mult, ALU.add), reads=["kf", "rr"], writes=["rr"])
            m.op("dve", lambda e: e.tensor_scalar(rr[:], rr[:], float(np.pi), float(-np.pi), ALU.min, ALU.max), reads=["rr"], writes=["rr"])
            if which == 0:
                m.op("act", lambda e, dst=dst: e.activation(out=dst[:], in_=rr[:], func=AF.Sin, scale=rc[:, 1:2]), reads=["rr", "rc"], writes=[dk])
            else:
                m.op("act", lambda e, dst=dst: e.activation(out=dst[:], in_=rr[:], func=AF.Sin), reads=["rr"], writes=[dk])
        m.op("pool", lambda e: e.tensor_scalar(cos2s[:], cos2[:], SC_ATT, None, ALU.mult), reads=["cos2"], writes=["cos2s"])
        m.op("pool", lambda e: e.tensor_scalar(sinSs[:], sinS[:], SC_ATT, None, ALU.mult), reads=["sinS"], writes=["sinSs"])
        for grp in range(2):
            pss, pssk = nextp()
            for j in range(4):
                jj = grp * 4 + j
                p, pk = nextp()
                for c in range(DC):
                    m.op("pe", lambda e, p=p, jj=jj, c=c: e.matmul(p[:], wl[:, jj, c, :], x[:, c, :], start=(c == 0), stop=(c == DC - 1)),
                         reads=["wl", xk], writes=[pk])
                m.op("act", lambda e, p=p, jj=jj: e.copy(lat[:, jj, :], p[:]), reads=[pk], writes=[("lat", jj)])
                m.op("act", lambda e, jj=jj: e.activation(out=sqt[:], in_=lat[:, jj, :], func=AF.Square), reads=[("lat", jj)], writes=["sqt"])
                m.op("pe", lambda e, pss=pss, j=j: e.matmul(pss[:], ones[:], sqt[:], start=(j == 0), stop=(j == 3)), reads=["ones", "sqt"], writes=[pssk])
            m.op("dve", lambda e, pss=pss, grp=grp: e.tensor_scalar(rstd[:, grp, :], pss[:], 1.0 / 512, RMS_EPS, ALU.mult, ALU.add), reads=[pssk], writes=[("rstd", grp)])
            m.op("act", lambda e, grp=grp: e.activation(out=stt_[:], in_=rstd[:, grp, :], func=AF.Sqrt), reads=[("rstd", grp)], writes=["stt_"])
            m.op("dve", lambda e, grp=grp: e.reciprocal(rstd[:, grp, :], stt_[:]), reads=["stt_"], writes=[("rstd", grp)])
            for j in range(4):
                jj = grp * 4 + j
                m.op("dve", lambda e, jj=jj, grp=grp, j=j: e.scalar_tensor_tensor(latn[:, jj, :], lat[:, jj, :], nw[:, grp, j:j + 1], rstd[:, grp, :], ALU.mult, ALU.mult),
                     reads=[("lat", jj), "nw", ("rstd", grp)], writes=[("latn", jj)])
        def rope(wsel_pe, wsel_rot, rhs_fn, nk, rkeys, ctab, stab, ck, sk, dst_ap):
            pA, pAk = nextp(); pB, pBk = nextp()
            for c in range(nk):
                m.op("pe", lambda e, c=c: e.matmul(pA[0:64, :], wsel_pe(c), rhs_fn(c), start=(c == 0), stop=(c == nk - 1)), reads=rkeys, writes=[pAk])
            for c in range(nk):
                m.op("pe", lambda e, c=c: e.matmul(pB[0:64, :], wsel_rot(c), rhs_fn(c), start=(c == 0), stop=(c == nk - 1)), reads=rkeys, writes=[pBk])
            m.op("dve", lambda e: e.tensor_tensor(t1[:], pA[0:64, :], ctab[:], ALU.mult), reads=[pAk, ck], writes=["t1"])
            m.op("dve", lambda e: e.tensor_tensor(t2[:], pB[0:64, :], stab[:], ALU.mult), reads=[pBk, sk], writes=["t2"])
            o, ok = nxt("evr", evr)
            m.op("dve", lambda e: e.tensor_tensor(o[:], t1[:], t2[:], ALU.add), reads=["t1", "t2"], writes=[ok])
            m.dma("sp", dst_ap, o[:], reads=[ok], semkey=ok)
        rope(lambda c: wl[:, 8, c, 0:64], lambda c: wl[:, 9, c, 0:64], lambda c: x[:, c, :], DC, ["wl", xk], cos2, sinS, "cos2", "sinS", krT[:, ts])
        for h in range(HM):
            p, pk = nextp()
            for c in range(4):
                m.op("pe", lambda e, p=p, h=h, c=c: e.matmul(p[:], wq[:, h, c, 0:128], latn[:, c, :], start=(c == 0), stop=(c == 3)),
                     reads=["wq", ("latn", c)], writes=[pk])
            o, ok = nxt("ev", ev)
            m.op("act", lambda e, o=o, p=p: e.activation(out=o[:], in_=p[:], func=AF.Copy, scale=SC_ATT), reads=[pk], writes=[ok])
            m.dma("sp", qT[h, 0:128, ts], o[:], reads=[ok], semkey=ok)
            rope(lambda c, h=h: wq[:, h, c, 128:192], lambda c, h=h: wq[:, h, c, 192:256], lambda c: latn[:, c, :], 4,
                 ["wq"] + [("latn", c) for c in range(4)], cos2s, sinSs, "cos2s", "sinSs", qT[h, 128:192, ts])
            p, pk = nextp()
            for c in range(4):
                m.op("pe", lambda e, p=p, h=h, c=c: e.matmul(p[:], wk[:, h, c, :], latn[:, 4 + c, :], start=(c == 0), stop=(c == 3)),
                     reads=["wk", ("latn", 4 + c)], writes=[pk])
            o, ok = nxt("ev", ev)
            m.op("act", lambda e, o=o, p=p: e.copy(o[:], p[:]), reads=[pk], writes=[ok])
            m.dma("sp", kT[h, :, ts], o[:], reads=[ok], semkey=ok)
        for sub in range(NSUB):
            tsub = slice(sub * 128, (sub + 1) * 128); tg = slice(mt * TB + sub * 128, mt * TB + (sub + 1) * 128)
            p, pk = nextp()
            for c in range(4):
                m.op("pe", lambda e, p=p, c=c, tsub=tsub: e.matmul(p[:], latn[:, 4 + c, tsub], wv[:, c, :], start=(c == 0), stop=(c == 3)),
                     reads=["wv", ("latn", 4 + c)], writes=[pk])
            o, ok = nxt("ev", ev)
            m.op("act", lambda e, o=o, p=p: e.copy(o[:], p[:]), reads=[pk], writes=[ok])
            m.dma("sp", Vd[:, tg, :].rearrange("h t v -> t h v"), o[:].rearrange("p (h v) -> p h v", h=HM), reads=[ok], semkey=ok)
            p, pk = nextp()
            for c in range(DC):
                m.op("pe", lambda e, p=p, c=c, tsub=tsub: e.matmul(p[:], x[:, c, tsub], wg[:, c, :], start=(c == 0), stop=(c == DC - 1)),
                     reads=["wg", xk], writes=[pk])
            o, ok = nxt("gst", gst)
            m.op("act", lambda e, o=o, p=p: e.activation(out=o[:], in_=p[:], func=AF.Sigmoid), reads=[pk], writes=[ok])
            m.dma("sp", gate_d[tg, :], o[:], reads=[ok], semkey=ok)
    m.release(mk1)
    NKC = S // 128
    Kn = m.sb("Kn", [128, S], BF16); Kr = m.sb("Kr", [64, S], BF16); Va = m.sb("Va", [128, NKC, 129], BF16)
    cmask = m.sb("cmask_s", [128, 4, 512], BF16)
    qn = [m.sb("qn%d" % i, [128, TB], BF16) for i in range(2)]; qr = [m.sb("qr%d" % i, [64, TB], BF16) for i in range(2)]
    P = [m.sb("P%d" % i, [128, TB], BF16) for i in range(3)]
    gt = [m.sb("gt%d" % i, [128, 4, 128]) for i in range(2)]; ost = [m.sb("ost%d" % i, [128, 4, 128]) for i in range(2)]
    rden = m.sb("rden", [128, 4])
    m.dma("pool", cmask[:], cm_d[:, :, :], writes=["cmask"], semkey="cmask")
    m.op("dve", lambda e: e.memset(Va[:, :, 128:129], 1.0), writes=["Va_ones"])
    m.dma("sp", Kr[:], krT[:, :], writes=["Kr"], semkey="Kr")
    it = 0; pc = 0
    for h in range(HM):
        m.dma("sp", Kn[:], kT[h], writes=["Kn"], semkey="Kn")
        for k0 in range(0, NKC, 16):
            k1 = min(NKC, k0 + 16)
            m.dma("sp", Va[:, k0:k1, 0:128], Vd[h, k0 * 128:k1 * 128, :].rearrange("(k p) v -> p k v", p=128), writes=["Va"], semkey="Va")
        for qm in range(NMT):
            ts = slice(qm * TB, (qm + 1) * TB)
            qi = it % 2; it += 1
            m.dma("sp", qn[qi][:], qT[h, 0:128, ts], writes=["qn%d" % qi], semkey="qn%d" % qi)
            m.dma("sp", qr[qi][:], qT[h, 128:192, ts], writes=["qr%d" % qi], semkey="qr%d" % qi)
            m.dma("sp", gt[qi][:], gate_d[ts, h * 128:(h + 1) * 128].rearrange("(q p) v -> p q v", p=128), writes=["gt%d" % qi], semkey="gt%d" % qi)
            nkc = 4 * qm + 4
            for kc in range(nkc):
                p = pb[kc % 2]; pk = "pb%d" % (kc % 2)
                ks = slice(kc * 128, (kc + 1) * 128)
                m.op("pe", lambda e, p=p, ks=ks, qi=qi: e.matmul(p[:], Kn[:, ks], qn[qi][:], start=True, stop=False), reads=["Kn", "qn%d" % qi], writes=[pk])
                m.op("pe", lambda e, p=p, ks=ks, qi=qi: e.matmul(p[:], Kr[:, ks], qr[qi][:], start=False, stop=True), reads=["Kr", "qr%d" % qi], writes=[pk])
                Pt = P[pc % 3]; Pk = "P%d" % (pc % 3); pc += 1
                m.op("act", lambda e, Pt=Pt, p=p: e.activation(out=Pt[:], in_=p[:], func=AF.Exp), reads=[pk], writes=[Pk])
                r = kc - 4 * qm
                if r >= 0:
                    m.op("pool", lambda e, Pt=Pt, r=r: e.tensor_tensor(Pt[:], Pt[:], cmask[:, r, :], ALU.mult), reads=[Pk, "cmask"], writes=[Pk])
                for qs in range(4):
                    if kc > 4 * qm + qs: continue
                    a = pb[2 + qs]; ak = "pb%d" % (2 + qs)
                    m.op("pe", lambda e, a=a, qs=qs, Pt=Pt, kc=kc, qm=qm: e.matmul(a[:, 0:129], Pt[:, qs * 128:(qs + 1) * 128], Va[:, kc, :],
                                                                             start=(kc == 0), stop=(kc == 4 * qm + qs)),
                         reads=[Pk, "Va", "Va_ones"], writes=[ak])
            o = ost[qi]; okk = "ost%d" % qi
            for qs in range(4):
                a = pb[2 + qs]; ak = "pb%d" % (2 + qs)
                m.op("dve", lambda e, a=a, qs=qs: e.reciprocal(rden[:, qs:qs + 1], a[:, 128:129]), reads=[ak], writes=[("rden", qs)])
                m.op("dve", lambda e, a=a, qs=qs, o=o, qi=qi: e.scalar_tensor_tensor(o[:, qs, :], a[:, 0:128], rden[:, qs:qs + 1], gt[qi][:, qs, :], ALU.mult, ALU.mult),
                     reads=[ak, ("rden", qs), "gt%d" % qi], writes=[(okk, qs)])
            m.dma("sp", om[ts, h * 128:(h + 1) * 128].rearrange("(q p) v -> p q v", p=128), o[:], reads=[(okk, qs) for qs in range(4)], semkey=okk)
    if ctx:
        m.release(mark0)
    else:
        m.finish("sp")
    return nc, m


def mla_inputs(layer, inp, b, g):
    GC = 3 * 2048; SSDC = 2048 + 3072 + 32
    o_mla = GC + SSDC
    W = inp["w_in"][layer]
    kpe = W[:, o_mla + 1024:o_mla + 1088]
    krot = np.concatenate([kpe[:, 32:64], kpe[:, 0:32]], axis=1)
    z64 = np.zeros((2048, 64), np.float32)
    wl = np.concatenate([W[:, o_mla:o_mla + 1024], kpe, z64, krot, z64], axis=1)
    d = {"wl": lay_w(wl)}
    gcol = 2 * 2048 + g * 512
    d["wg"] = np.ascontiguousarray(W[:, gcol:gcol + 512].reshape(DC, 128, 512).transpose(1, 0, 2))
    wqb = inp["mla_w_q_b"][layer].reshape(512, 16, 192)[:, g * 4:(g + 1) * 4, :]
    wq = np.concatenate([wqb[:, :, 0:192], wqb[:, :, 160:192], wqb[:, :, 128:160]], axis=2)
    d["wq"] = np.ascontiguousarray(wq.reshape(4, 128, 4, 256).transpose(2, 1, 0, 3))
    wkv = inp["mla_w_kv_b"][layer].reshape(512, 16, 256)[:, g * 4:(g + 1) * 4, :]
    d["wk"] = np.ascontiguousarray(wkv[:, :, 0:128].reshape(4, 128, 4, 128).transpose(2, 1, 0, 3))
    d["wv"] = np.ascontiguousarray(wkv[:, :, 128:256].reshape(4, 128, 512).transpose(1, 0, 2))
    d["nw"] = np.ascontiguousarray(np.stack([lay_vec(inp["mla_q_norm_w"][layer]), lay_vec(inp["mla_kv_norm_w"][layer])], axis=1))
    d["pos"] = np.ascontiguousarray(inp["positions"][b][None, :]).astype(np.int32)
    inv = (10000.0 ** (-np.arange(0, 64, 2, dtype=np.float32) / 64)).astype(np.float32)
    rc = np.zeros((64, 2), np.float32); rc[:, 0] = np.concatenate([inv, inv]); rc[:32, 1] = -1.0; rc[32:, 1] = 1.0
    d["rc"] = rc
    pp = np.arange(128)[:, None]; jj = np.arange(512)[None, :]
    d["cmask"] = np.ascontiguousarray(np.stack([((r * 128 + pp) <= jj).astype(np.float32) for r in range(4)], axis=1))
    return d


def build_ssd(S, TB=512, ctx=None):
    if ctx is None:
        nc = bass.Bass("TRN2", target_bir_lowering=False); m = MK(nc); pfx = ""
    else:
        nc, m, pfx = ctx["nc"], ctx["m"], ctx["pfx"]
    m.pfx = pfx
    mark0 = m.mark()
    dr = lambda name, shape, kind="ExternalInput", dt=F32: nc.dram_tensor(pfx + name, list(shape), dt, kind=kind).ap()
    xT = ctx["xT"] if ctx else dr("xT", [D, S])
    wfm_d = dr("wfm", [6, 128, DC, 128]); wtm_d = dr("wtm", [128, DC, 1032])
    cw_d = dr("cw", [128, 6, 4]); cb_d = dr("cb", [128, 6]); rows_d = dr("rows", [1, 24 + 1024])
    tri_d = dr("triU", [128, 128]); nm_d = dr("negmask", [128, 128]); id_d = dr("ident", [128, 128])
    om = dr("om", [S, 512], kind="ExternalOutput")
    xT_v = xT.rearrange("(c p) t -> p c t", p=128)
    NSUB = TB // 128; NMT = S // TB
    pb = ctx["pb"] if ctx else [m.ps("pb%d" % i, [128, 512]) for i in range(8)]
    pcnt = [0]
    def nextp():
        i = pcnt[0] % 8; pcnt[0] += 1
        return pb[i], "pb%d" % i
    ones = m.sb("ones", [128, 128]); ident = m.sb("ident_s", [128, 128]); triU = m.sb("triU_s", [128, 128]); negm = m.sb("negm_s", [128, 128])
    cw = m.sb("cw_s", [128, 6, 4]); cb = m.sb("cb_s", [128, 6]); rows = m.sb("rows_s", [128, 24 + 1024])
    wfm = m.sb("wfm_s", [128, 6, DC, 128], BF16); wtm = m.sb("wtm_s", [128, DC, 1032], BF16)
    m.op("dve", lambda e: e.memset(ones[:], 1.0), writes=["ones"])
    m.dma("sp", ident[:], id_d[:, :], writes=["ident"], semkey="c0"); m.dma("sp", triU[:], tri_d[:, :], writes=["triU"], semkey="c1")
    m.dma("sp", negm[:], nm_d[:, :], writes=["negm"], semkey="c2"); m.dma("sp", cw[:], cw_d[:, :, :], writes=["cw"], semkey="c3")
    m.dma("sp", cb[:], cb_d[:, :], writes=["cb"], semkey="c4")
    m.dma("sp", rows[:], rows_d[0:1, :].partition_broadcast(128), writes=["rows"], semkey="c5")
    for j in range(6):
        m.dma("pool", wfm[:, j, :, :], wfm_d[j], writes=["wfm"], semkey="wfm")
    m.dma("pool", wtm[:], wtm_d[:, :, :], writes=["wtm"], semkey="wtm")
    a_b = m.sb("a_b", [128, 8])
    m.op("act", lambda e: e.activation(out=a_b[:], in_=rows[:, 8:16], func=AF.Exp), reads=["rows"], writes=["a_b"])
    m.op("dve", lambda e: e.tensor_scalar(a_b[:], a_b[:], -1.0, None, ALU.mult), reads=["a_b"], writes=["a_b"])
    dtb = rows[:, 0:8]; dsk = rows[:, 24:24 + 512]; nwb = rows[:, 24 + 512:24 + 1024]

    xb = [m.sb("xb%d" % i, [128, DC, TB], BF16) for i in range(2)]
    xh = m.sb("xh", [128, 6, TB + 3]); xc = m.sb("xc", [128, 6, TB]); cacc = m.sb("cacc", [128, TB])
    hT = m.sb("hT", [128, 512])
    xs_tok = m.sb("xs_tok", [128, 512]); B_tok = m.sb("B_tok", [128, 128]); sm = m.sb("sm8", [128, 8, 8])
    tri_e = [m.sb("tri_e%d" % i, [128, 128]) for i in range(2)]
    cbT = m.sb("cbT", [128, 128]); decT = m.sb("decT", [128, 8, 128]); MT = m.sb("MT", [128, 8, 128])
    xdt = m.sb("xdt", [128, 512]); xw = m.sb("xw", [128, 512]); yi = m.sb("yi", [128, 512]); y = m.sb("y", [128, 512])
    zs = m.sb("zs", [128, 512]); gs = m.sb("gs", [128, 512]); junk = m.sb("junk", [128, 512]); ss = m.sb("ss", [128, 4])
    outt = [m.sb("outt%d" % i, [128, 512]) for i in range(2)]
    m.op("dve", lambda e: e.memset(hT[:], 0.0), writes=["hT"])
    m.op("dve", lambda e: e.memset(xh[:, :, 0:3], 0.0), writes=[("xh_halo", j) for j in range(6)])
    def bc(ap8):
        return ap8.unsqueeze(2).to_broadcast([128, 8, 64])
    v3 = lambda t: t[:].rearrange("p (e q) -> p e q", e=8)

    def load_x(mt):
        i = mt % 2
        m.dma("pool", xb[i][:], xT_v[:, :, mt * TB:(mt + 1) * TB], writes=["xb%d" % i], semkey="xb%d" % i)
    load_x(0)
    oc = 0
    for mt in range(NMT):
        if mt + 1 < NMT: load_x(mt + 1)
        x = xb[mt % 2]; xk = "xb%d" % (mt % 2)
        for j in range(6):
            p, pk = nextp()
            for c in range(DC):
                m.op("pe", lambda e, p=p, j=j, c=c: e.matmul(p[:], wfm[:, j, c, :], x[:, c, :], start=(c == 0), stop=(c == DC - 1)), reads=["wfm", xk], writes=[pk])
            m.op("act", lambda e, p=p, j=j: e.copy(xh[:, j, 3:TB + 3], p[:]), reads=[pk, ("xh_halo", j)], writes=[("xh", j)])
            m.op("dve", lambda e, j=j: e.tensor_scalar(cacc[:], xh[:, j, 0:TB], cw[:, j, 0:1], None, ALU.mult), reads=[("xh", j), ("xh_halo", j), "cw"], writes=["cacc"])
            for k in range(1, 4):
                m.op("dve", lambda e, j=j, k=k: e.scalar_tensor_tensor(cacc[:], xh[:, j, k:TB + k], cw[:, j, k:k + 1], cacc[:], ALU.mult, ALU.add),
                     reads=[("xh", j), ("xh_halo", j), "cw", "cacc"], writes=["cacc"])
            m.op("act", lambda e, j=j: e.activation(out=xc[:, j, :], in_=cacc[:], func=AF.Silu, bias=cb[:, j:j + 1]), reads=["cacc", "cb"], writes=[("xc", j)])
            m.op("pool", lambda e, j=j: e.tensor_copy(xh[:, j, 0:3], xh[:, j, TB:TB + 3]), reads=[("xh", j), "cacc"], writes=[("xh_halo", j)])
        for sub in range(NSUB):
            tsub = slice(sub * 128, (sub + 1) * 128); tg = slice(mt * TB + sub * 128, mt * TB + (sub + 1) * 128)
            p, pk = nextp()
            for j in range(4):
                m.op("pe", lambda e, p=p, j=j, tsub=tsub: e.transpose(p[:, j * 128:(j + 1) * 128], xc[:, j, tsub], ident[:]), reads=[("xc", j), "ident"], writes=[pk])
            m.op("act", lambda e, p=p: e.copy(xs_tok[:], p[:]), reads=[pk], writes=["xs_tok"])
            p, pk = nextp()
            m.op("pe", lambda e, p=p, tsub=tsub: e.transpose(p[:, 0:128], xc[:, 4, tsub], ident[:]), reads=[("xc", 4), "ident"], writes=[pk])
            m.op("act", lambda e, p=p: e.copy(B_tok[:], p[:, 0:128]), reads=[pk], writes=["B_tok"])
            p, pk = nextp()
            for c in range(DC):
                m.op("pe", lambda e, p=p, c=c, tsub=tsub: e.matmul(p[:, 0:8], x[:, c, tsub], wtm[:, c, 1024:1032], start=(c == 0), stop=(c == DC - 1)), reads=["wtm", xk], writes=[pk])
            m.op("dve", lambda e, p=p: e.tensor_tensor(sm[:, 7, :], p[:, 0:8], dtb, ALU.add), reads=[pk, "rows"], writes=[("sm", 7)])
            m.op("act", lambda e: e.activation(out=sm[:, 7, :], in_=sm[:, 7, :], func=AF.Exp), reads=[("sm", 7)], writes=[("sm", 7)])
            m.op("act", lambda e: e.activation(out=sm[:, 0, :], in_=sm[:, 7, :], func=AF.Ln, bias=1.0), reads=[("sm", 7)], writes=[("sm", 0)])
            m.op("dve", lambda e: e.tensor_tensor(sm[:, 1, :], sm[:, 0, :], a_b[:], ALU.mult), reads=[("sm", 0), "a_b"], writes=[("sm", 1)])
            p, pk = nextp()
            m.op("pe", lambda e, p=p: e.matmul(p[:, 0:8], triU[:], sm[:, 1, :], start=True, stop=True), reads=["triU", ("sm", 1)], writes=[pk])
            m.op("dve", lambda e, p=p: e.tensor_copy(sm[:, 2, :], p[:, 0:8]), reads=[pk], writes=[("sm", 2)])
            m.op("dve", lambda e, p=p: e.tensor_scalar(sm[:, 3, :], p[:, 0:8], -1.0, None, ALU.mult), reads=[pk], writes=[("sm", 3)])
            m.op("act", lambda e, p=p: e.activation(out=sm[:, 4, :], in_=p[:, 0:8], func=AF.Exp), reads=[pk], writes=[("sm", 4)])
            p2, p2k = nextp()
            m.op("pe", lambda e, p2=p2: e.matmul(p2[:, 0:8], ones[:], sm[:, 1, :], start=True, stop=True), reads=["ones", ("sm", 1)], writes=[p2k])
            m.op("act", lambda e, p2=p2: e.activation(out=sm[:, 5, :], in_=p2[:, 0:8], func=AF.Exp), reads=[p2k], writes=[("sm", 5)])
            m.op("dve", lambda e, p2=p2: e.tensor_tensor(sm[:, 7, :], p2[:, 0:8], sm[:, 2, :], ALU.subtract), reads=[p2k, ("sm", 2)], writes=[("sm", 7)])
            m.op("act", lambda e: e.activation(out=sm[:, 6, :], in_=sm[:, 7, :], func=AF.Exp), reads=[("sm", 7)], writes=[("sm", 6)])
            m.op("dve", lambda e: e.tensor_tensor(v3(xdt), v3(xs_tok), bc(sm[:, 0, :]), ALU.mult), reads=["xs_tok", ("sm", 0)], writes=["xdt"])
            m.op("pool", lambda e: e.tensor_tensor(v3(xw), v3(xdt), bc(sm[:, 6, :]), ALU.mult), reads=["xdt", ("sm", 6)], writes=["xw"])
            p, pk = nextp()
            m.op("pe", lambda e, p=p, tsub=tsub: e.matmul(p[:, 0:128], xc[:, 4, tsub], xc[:, 5, tsub], start=True, stop=True), reads=[("xc", 4), ("xc", 5)], writes=[pk])
            m.op("act", lambda e, p=p: e.copy(cbT[:], p[:, 0:128]), reads=[pk], writes=["cbT"])
            for half in range(2):
                p, pk = nextp()
                for q in range(4):
                    hd = half * 4 + q
                    te = tri_e[hd % 2]; tek = "tri_e%d" % (hd % 2)
                    m.op("dve", lambda e, te=te, hd=hd: e.tensor_scalar(te[:], triU[:], sm[:, 1, hd:hd + 1], None, ALU.mult), reads=["triU", ("sm", 1)], writes=[tek])
                    m.op("pe", lambda e, p=p, q=q, te=te: e.matmul(p[:, q * 128:(q + 1) * 128], ones[:], te[:], start=True, stop=False), reads=["ones", tek], writes=[pk])
                    m.op("pe", lambda e, p=p, q=q: e.matmul(p[:, q * 128:(q + 1) * 128], ident[:], negm[:], start=False, stop=True), reads=["ident", "negm"], writes=[pk])
                    m.op("act", lambda e, p=p, q=q, hd=hd: e.activation(out=decT[:, hd, :], in_=p[:, q * 128:(q + 1) * 128], func=AF.Exp, bias=sm[:, 3, hd:hd + 1]),
                         reads=[pk, ("sm", 3)], writes=[("decT", hd)])
                eng = "dve" if half == 0 else "pool"
                m.op(eng, lambda e, half=half: e.tensor_tensor(MT[:, half * 4:(half + 1) * 4, :], decT[:, half * 4:(half + 1) * 4, :],
                                                               cbT[:].unsqueeze(1).to_broadcast([128, 4, 128]), ALU.mult),
                     reads=[("decT", half * 4 + q) for q in range(4)] + ["cbT"], writes=[("MT", half)])
            pY, pYk = nextp()
            for hd in range(8):
                m.op("pe", lambda e, pY=pY, hd=hd: e.matmul(pY[:, hd * 64:(hd + 1) * 64], MT[:, hd, :], xdt[:, hd * 64:(hd + 1) * 64], start=True, stop=True),
                     reads=[("MT", hd // 4), "xdt"], writes=[pYk])
            pI, pIk = nextp()
            m.op("pe", lambda e, pI=pI, tsub=tsub: e.matmul(pI[:], xc[:, 5, tsub], hT[:], start=True, stop=True), reads=[("xc", 5), "hT"], writes=[pIk])
            m.op("dve", lambda e, pI=pI: e.tensor_tensor(v3(yi), pI[:].rearrange("p (e q) -> p e q", e=8), bc(sm[:, 4, :]), ALU.mult), reads=[pIk, ("sm", 4)], writes=["yi"])
            m.op("dve", lambda e, pY=pY: e.tensor_tensor(y[:], pY[:], yi[:], ALU.add), reads=[pYk, "yi"], writes=["y"])
            m.op("pool", lambda e: e.tensor_tensor(yi[:], xs_tok[:], dsk, ALU.mult), reads=["xs_tok", "rows", "yi"], writes=["yi"])
            m.op("dve", lambda e: e.tensor_tensor(y[:], y[:], yi[:], ALU.add), reads=["y", "yi"], writes=["y"])
            pS, pSk = nextp()
            m.op("pe", lambda e, pS=pS: e.matmul(pS[:], B_tok[:], xw[:], start=True, stop=True), reads=["B_tok", "xw"], writes=[pSk])
            m.op("pool", lambda e: e.tensor_tensor(v3(hT), v3(hT), bc(sm[:, 5, :]), ALU.mult), reads=["hT", ("sm", 5)], writes=["hT"])
            m.op("dve", lambda e, pS=pS: e.tensor_tensor(hT[:], hT[:], pS[:], ALU.add), reads=["hT", pSk], writes=["hT"])
            p, pk = nextp()
            for c in range(DC):
                m.op("pe", lambda e, p=p, c=c, tsub=tsub: e.matmul(p[:], x[:, c, tsub], wtm[:, c, 0:512], start=(c == 0), stop=(c == DC - 1)), reads=["wtm", xk], writes=[pk])
            m.op("act", lambda e, p=p: e.activation(out=zs[:], in_=p[:], func=AF.Silu), reads=[pk], writes=["zs"])
            m.op("dve", lambda e: e.tensor_tensor(y[:], y[:], zs[:], ALU.mult), reads=["y", "zs"], writes=["y"])
            m.op("act", lambda e: e.activation(out=junk[:], in_=y[:], func=AF.Square, accum_out=ss[:, 0:1]), reads=["y"], writes=["junk", "ss"])
            m.op("dve", lambda e: e.tensor_scalar(ss[:, 1:2], ss[:, 0:1], 1.0 / 512, RMS_EPS, ALU.mult, ALU.add), reads=["ss"], writes=["ss"])
            m.op("act", lambda e: e.activation(out=ss[:, 2:3], in_=ss[:, 1:2], func=AF.Sqrt), reads=["ss"], writes=["ss"])
            m.op("dve", lambda e: e.reciprocal(ss[:, 3:4], ss[:, 2:3]), reads=["ss"], writes=["ss"])
            m.op("dve", lambda e: e.scalar_tensor_tensor(y[:], y[:], ss[:, 3:4], nwb, ALU.mult, ALU.mult), reads=["y", "ss", "rows"], writes=["y"])
            p, pk = nextp()
            for c in range(DC):
                m.op("pe", lambda e, p=p, c=c, tsub=tsub: e.matmul(p[:], x[:, c, tsub], wtm[:, c, 512:1024], start=(c == 0), stop=(c == DC - 1)), reads=["wtm", xk], writes=[pk])
            m.op("act", lambda e, p=p: e.activation(out=gs[:], in_=p[:], func=AF.Sigmoid), reads=[pk], writes=["gs"])
            o = outt[oc % 2]; ok = "outt%d" % (oc % 2); oc += 1
            m.op("dve", lambda e, o=o: e.tensor_tensor(o[:], y[:], gs[:], ALU.mult), reads=["y", "gs"], writes=[ok])
            m.dma("sp", om[tg, :], o[:], reads=[ok], semkey=ok)
    if ctx:
        m.release(mark0)
    else:
        m.finish("sp")
    return nc, m


def ssd_inputs(layer, inp, b, g):
    o = 3 * 2048
    W = inp["w_in"][layer]
    xo = o + 2048
    cols_x = slice(xo + g * 512, xo + (g + 1) * 512); cols_B = slice(xo + 2048 + g * 128, xo + 2048 + (g + 1) * 128)
    cols_C = slice(xo + 2560 + g * 128, xo + 2560 + (g + 1) * 128)
    d = {"wfm": lay_w(np.concatenate([W[:, cols_x], W[:, cols_B], W[:, cols_C]], axis=1))}
    wt = np.concatenate([W[:, o + g * 512:o + (g + 1) * 512], W[:, g * 512:(g + 1) * 512], W[:, xo + 3072 + g * 8:xo + 3072 + (g + 1) * 8]], axis=1)
    d["wtm"] = np.ascontiguousarray(wt.reshape(DC, 128, 1032).transpose(1, 0, 2))
    ch = np.concatenate([np.arange(g * 512, (g + 1) * 512), np.arange(2048 + g * 128, 2048 + (g + 1) * 128), np.arange(2560 + g * 128, 2560 + (g + 1) * 128)])
    cwv = inp["ssd_conv_w"][layer][:, ch]
    d["cw"] = np.ascontiguousarray(cwv.T.reshape(6, 128, 4).transpose(1, 0, 2))
    d["cb"] = np.ascontiguousarray(inp["ssd_conv_b"][layer][ch].reshape(6, 128).T)
    hs = slice(g * 8, (g + 1) * 8)
    rows = np.concatenate([inp["ssd_dt_bias"][layer][hs], inp["ssd_a_log"][layer][hs], np.zeros(8, np.float32),
                           np.repeat(inp["ssd_d"][layer][hs], 64), inp["ssd_norm_w"][layer][g * 512:(g + 1) * 512]]).astype(np.float32)
    d["rows"] = rows[None, :]
    ii = np.arange(128)
    d["triU"] = (ii[:, None] <= ii[None, :]).astype(np.float32)
    d["negmask"] = np.where(ii[None, :] >= ii[:, None], 0.0, -30000.0).astype(np.float32)
    d["ident"] = np.eye(128, dtype=np.float32)
    return d


DEC_C = float(np.exp(-0.5))
GN_EPS = 64e-5


def build_rwkv(S, has_v, TB=512, TB2=256, ctx=None):
    NCH = 16 + (1 if has_v else 0)
    if ctx is None:
        nc = bass.Bass("TRN2", target_bir_lowering=False); m = MK(nc); pfx = ""
    else:
        nc, m, pfx = ctx["nc"], ctx["m"], ctx["pfx"]
    m.pfx = pfx
    mark0 = m.mark()
    dr = lambda name, shape, kind="ExternalInput", dt=F32: nc.dram_tensor(pfx + name, list(shape), dt, kind=kind).ap()
    xT = ctx["xT"] if ctx else dr("xT", [D, S])
    wfm_d = dr("wfm", [NCH, 128, DC, 128]); wg_d = dr("wg", [128, DC, 512])
    mu_d = dr("mu", [128, 17]); prm_d = dr("prm", [128, 4, 8]); rows_d = dr("rows", [1, 1024])
    w2_d = dr("w2", [96, 512]); a2_d = dr("a2", [96, 512]); g2_d = dr("g2", [256, 512])
    if has_v:
        v2_d = dr("v2", [64, 512])
    id_d = dr("ident", [128, 128]); bd_d = dr("blockdiag", [128, 128]); blk2_d = dr("blk8", [128, 4, 8]); hm_d = dr("hmask", [128, 2]); mask4_d = dr("mask4", [128, 512]); sl_d = dr("sl", [128, 128])
    if has_v:
        vfT = dr("vfT", [512, S])
    else:
        vf_out = dr("vf_out", [512, S], kind="ExternalOutput")
    om = dr("om", [S, 512], kind="ExternalOutput")
    shd = dr("shd", [NCH * 128, S], kind="Internal"); gate_d = dr("gate_s", [S, 512], kind="Internal")
    xT_v = xT.rearrange("(c p) t -> p c t", p=128)
    pb = ctx["pb"] if ctx else [m.ps("pb%d" % i, [128, 512]) for i in range(8)]
    pcnt = [0]
    NROT = [8]
    def nextp():
        i = pcnt[0] % NROT[0]; pcnt[0] += 1
        return pb[i], "pb%d" % i
    ident = m.sb("ident_s", [128, 128]); mu = m.sb("mu_s", [128, 17])
    m.dma("sp", ident[:], id_d[:, :], writes=["ident"], semkey="c0"); m.dma("sp", mu[:], mu_d[:, :], writes=["mu"], semkey="c1")
    mk1 = m.mark()
    wfm = m.sb("wfm_s", [128, NCH, DC, 128], BF16); wg = m.sb("wg_s", [128, DC, 512], BF16)
    for j in range(NCH):
        m.dma("pool", wfm[:, j, :, :], wfm_d[j], writes=["wfm"], semkey="wfm")
    m.dma("pool", wg[:], wg_d[:, :, :], writes=["wg"], semkey="wg")
    xb = [m.sb("xb%d" % i, [128, DC, TB], BF16) for i in range(2)]
    ph = [m.sb("ph%d" % i, [128, TB + 1]) for i in range(2)]; dd = [m.sb("dd%d" % i, [128, TB]) for i in range(2)]
    sh = [m.sb("sh%d" % i, [128, TB]) for i in range(3)]; halo = m.sb("halo", [128, 17]); gst = [m.sb("gst%d" % i, [128, 512]) for i in range(2)]
    m.op("dve", lambda e: e.memset(halo[:], 0.0), writes=[("halo", j) for j in range(17)])
    def load_x(mt):
        i = mt % 2
        m.dma("pool", xb[i][:], xT_v[:, :, mt * TB:(mt + 1) * TB], writes=["xb%d" % i], semkey="xb%d" % i)
    load_x(0)
    NMT = S // TB
    cc = 0; gc = 0
    for mt in range(NMT):
        ts = slice(mt * TB, (mt + 1) * TB)
        if mt + 1 < NMT: load_x(mt + 1)
        x = xb[mt % 2]; xk = "xb%d" % (mt % 2)
        for j in range(NCH):
            p, pk = nextp()
            for c in range(DC):
                m.op("pe", lambda e, p=p, j=j, c=c: e.matmul(p[:], wfm[:, j, c, :], x[:, c, :], start=(c == 0), stop=(c == DC - 1)), reads=["wfm", xk], writes=[pk])
            a = ph[cc % 2]; ak = "ph%d" % (cc % 2); d_ = dd[cc % 2]; dk = "dd%d" % (cc % 2); o = sh[cc % 3]; ok = "sh%d" % (cc % 3); cc += 1
            m.op("act", lambda e, a=a, p=p: e.copy(a[:, 1:TB + 1], p[:]), reads=[pk], writes=[ak])
            m.op("act", lambda e, a=a, j=j: e.copy(a[:, 0:1], halo[:, j:j + 1]), reads=[("halo", j)], writes=[ak + "h"])
            m.op("pool", lambda e, a=a, d_=d_: e.tensor_tensor(d_[:], a[:, 0:TB], a[:, 1:TB + 1], ALU.subtract), reads=[ak, ak + "h"], writes=[dk])
            m.op("dve", lambda e, a=a, d_=d_, o=o, j=j: e.scalar_tensor_tensor(o[:], d_[:], mu[:, j:j + 1], a[:, 1:TB + 1], ALU.mult, ALU.add), reads=[dk, ak, "mu"], writes=[ok])
            m.op("pool", lambda e, a=a, j=j: e.tensor_copy(halo[:, j:j + 1], a[:, TB:TB + 1]), reads=[ak], writes=[("halo", j)])
            m.dma("sp", shd[j * 128:(j + 1) * 128, ts], o[:], reads=[ok], semkey=ok)
            if (not has_v) and 8 <= j < 12:
                m.dma("sp", vf_out[(j - 8) * 128:(j - 7) * 128, ts], o[:], reads=[ok], semkey=ok)
        for sub in range(TB // 128):
            tsub = slice(sub * 128, (sub + 1) * 128); tg = slice(mt * TB + sub * 128, mt * TB + (sub + 1) * 128)
            p, pk = nextp()
            for c in range(DC):
                m.op("pe", lambda e, p=p, c=c, tsub=tsub: e.matmul(p[:], x[:, c, tsub], wg[:, c, :], start=(c == 0), stop=(c == DC - 1)), reads=["wg", xk], writes=[pk])
            o = gst[gc % 2]; ok = "gst%d" % (gc % 2); gc += 1
            m.op("act", lambda e, o=o, p=p: e.activation(out=o[:], in_=p[:], func=AF.Sigmoid), reads=[pk], writes=[ok])
            m.dma("sp", gate_d[tg, :], o[:], reads=[ok], semkey=ok)
    m.release(mk1)
    NROT[0] = 5; pcnt[0] = 0
    pY = pb[7]; pS = pb[6]; pBn = pb[5]
    m.psum_keys.update(["pY", "pS", "pBn"])
    NT2 = TB2 // 128
    prm = m.sb("prm_s", [128, 4, 8]); rows = m.sb("rows_s", [128, 1024]); bdg = m.sb("bdg", [128, 128]); blk2 = m.sb("blk8_s", [128, 4, 8]); hm = m.sb("hm_s", [128, 2])
    mask4 = m.sb("mask4_s", [128, 512]); sl = m.sb("sl_s", [128, 128]); onesr = m.sb("onesr", [128, 128])
    w2b = m.sb("w2b", [128, 512], BF16); a2b = m.sb("a2b", [128, 512], BF16); g2b = m.sb("g2b", [128, 2, 512], BF16); v2b = m.sb("v2b", [64, 512], BF16)
    m.dma("sp", prm[:], prm_d[:, :, :], writes=["prm"], semkey="c2"); m.dma("sp", rows[:], rows_d[0:1, :].partition_broadcast(128), writes=["rows"], semkey="c3")
    m.dma("sp", bdg[:], bd_d[:, :], writes=["bdg"], semkey="c4"); m.dma("sp", blk2[:], blk2_d[:, :, :], writes=["blk2"], semkey="c5"); m.dma("sp", hm[:], hm_d[:, :], writes=["hm"], semkey="c12")
    m.dma("sp", mask4[:], mask4_d[:, :], writes=["mask4"], semkey="c6"); m.dma("sp", sl[:], sl_d[:, :], writes=["sl"], semkey="c7")
    m.dma("pool", w2b[0:96, :], w2_d[:, :], writes=["w2b"], semkey="c8"); m.dma("pool", a2b[0:96, :], a2_d[:, :], writes=["a2b"], semkey="c9")
    m.dma("pool", g2b[:], g2_d.rearrange("(c p) n -> p c n", p=128), writes=["g2b"], semkey="c10")
    if has_v:
        m.dma("pool", v2b[:], v2_d[:, :], writes=["v2b"], semkey="c11")
    m.op("dve", lambda e: e.memset(onesr[:], 1.0), writes=["onesr"])
    lnw = rows[:, 0:512]; lnb = rows[:, 512:1024]
    PK, PA, PR, PW0, PA0, PV0 = range(6)
    rkv = [m.sb("rkv%d" % i, [128, 3, 4, TB2]) for i in range(2)]; lo = [m.sb("lo%d" % i, [128, 5, TB2]) for i in range(2)]
    vfb = [m.sb("vfb%d" % i, [128, 4, TB2]) for i in range(2)] if has_v else None
    tanhw = m.sb("tanhw", [128, TB2], BF16); alob = m.sb("alob", [128, TB2], BF16); sgl = m.sb("sgl", [128, 2, TB2], BF16); vlob = m.sb("vlob", [128, TB2], BF16)
    T = {n: m.sb("t_" + n, [128, TB2]) for n in ["lw", "al", "vm", "kk", "sq", "inv", "kkn", "km", "b", "cl", "ep", "em", "ew", "ee", "t1", "kh", "bh", "rkr"]}
    AR = m.sb("AR", [128, 4, NT2, 2, 128]); BK = m.sb("BK", [128, 4, NT2, 2, 128])
    khat = m.sb("khat", [128, NT2, 512]); bhat = m.sb("bhat", [128, NT2, 512]); Vtok = m.sb("Vtok", [128, NT2, 512])
    bonus = m.sb("bonus", [128, NT2, 8]); gamT = m.sb("gamT", [128, 4, NT2])
    M4 = m.sb("M4", [128, 4, 512]); Mpow = m.sb("Mpow", [128, 4, 6, 128]); Ncur = [m.sb("Ncur%d" % i, [128, 4, 128]) for i in range(2)]
    Ub = [m.sb("Ub%d" % i, [128, 4, 64]) for i in range(2)]; Uall = m.sb("Uall", [128, 512])
    ST = m.sb("ST", [128, 4, 64])
    ysb = m.sb("ysb", [128, 512]); ysq = m.sb("ysq", [128, 512]); gtok = m.sb("gtok", [128, 512]); gts = [m.sb("gts%d" % i, [128, 512]) for i in range(2)]
    st8 = m.sb("st8", [128, 6, 8]); bv = m.sb("bv", [128, 512]); outt = [m.sb("outt%d" % i, [128, 512]) for i in range(2)]
    m.op("dve", lambda e: e.memset(ST[:], 0.0), writes=["ST"])
    ARf = AR[:].rearrange("p a n b t -> p (a n b t)"); BKf = BK[:].rearrange("p a n b t -> p (a n b t)")
    arc = lambda pr, n, a: ((pr * NT2 + n) * 2 + a) * 128
    ARm = m.sb("ARm", [128, 4, NT2, 2, 2, 128]); ARmf = ARm[:].rearrange("p a n h b t -> p (a n h b t)")
    armc = lambda pr, n, hf, a: (((pr * NT2 + n) * 2 + hf) * 2 + a) * 128
    M4f = M4[:].rearrange("p h c -> p (h c)"); Mpf = Mpow[:].rearrange("p h l t -> p (h l t)")
    Ncf = [t[:].rearrange("p h t -> p (h t)") for t in Ncur]; Ubf = [t[:].rearrange("p h v -> p (h v)") for t in Ub]
    STf = ST[:].rearrange("p a v -> p (a v)")
    khf = khat[:].rearrange("p n c -> p (n c)"); bhf = bhat[:].rearrange("p n c -> p (n c)"); Vtf = Vtok[:].rearrange("p n c -> p (n c)")
    sglf = sgl[:].rearrange("p k t -> p (k t)"); g2f = g2b[:].rearrange("p k c -> p (k c)")
    vv = m.sb("vv", [128, TB2])
    def bc(ap8):
        return ap8.unsqueeze(2).to_broadcast([128, 8, 64])
    v3 = lambda a: a.rearrange("p (e q) -> p e q", e=8)
    tv = lambda a: a.rearrange("p (n t) -> p n t", n=NT2)
    NM2 = S // TB2
    def load2(i2):
        bi = i2 % 2; ts = slice(i2 * TB2, (i2 + 1) * TB2)
        for kind in range(3):
            m.dma("sp", rkv[bi][:, kind, :, :], shd[kind * 512:(kind + 1) * 512, ts].rearrange("(c p) t -> p c t", p=128), writes=[("rkv", bi, kind, pr) for pr in range(4)], semkey="rkv%d" % bi)
        nl = NCH - 12
        m.dma("sp", lo[bi][:, 0:nl, :], shd[1536:1536 + nl * 128, ts].rearrange("(c p) t -> p c t", p=128), writes=[("lo", bi)], semkey="lo%d" % bi)
        if has_v:
            m.dma("sp", vfb[bi][:], vfT[:, ts].rearrange("(c p) t -> p c t", p=128), writes=[("vfb", bi)], semkey="vfb%d" % bi)
    load2(0)
    oc = 0; gcn = 0
    for i2 in range(NM2):
        if i2 + 1 < NM2: load2(i2 + 1)
        bi = i2 % 2
        RK = rkv[bi]; LO = lo[bi]
        m.op("act", lambda e: e.activation(out=tanhw[:], in_=LO[:, 0, :], func=AF.Tanh), reads=[("lo", bi)], writes=["tanhw"])
        m.op("act", lambda e: e.copy(alob[:], LO[:, 1, :]), reads=[("lo", bi)], writes=["alob"])
        m.op("act", lambda e: e.activation(out=sgl[:], in_=LO[:, 2:4, :], func=AF.Sigmoid), reads=[("lo", bi)], writes=["sgl"])
        if has_v:
            m.op("act", lambda e: e.copy(vlob[:], LO[:, 4, :]), reads=[("lo", bi)], writes=["vlob"])
        for pr in range(4):
            cs = slice(pr * 128, (pr + 1) * 128)
            r_ = RK[:, 0, pr, :]; k_ = RK[:, 1, pr, :]; v_ = RK[:, 2, pr, :]
            rk_keys = [("rkv", bi, kind, pr) for kind in range(3)]
            P_ = lambda col: prm[:, pr, col:col + 1]
            p, pk = nextp()
            m.op("pe", lambda e, p=p, cs=cs: e.matmul(p[:, 0:TB2], w2b[0:96, cs], tanhw[0:96, :], start=True, stop=True), reads=["w2b", "tanhw"], writes=[pk])
            m.op("act", lambda e, p=p: e.activation(out=T["lw"][:], in_=p[:, 0:TB2], func=AF.Sigmoid, bias=P_(PW0)), reads=[pk, "prm"], writes=["lw"])
            m.op("pool", lambda e: e.tensor_scalar(T["lw"][:], T["lw"][:], -DEC_C, None, ALU.mult), reads=["lw"], writes=["lw"])
            p, pk = nextp()
            m.op("pe", lambda e, p=p, cs=cs: e.matmul(p[:, 0:TB2], a2b[0:96, cs], alob[0:96, :], start=True, stop=True), reads=["a2b", "alob"], writes=[pk])
            m.op("act", lambda e, p=p: e.activation(out=T["al"][:], in_=p[:, 0:TB2], func=AF.Sigmoid, bias=P_(PA0)), reads=[pk, "prm"], writes=["al"])
            if has_v:
                p, pk = nextp()
                m.op("pe", lambda e, p=p, cs=cs: e.matmul(p[:, 0:TB2], v2b[0:64, cs], vlob[0:64, :], start=True, stop=True), reads=["v2b", "vlob"], writes=[pk])
                m.op("act", lambda e, p=p: e.activation(out=T["vm"][:], in_=p[:, 0:TB2], func=AF.Sigmoid, bias=P_(PV0)), reads=[pk, "prm"], writes=["vm"])
                m.op("pool", lambda e: e.tensor_tensor(T["t1"][:], vfb[bi][:, pr, :], v_, ALU.subtract), reads=[("vfb", bi), ("rkv", bi, 2, pr)], writes=["t1"])
                m.op("pool", lambda e: e.tensor_tensor(T["t1"][:], T["t1"][:], T["vm"][:], ALU.mult), reads=["t1", "vm"], writes=["t1"])
                m.op("dve", lambda e: e.tensor_tensor(v_, v_, T["t1"][:], ALU.add), reads=["t1", ("rkv", bi, 2, pr)], writes=[("rkv", bi, 2, pr)])
            m.op("pool", lambda e: e.tensor_scalar(T["kk"][:], k_, P_(PK), None, ALU.mult), reads=[("rkv", bi, 1, pr), "prm"], writes=["kk"])
            m.op("act", lambda e: e.activation(out=T["sq"][:], in_=T["kk"][:], func=AF.Square), reads=["kk"], writes=["sq"])
            p, pk = nextp()
            m.op("pe", lambda e, p=p: e.matmul(p[:, 0:TB2], bdg[:], T["sq"][:], start=True, stop=True), reads=["bdg", "sq"], writes=[pk])
            m.op("act", lambda e, p=p: e.activation(out=T["inv"][:], in_=p[:, 0:TB2], func=AF.Sqrt), reads=[pk], writes=["inv"])
            m.op("dve", lambda e: e.tensor_scalar(T["inv"][:], T["inv"][:], 1e-12, None, ALU.max), reads=["inv"], writes=["inv"])
            m.op("dve", lambda e: e.reciprocal(T["inv"][:], T["inv"][:]), reads=["inv"], writes=["inv"])
            m.op("dve", lambda e: e.tensor_tensor(T["kkn"][:], T["kk"][:], T["inv"][:], ALU.mult), reads=["kk", "inv"], writes=["kkn"])
            m.op("dve", lambda e: e.tensor_scalar(T["t1"][:], T["al"][:], -1.0, P_(PA), ALU.add, ALU.mult), reads=["al", "prm", "t1"], writes=["t1"])
            m.op("dve", lambda e: e.scalar_tensor_tensor(T["km"][:], T["t1"][:], 1.0, k_, ALU.add, ALU.mult), reads=["t1", ("rkv", bi, 1, pr)], writes=["km"])
            m.op("pool", lambda e: e.tensor_tensor(T["b"][:], T["kkn"][:], T["al"][:], ALU.mult), reads=["kkn", "al"], writes=["b"])
            for n in range(NT2):
                tn = slice(n * 128, (n + 1) * 128)
                m.op("dve", lambda e, tn=tn: e.tensor_tensor_scan(T["cl"][:, tn], onesr[:], T["lw"][:, tn], 0.0, ALU.mult, ALU.add), reads=["lw", "onesr"], writes=[("cl", n)])
            clk = [("cl", n) for n in range(NT2)]
            m.op("act", lambda e: e.activation(out=T["ep"][:], in_=T["cl"][:], func=AF.Exp), reads=clk, writes=["ep"])
            m.op("act", lambda e: e.activation(out=T["em"][:], in_=T["cl"][:], func=AF.Exp, scale=-1.0), reads=clk, writes=["em"])
            m.op("pool", lambda e: e.tensor_tensor(T["ew"][:], T["cl"][:], T["lw"][:], ALU.subtract), reads=clk + ["lw"], writes=["ew"])
            m.op("act", lambda e: e.activation(out=T["ew"][:], in_=T["ew"][:], func=AF.Exp), reads=["ew"], writes=["ew"])
            m.op("dve", lambda e: e.tensor_tensor(AR[:, pr, :, 1, :], tv(r_), tv(T["ep"][:]), ALU.mult), reads=[("rkv", bi, 0, pr), "ep"], writes=[("AR", pr)])
            m.op("dve", lambda e: e.scalar_tensor_tensor(AR[:, pr, :, 0, :], tv(T["kkn"][:]), -1.0, tv(T["ew"][:]), ALU.mult, ALU.mult), reads=["kkn", "ew", ("AR", pr)], writes=[("AR", pr)])
            for hf in range(2):
                m.op("pool", lambda e, hf=hf: e.tensor_scalar(ARm[:, pr, :, hf, :, :], AR[:, pr, :, :, :], hm[:, hf:hf + 1], None, ALU.mult), reads=[("AR", pr), "hm", ("ARm", pr)], writes=[("ARm", pr)])
            m.op("pool", lambda e: e.tensor_tensor(BK[:, pr, :, 0, :], tv(T["b"][:]), tv(T["em"][:]), ALU.mult), reads=["b", "em"], writes=[("BK", pr)])
            m.op("pool", lambda e: e.tensor_tensor(BK[:, pr, :, 1, :], tv(T["km"][:]), tv(T["em"][:]), ALU.mult), reads=["km", "em", ("BK", pr)], writes=[("BK", pr)])
            for n in range(NT2):
                tn = slice(n * 128, (n + 1) * 128); last = n * 128 + 127
                m.op("act", lambda e, tn=tn, last=last: e.activation(out=T["ee"][:, tn], in_=T["cl"][:, tn], func=AF.Exp, scale=-1.0, bias=T["cl"][:, last:last + 1]),
                     reads=clk, writes=[("ee", n)])
                m.op("pool", lambda e, n=n, last=last: e.tensor_copy(gamT[:, pr, n:n + 1], T["ep"][:, last:last + 1]), reads=["ep"], writes=[("gamT", pr)])
            eek = [("ee", n) for n in range(NT2)]
            m.op("dve", lambda e: e.tensor_tensor(T["kh"][:], T["km"][:], T["ee"][:], ALU.mult), reads=["km"] + eek, writes=["kh"])
            m.op("pool", lambda e: e.tensor_tensor(T["bh"][:], T["b"][:], T["ee"][:], ALU.mult), reads=["b"] + eek, writes=["bh"])
            m.op("dve", lambda e: e.scalar_tensor_tensor(T["rkr"][:], r_, P_(PR), T["km"][:], ALU.mult, ALU.mult), reads=[("rkv", bi, 0, pr), "prm", "km"], writes=["rkr"])
            m.op("dve", lambda e: e.tensor_copy(vv[:], v_), reads=[("rkv", bi, 2, pr)], writes=["vv"])
            for n in range(NT2):
                tn = slice(n * 128, (n + 1) * 128)
                p, pk = nextp()
                m.op("pe", lambda e, p=p, tn=tn: e.transpose(p[:, 0:128], T["kh"][:, tn], ident[:]), reads=["kh", "ident"], writes=[pk])
                m.op("pe", lambda e, p=p, tn=tn: e.transpose(p[:, 128:256], T["bh"][:, tn], ident[:]), reads=["bh", "ident"], writes=[pk])
                m.op("pe", lambda e, p=p, tn=tn: e.transpose(p[:, 256:384], vv[:, tn], ident[:]), reads=["vv", "ident"], writes=[pk])
                m.op("act", lambda e, p=p, n=n, cs=cs: e.copy(khat[:, n, cs], p[:, 0:128]), reads=[pk], writes=[("khat", n)])
                m.op("act", lambda e, p=p, n=n, cs=cs: e.copy(bhat[:, n, cs], p[:, 128:256]), reads=[pk], writes=[("bhat", n)])
                m.op("dve", lambda e, p=p, n=n, cs=cs: e.tensor_copy(Vtok[:, n, cs], p[:, 256:384]), reads=[pk], writes=[("Vtok", n)])
                m.op("pe", lambda e, n=n, tn=tn: e.matmul(pBn[:, n * 8:(n + 1) * 8], T["rkr"][:, tn], blk2[:, pr, :], start=(pr == 0 and n == 0), stop=(pr == 3), skip_group_check=True), reads=["rkr", "blk2"], writes=["pBn"])
        m.op("act", lambda e: e.copy(bonus[:].rearrange("p n e -> p (n e)"), pBn[:, 0:NT2 * 8]), reads=["pBn"], writes=["bonus"])
        for n in range(NT2):
            tn = slice(n * 128, (n + 1) * 128); tg = slice(i2 * TB2 + n * 128, i2 * TB2 + (n + 1) * 128)
            gi_ = gcn % 2; gcn += 1
            m.dma("sp", gts[gi_][:], gate_d[tg, :], writes=["gts%d" % gi_], semkey="gts%d" % gi_)
            for grp in range(2):
                def opnd(hh):
                    h = grp * 4 + hh; pr = h // 2
                    return h, pr, h % 2
                for hh in range(4):
                    h, pr, prt = opnd(hh)
                    p, pk = nextp()
                    rhsAR = ARmf[:, armc(pr, n, prt, 0):armc(pr, n, prt, 0) + 256]
                    m.op("pe", lambda e, p=p, prt=prt, pr=pr, rhsAR=rhsAR: e.matmul(p[:, 0:256], BKf[:, arc(pr, n, 0):arc(pr, n, 0) + 128], rhsAR, start=True, stop=True), reads=[("BK", pr), ("ARm", pr)], writes=[pk])
                    m.op("pe", lambda e, p=p, prt=prt, pr=pr, rhsAR=rhsAR: e.matmul(p[:, 256:512], BKf[:, arc(pr, n, 1):arc(pr, n, 1) + 128], rhsAR, start=True, stop=True), reads=[("BK", pr), ("ARm", pr)], writes=[pk])
                    m.op("dve", lambda e, p=p, hh=hh: e.tensor_tensor(M4[:, hh, :], p[:], mask4[:], ALU.mult), reads=[pk, "mask4"], writes=[("M4", hh)])
                p, pk = nextp()
                for hh in range(4):
                    h, pr, prt = opnd(hh)
                    m.op("pe", lambda e, p=p, hh=hh, prt=prt, pr=pr: e.matmul(p[:, hh * 128:(hh + 1) * 128], ARmf[:, armc(pr, n, prt, 0):armc(pr, n, prt, 0) + 128], BKf[:, arc(pr, n, 0):arc(pr, n, 0) + 128], start=True, stop=True), reads=[("BK", pr), ("ARm", pr)], writes=[pk])
                m.op("dve", lambda e, p=p: e.tensor_tensor(Ncur[0][:], p[:].rearrange("p (h t) -> p h t", h=4), sl[:].unsqueeze(1).to_broadcast([128, 4, 128]), ALU.mult), reads=[pk, "sl"], writes=["Ncur0"])
                Mlev = lambda hh, lev: (M4f[:, hh * 512:hh * 512 + 128] if lev == 0 else Mpf[:, (hh * 6 + lev - 1) * 128:(hh * 6 + lev) * 128])
                mkey = lambda hh, lev: (("M4", hh) if lev == 0 else ("Mpow", lev))
                for lev in range(1, 7):
                    nprev = Ncf[(lev - 1) % 2]; nprevk = "Ncur%d" % ((lev - 1) % 2); nnew = Ncur[lev % 2]; nnewk = "Ncur%d" % (lev % 2)
                    p, pk = nextp()
                    for hh in range(4):
                        m.op("pe", lambda e, p=p, hh=hh, lev=lev, nprev=nprev: e.matmul(p[:, hh * 128:(hh + 1) * 128], nprev[:, hh * 128:(hh + 1) * 128], Mlev(hh, lev - 1), start=True, stop=True),
                             reads=[nprevk, mkey(hh, lev - 1)], writes=[pk])
                    m.op("act", lambda e, p=p, lev=lev: e.copy(Mpow[:, :, lev - 1, :], p[:].rearrange("p (h t) -> p h t", h=4)), reads=[pk], writes=[("Mpow", lev)])
                    if lev < 6:
                        p2, p2k = nextp()
                        for hh in range(4):
                            m.op("pe", lambda e, p2=p2, hh=hh, lev=lev, nprev=nprev: e.matmul(p2[:, hh * 128:(hh + 1) * 128], Mlev(hh, lev - 1), nprev[:, hh * 128:(hh + 1) * 128], start=True, stop=True),
                                 reads=[nprevk, mkey(hh, lev - 1)], writes=[p2k])
                        m.op("dve", lambda e, p2=p2, nnew=nnew: e.tensor_copy(nnew[:], p2[:].rearrange("p (h t) -> p h t", h=4)), reads=[p2k], writes=[nnewk])
                p, pk = nextp()
                for hh in range(4):
                    h, pr, prt = opnd(hh)
                    m.op("pe", lambda e, p=p, hh=hh, prt=prt, pr=pr: e.matmul(p[:, hh * 64:(hh + 1) * 64], ARmf[:, armc(pr, n, prt, 0):armc(pr, n, prt, 0) + 128], STf[:, pr * 64:(pr + 1) * 64], start=True, stop=False), reads=[("ARm", pr), "ST"], writes=[pk])
                    m.op("pe", lambda e, p=p, hh=hh, h=h: e.matmul(p[:, hh * 64:(hh + 1) * 64], M4f[:, hh * 512 + 256:hh * 512 + 384], Vtf[:, n * 512 + h * 64:n * 512 + (h + 1) * 64], start=False, stop=True), reads=[("M4", hh), ("Vtok", n)], writes=[pk])
                m.op("act", lambda e, p=p: e.copy(Ub[0][:].rearrange("p h v -> p (h v)"), p[:, 0:256]), reads=[pk], writes=["Ub0"])
                for lev in range(7):
                    uc = Ubf[lev % 2]; uck = "Ub%d" % (lev % 2)
                    p, pk = nextp()
                    for hh in range(4):
                        m.op("pe", lambda e, p=p, hh=hh, uc=uc: e.matmul(p[:, hh * 64:(hh + 1) * 64], ident[:], uc[:, hh * 64:(hh + 1) * 64], start=True, stop=False), reads=["ident", uck], writes=[pk])
                        m.op("pe", lambda e, p=p, hh=hh, uc=uc, lev=lev: e.matmul(p[:, hh * 64:(hh + 1) * 64], Mlev(hh, lev), uc[:, hh * 64:(hh + 1) * 64], start=False, stop=True), reads=[mkey(hh, lev), uck], writes=[pk])
                    if lev < 6:
                        un = Ub[(lev + 1) % 2]; unk = "Ub%d" % ((lev + 1) % 2)
                        eng = "act" if lev % 2 == 0 else "dve"
                        if eng == "act":
                            m.op("act", lambda e, p=p, un=un: e.copy(un[:].rearrange("p h v -> p (h v)"), p[:, 0:256]), reads=[pk], writes=[unk])
                        else:
                            m.op("dve", lambda e, p=p, un=un: e.tensor_copy(un[:].rearrange("p h v -> p (h v)"), p[:, 0:256]), reads=[pk], writes=[unk])
                    else:
                        m.op("act", lambda e, p=p, grp=grp: e.copy(Uall[:, grp * 256:(grp + 1) * 256], p[:, 0:256]), reads=[pk], writes=[("Uall", grp)])
                for hh in range(4):
                    h, pr, prt = opnd(hh)
                    hs = slice(h * 64, (h + 1) * 64)
                    m.op("pe", lambda e, hs=hs, prt=prt, pr=pr: e.matmul(pY[:, hs], ARmf[:, armc(pr, n, prt, 1):armc(pr, n, prt, 1) + 128], STf[:, pr * 64:(pr + 1) * 64], start=True, stop=False), reads=[("ARm", pr), "ST"], writes=["pY"])
                    m.op("pe", lambda e, hs=hs, hh=hh: e.matmul(pY[:, hs], M4f[:, hh * 512 + 128:hh * 512 + 256], Uall[:, hs], start=False, stop=False), reads=[("M4", hh), ("Uall", grp)], writes=["pY"])
                    m.op("pe", lambda e, hs=hs, hh=hh, h=h: e.matmul(pY[:, hs], M4f[:, hh * 512 + 384:hh * 512 + 512], Vtf[:, n * 512 + h * 64:n * 512 + (h + 1) * 64], start=False, stop=True), reads=[("M4", hh), ("Vtok", n)], writes=["pY"])
            for pr in range(4):
                cs = slice(pr * 128, (pr + 1) * 128)
                m.op("pe", lambda e, cs=cs, pr=pr: e.matmul(pS[:, cs], bhf[:, n * 512 + pr * 128:n * 512 + (pr + 1) * 128], Uall[:, cs], start=True, stop=False), reads=[("bhat", n), ("Uall", 0), ("Uall", 1)], writes=["pS"])
                m.op("pe", lambda e, cs=cs, pr=pr: e.matmul(pS[:, cs], khf[:, n * 512 + pr * 128:n * 512 + (pr + 1) * 128], Vtf[:, n * 512 + pr * 128:n * 512 + (pr + 1) * 128], start=False, stop=True), reads=[("khat", n), ("Vtok", n)], writes=["pS"])
            for pr in range(4):
                for hf in range(2):
                    prt = slice(hf * 64, hf * 64 + 64); c0 = pr * 128 + hf * 64
                    m.op("dve", lambda e, pr=pr, prt=prt, c0=c0: e.scalar_tensor_tensor(ST[prt, pr, :], ST[prt, pr, :], gamT[prt, pr, n:n + 1], pS[prt, c0:c0 + 64], ALU.mult, ALU.add),
                         reads=["ST", ("gamT", pr), "pS", "pY"], writes=["ST"])
            p, pk = nextp()
            for kc in range(2):
                m.op("pe", lambda e, p=p, kc=kc, tn=tn: e.matmul(p[:], sglf[:, kc * TB2 + tn.start:kc * TB2 + tn.stop], g2f[:, kc * 512:(kc + 1) * 512], start=(kc == 0), stop=(kc == 1)), reads=["sgl", "g2b"], writes=[pk])
            m.op("act", lambda e, p=p: e.copy(gtok[:], p[:]), reads=[pk], writes=["gtok"])
            m.op("act", lambda e: e.copy(ysb[:], pY[:]), reads=["pY"], writes=["ysb"])
            m.op("act", lambda e: e.activation(out=ysq[:], in_=ysb[:], func=AF.Square), reads=["ysb"], writes=["ysq"])
            m.op("dve", lambda e: e.tensor_reduce(st8[:, 0, :], v3(ysb[:]), AX.X, ALU.add), reads=["ysb"], writes=["st8"])
            m.op("dve", lambda e: e.tensor_reduce(st8[:, 1, :], v3(ysq[:]), AX.X, ALU.add), reads=["ysq", "st8"], writes=["st8"])
            m.op("dve", lambda e: e.tensor_scalar(st8[:, 2, :], st8[:, 0, :], 1.0 / 64, None, ALU.mult), reads=["st8"], writes=["st8"])
            m.op("dve", lambda e: e.tensor_tensor(st8[:, 3, :], st8[:, 2, :], st8[:, 2, :], ALU.mult), reads=["st8"], writes=["st8"])
            m.op("dve", lambda e: e.scalar_tensor_tensor(st8[:, 4, :], st8[:, 1, :], 1.0 / 64, st8[:, 3, :], ALU.mult, ALU.subtract), reads=["st8"], writes=["st8"])
            m.op("dve", lambda e: e.tensor_scalar(st8[:, 4, :], st8[:, 4, :], GN_EPS, None, ALU.add), reads=["st8"], writes=["st8"])
            m.op("act", lambda e: e.activation(out=st8[:, 5, :], in_=st8[:, 4, :], func=AF.Sqrt), reads=["st8"], writes=["st8"])
            m.op("dve", lambda e: e.reciprocal(st8[:, 5, :], st8[:, 5, :]), reads=["st8"], writes=["st8"])
            m.op("dve", lambda e: e.tensor_tensor(v3(ysb[:]), v3(ysb[:]), bc(st8[:, 2, :]), ALU.subtract), reads=["ysb", "st8"], writes=["ysb"])
            m.op("dve", lambda e: e.tensor_tensor(v3(ysb[:]), v3(ysb[:]), bc(st8[:, 5, :]), ALU.mult), reads=["ysb", "st8"], writes=["ysb"])
            m.op("pool", lambda e: e.tensor_tensor(ysb[:], ysb[:], lnw, ALU.mult), reads=["ysb", "rows"], writes=["ysb"])
            m.op("pool", lambda e: e.tensor_tensor(ysb[:], ysb[:], lnb, ALU.add), reads=["ysb", "rows"], writes=["ysb"])
            m.op("dve", lambda e: e.tensor_tensor(v3(bv[:]), v3(Vtok[:, n, :]), bc(bonus[:, n, :]), ALU.mult), reads=[("Vtok", n), "bonus"], writes=["bv"])
            m.op("pool", lambda e: e.tensor_tensor(ysb[:], ysb[:], bv[:], ALU.add), reads=["ysb", "bv"], writes=["ysb"])
            m.op("pool", lambda e: e.tensor_tensor(ysb[:], ysb[:], gtok[:], ALU.mult), reads=["ysb", "gtok"], writes=["ysb"])
            o = outt[oc % 2]; ok = "outt%d" % (oc % 2); oc += 1
            m.op("dve", lambda e, o=o: e.tensor_tensor(o[:], ysb[:], gts[gi_][:], ALU.mult), reads=["ysb", "gts%d" % gi_], writes=[ok])
            m.dma("sp", om[tg, :], o[:], reads=[ok], semkey=ok)
    if ctx:
        m.release(mark0)
    else:
        m.finish("sp")
    return nc, m


def rwkv_inputs(layer, inp, b, g):
    o = 3 * 2048 + 5152 + 1088
    W = inp["w_in"][layer]; mu = inp["rwkv_mu"][layer]
    has_v = layer > 0
    cs = slice(g * 512, (g + 1) * 512)
    def pad128(a):
        n = (-a.shape[-1]) % 128
        return np.concatenate([a, np.zeros(a.shape[:-1] + (n,), a.dtype)], axis=-1) if n else a
    cols = [W[:, o + g * 512:o + (g + 1) * 512], W[:, o + 2048 + g * 512:o + 2048 + (g + 1) * 512], W[:, o + 4096 + g * 512:o + 4096 + (g + 1) * 512],
            pad128(W[:, o + 6144:o + 6240]), pad128(W[:, o + 6240:o + 6336]), W[:, o + 6336:o + 6592]]
    mus = [mu[g * 512:(g + 1) * 512], mu[2048 + g * 512:2048 + (g + 1) * 512], mu[4096 + g * 512:4096 + (g + 1) * 512],
           pad128(mu[6144:6240]), pad128(mu[6240:6336]), mu[6336:6592]]
    if has_v:
        cols.append(pad128(inp["w_in_vres"][layer - 1])); mus.append(pad128(inp["rwkv_mu_vres"][layer - 1]))
    d = {"wfm": lay_w(np.concatenate(cols, axis=1))}
    muv = np.concatenate(mus)
    mu17 = np.zeros((128, 17), np.float32); mu17[:, :muv.size // 128] = muv.reshape(-1, 128).T
    d["mu"] = mu17
    gcol = 2048 + g * 512
    d["wg"] = np.ascontiguousarray(W[:, gcol:gcol + 512].reshape(DC, 128, 512).transpose(1, 0, 2))
    prm = np.zeros((128, 4, 8), np.float32)
    vecs = [inp["rwkv_k_k"][layer], inp["rwkv_k_a"][layer], inp["rwkv_r_k"][layer], inp["rwkv_w0"][layer], inp["rwkv_a0"][layer]]
    if has_v: vecs.append(inp["rwkv_v0"][layer - 1])
    for i, v in enumerate(vecs):
        prm[:, :, i] = v[cs].reshape(4, 128).T
    d["prm"] = prm
    d["rows"] = np.concatenate([inp["rwkv_ln_w"][layer][cs], inp["rwkv_ln_b"][layer][cs]])[None, :].astype(np.float32)
    d["w2"] = np.ascontiguousarray(inp["rwkv_w2"][layer][:, cs]); d["a2"] = np.ascontiguousarray(inp["rwkv_a2"][layer][:, cs])
    d["g2"] = np.ascontiguousarray(inp["rwkv_g2"][layer][:, cs])
    if has_v:
        d["v2"] = np.ascontiguousarray(inp["rwkv_v2"][layer - 1][:, cs])
    ii = np.arange(128)
    d["ident"] = np.eye(128, dtype=np.float32)
    d["blockdiag"] = ((ii[:, None] // 64) == (ii[None, :] // 64)).astype(np.float32)
    blk8 = np.zeros((128, 4, 8), np.float32)
    for pr in range(4):
        blk8[:64, pr, 2 * pr] = 1; blk8[64:, pr, 2 * pr + 1] = 1
    d["blk8"] = blk8
    hmk = np.zeros((128, 2), np.float32); hmk[:64, 0] = 1; hmk[64:, 1] = 1
    d["hmask"] = hmk
    su = (ii[:, None] < ii[None, :]).astype(np.float32); iu = (ii[:, None] <= ii[None, :]).astype(np.float32)
    d["mask4"] = np.concatenate([su, iu, su, iu], axis=1)
    d["sl"] = (ii[None, :] < ii[:, None]).astype(np.float32)
    return d


def build_mixers(S, has_v):
    nc = bass.Bass("TRN2", target_bir_lowering=False)
    m = MK(nc)
    xT = nc.dram_tensor("xT", [D, S], F32, kind="ExternalInput").ap()
    pb = [m.ps("pb%d" % i, [128, 512]) for i in range(8)]
    ctx = {"nc": nc, "m": m, "xT": xT, "pb": pb}
    build_ssd(S, ctx=dict(ctx, pfx="ssd_"))
    build_rwkv(S, has_v, ctx=dict(ctx, pfx="rwkv_"))
    build_mla(S, ctx=dict(ctx, pfx="mla_"))
    m.finish("sp")
    return nc, m


SEQ = 16384; NB = 2; NCORE = 8
_PROG = {}


def _prog(key, fn):
    if key not in _PROG:
        _PROG[key] = fn()[0]
    return _PROG[key]


def _launch(nc, in_maps):
    res = run_bass_kernel_spmd(nc, in_maps, core_ids=list(range(NCORE)))
    return res.results


def kernel(**inputs):
    import time, gc
    gc.disable()
    try:
        return _kernel(inputs)
    finally:
        gc.enable()


def _kernel(inputs):
    import time
    t00 = time.time()
    inp = {k: np.asarray(v) for k, v in inputs.items()}
    x = inp["x"]
    xT = [np.ascontiguousarray(x[b].T) for b in range(NB)]
    vf = [None] * NCORE
    names = ("ssd", "rwkv", "mla")
    mk_in = {"ssd": ssd_inputs, "rwkv": rwkv_inputs, "mla": mla_inputs}
    for layer in range(2):
        nc = _prog(("mix", layer > 0), lambda: build_mixers(SEQ, layer > 0))
        print("[kernel] layer %d mixers built (total %.1fs)" % (layer, time.time() - t00), flush=True)
        shared = {}
        maps = []
        for c in range(NCORE):
            b, g = c // 4, c % 4
            if g not in shared:
                dd = {}
                for name in names:
                    for k, v in mk_in[name](layer, inp, 0, g).items():
                        dd[name + "_" + k] = v
                shared[g] = dd
            d = dict(shared[g])
            d["mla_pos"] = np.ascontiguousarray(inp["positions"][b][None, :]).astype(np.int32)
            d["xT"] = xT[b]
            if layer > 0:
                d["rwkv_vfT"] = vf[c]
            maps.append(d)
        r = _launch(nc, maps)
        outs = {name: [r[c][name + "_om"] for c in range(NCORE)] for name in names}
        if layer == 0:
            vf = [r[c]["rwkv_vf_out"] for c in range(NCORE)]
        print("[kernel] layer %d mixers done (total %.1fs)" % (layer, time.time() - t00), flush=True)
        mT = {name: [np.ascontiguousarray(np.concatenate([outs[name][b * 4 + g] for g in range(4)], axis=1).T) for b in range(NB)] for name in outs}
        moe = (layer % 2 == 1)
        NTOK = SEQ // 4
        nc = _prog(("ffn", moe), lambda: build_ffn(NTOK, NEXP if moe else 0))
        base = ffn_inputs(layer, inp, moe)
        maps = []
        for c in range(NCORE):
            b, q = c // 4, c % 4
            ts = slice(q * NTOK, (q + 1) * NTOK)
            d = dict(base)
            d["xT"] = np.ascontiguousarray(xT[b][:, ts])
            for i, name in enumerate(names):
                d["m%d" % i] = np.ascontiguousarray(mT[name][b][:, ts])
            maps.append(d)
        r = _launch(nc, maps)
        xT = [np.ascontiguousarray(np.concatenate([r[b * 4 + q]["yT"] for q in range(4)], axis=1)) for b in range(NB)]
        print("[kernel] layer %d ffn done (total %.1fs)" % (layer, time.time() - t00), flush=True)
    out = np.stack([np.ascontiguousarray(xT[b].T) for b in range(NB)], axis=0).astype(np.float32)
    return out
```
